# Optimizing a Trainium2 kernel written in Bass

```python
import math
import jax, jax.numpy as jnp
from jax import lax
import numpy as np

D_MODEL = 1024
BATCH = 8
SEQ = 4096
DEPTH = 4

D_CONV = D_MODEL // 2
CONV_WIDTH = 31
MLSTM_HEADS = 4
D_MLSTM = D_MODEL // 2
MLSTM_HEAD_DIM = D_MLSTM // MLSTM_HEADS
MLSTM_CHUNK = 128
D_GMLP = D_MODEL // 2
GMLP_GROUPS = 4
GMLP_GROUP_DIM = D_GMLP // GMLP_GROUPS
GMLP_CHUNK = 128
SB_HEADS = 8
D_SB = D_MODEL // 2
SB_HEAD_DIM = D_SB // SB_HEADS
SB_QBLOCK = 128
D_FF = 2816
N_EXPERTS = 8
TOP_K = 2
D_MIX_EVEN = D_CONV + D_MLSTM
D_MIX_ODD = D_GMLP + D_SB
IN_EVEN = 2 * D_CONV + 4 * D_MLSTM + 2 * MLSTM_HEADS
IN_ODD = 2 * D_GMLP + 3 * D_SB
N_EVEN = (DEPTH + 1) // 2
N_ODD = DEPTH // 2
ALPHA = (2 * DEPTH) ** 0.25
BETA_INIT = (8 * DEPTH) ** -0.25
LN_EPS = 1e-5

kernel_name = "hybrid_conv_mlstm_gmlp_stickbreak_moe_deepnorm"


def layer_norm(x, g, b):
    x32 = x.astype(jnp.float32)
    mu = jnp.mean(x32, axis=-1, keepdims=True)
    var = jnp.mean(jnp.square(x32 - mu), axis=-1, keepdims=True)
    return ((x32 - mu) * lax.rsqrt(var + LN_EPS) * g + b).astype(x.dtype)


def swiglu(h, w1, w3, w2):
    return (jax.nn.silu(h @ w1) * (h @ w3)) @ w2


def causal_depthwise_conv(x, w, b):
    y = lax.conv_general_dilated(
        x, w[:, None, :].astype(x.dtype), window_strides=(1,),
        padding=[(CONV_WIDTH - 1, 0)], dimension_numbers=("NWC", "WIO", "NWC"),
        feature_group_count=x.shape[-1])
    return y + b.astype(x.dtype)


def mlstm_chunkwise(q, k, v, i_pre, f_pre):
    B, S, H, d = q.shape
    L = MLSTM_CHUNK
    nc = S // L
    f32 = jnp.float32

    def chunks(t):
        return t.astype(f32).reshape(B, nc, L, H, -1).transpose(0, 3, 1, 2, 4)

    qc, kc, vc = chunks(q), chunks(k) * (d ** -0.5), chunks(v)
    logf = chunks(jax.nn.log_sigmoid(f_pre.astype(f32))[..., None])[..., 0]
    ig = chunks(i_pre[..., None])[..., 0]
    b = jnp.cumsum(logf, axis=-1)
    g = b[..., -1]
    causal = jnp.tril(jnp.ones((L, L), bool))
    log_d = jnp.where(causal, b[..., :, None] - b[..., None, :] + ig[..., None, :], -jnp.inf)
    log_w = g[..., None] - b + ig
    a = jnp.max(log_w, axis=-1)
    w = jnp.exp(log_w - a[..., None])
    kv_loc = jnp.einsum("bhcld,bhcle->bhcde", kc * w[..., None], vc)
    n_loc = jnp.einsum("bhcl,bhcld->bhcd", w, kc)

    def step(carry, inp):
        C, n, m = carry
        kv_c, n_c, g_c, a_c = inp
        m_new = jnp.maximum(g_c + m, a_c)
        s_old = jnp.exp(g_c + m - m_new)
        s_new = jnp.exp(a_c - m_new)
        C_new = s_old[..., None, None] * C + s_new[..., None, None] * kv_c
        n_new = s_old[..., None] * n + s_new[..., None] * n_c
        return (C_new, n_new, m_new), (C, n, m)

    init = (jnp.zeros((B, H, d, d), f32), jnp.zeros((B, H, d), f32), jnp.zeros((B, H), f32))
    xs = (jnp.moveaxis(kv_loc, 2, 0), jnp.moveaxis(n_loc, 2, 0), jnp.moveaxis(g, 2, 0), jnp.moveaxis(a, 2, 0))
    _, (C_prev, n_prev, m_prev) = lax.scan(step, init, xs)
    C_prev = jnp.moveaxis(C_prev, 0, 2)
    n_prev = jnp.moveaxis(n_prev, 0, 2)
    m_prev = jnp.moveaxis(m_prev, 0, 2)

    log_inter = b + m_prev[..., None]
    m_t = jnp.maximum(log_inter, jnp.max(log_d, axis=-1))
    d_intra = jnp.exp(log_d - m_t[..., None])
    s_inter = jnp.exp(log_inter - m_t)
    qk = jnp.einsum("bhcld,bhcsd->bhcls", qc, kc) * d_intra
    num = jnp.einsum("bhcls,bhcse->bhcle", qk, vc) + s_inter[..., None] * jnp.einsum("bhcld,bhcde->bhcle", qc, C_prev)
    den = jnp.sum(qk, axis=-1) + s_inter * jnp.einsum("bhcld,bhcd->bhcl", qc, n_prev)
    h = num / jnp.maximum(jnp.abs(den), jnp.exp(-m_t))[..., None]
    return h.transpose(0, 2, 3, 1, 4).reshape(B, S, H, d)


def stick_breaking_attention(q, k, v):
    B, S, H, d = q.shape
    scale = d ** -0.5
    outs = []
    for blk in range(S // SB_QBLOCK):
        q0 = blk * SB_QBLOCK
        kend = q0 + SB_QBLOCK
        z = jnp.einsum("blhd,bshd->bhls", q[:, q0:kend], k[:, :kend]).astype(jnp.float32) * scale
        t_idx = q0 + jnp.arange(SB_QBLOCK)[:, None]
        s_idx = jnp.arange(kend)[None, :]
        mask = s_idx < t_idx
        log_1mb = jnp.where(mask, jax.nn.log_sigmoid(-z), 0.0)
        rc = lax.cumsum(log_1mb, axis=3, reverse=True) - log_1mb
        att = jnp.where(mask, jnp.exp(jax.nn.log_sigmoid(z) + rc), 0.0)
        outs.append(jnp.einsum("bhls,bshd->blhd", att, v[:, :kend].astype(jnp.float32)))
    return jnp.concatenate(outs, axis=1)


def conv_mlstm_mixer(h, w_in, gate_bias, conv_w, conv_b, norm_g, norm_b, head_g, w_out):
    B, S, _ = h.shape
    p = h @ w_in
    cuts = [D_CONV, 2 * D_CONV, 2 * D_CONV + D_MLSTM, 2 * D_CONV + 2 * D_MLSTM,
            2 * D_CONV + 3 * D_MLSTM, 2 * D_CONV + 4 * D_MLSTM]
    a_val, a_gate, q, k, v, o, gates = jnp.split(p, cuts, axis=-1)
    a = causal_depthwise_conv(a_val * jax.nn.sigmoid(a_gate), conv_w, conv_b)
    a = jax.nn.silu(layer_norm(a, norm_g, norm_b))
    gates = gates.astype(jnp.float32) + gate_bias
    i_pre, f_pre = gates[..., :MLSTM_HEADS], gates[..., MLSTM_HEADS:]
    shp = (B, S, MLSTM_HEADS, MLSTM_HEAD_DIM)
    hb = mlstm_chunkwise(q.reshape(shp), k.reshape(shp), v.reshape(shp), i_pre, f_pre)
    mu = jnp.mean(hb, axis=-1, keepdims=True)
    var = jnp.mean(jnp.square(hb - mu), axis=-1, keepdims=True)
    hb = ((hb - mu) * lax.rsqrt(var + LN_EPS)).reshape(B, S, D_MLSTM) * head_g
    hb = (jax.nn.sigmoid(o.astype(jnp.float32)) * hb).astype(h.dtype)
    return jnp.concatenate([a, hb], axis=-1) @ w_out


def gmlp_sb_mixer(h, w_in, v_g, v_b, w_s, b_s, w_out):
    B, S, _ = h.shape
    p = h @ w_in
    uv, q, k, v = jnp.split(p, [2 * D_GMLP, 2 * D_GMLP + D_SB, 2 * D_GMLP + 2 * D_SB], axis=-1)
    u, z = jnp.split(jax.nn.gelu(uv, approximate=False), 2, axis=-1)
    z = layer_norm(z, v_g, v_b)
    nc = S // GMLP_CHUNK
    z = z.reshape(B, nc, GMLP_CHUNK, GMLP_GROUPS, GMLP_GROUP_DIM)
    w_causal = jnp.where(jnp.tril(jnp.ones((GMLP_CHUNK, GMLP_CHUNK), bool)), w_s, 0.0).astype(z.dtype)
    sg = jnp.einsum("gts,bcsgk->bctgk", w_causal, z) + b_s.T[:, :, None].astype(z.dtype)
    c_out = u * sg.reshape(B, S, D_GMLP)
    shp = (B, S, SB_HEADS, SB_HEAD_DIM)
    d_out = stick_breaking_attention(q.reshape(shp), k.reshape(shp), v.reshape(shp))
    d_out = d_out.reshape(B, S, D_SB).astype(h.dtype)
    return jnp.concatenate([c_out, d_out], axis=-1) @ w_out


def moe_swiglu(h, router_w, router_b, w1, w3, w2):
    logits = (h @ router_w + router_b).astype(jnp.float32)
    top_val, top_idx = lax.top_k(logits, TOP_K)
    top_gate = jax.nn.softmax(top_val, axis=-1)
    combine = jnp.sum(jax.nn.one_hot(top_idx, N_EXPERTS, dtype=jnp.float32) * top_gate[..., None], axis=-2)
    combine = combine.astype(h.dtype)
    out = jnp.zeros_like(h)
    for e in range(N_EXPERTS):
        out = out + combine[..., e:e + 1] * swiglu(h, w1[e], w3[e], w2[e])
    return out


def setup_inputs(seed: int = 0) -> dict:
    key = jax.random.key(seed)
    ks = iter(jax.random.split(key, 40))

    def nrm(shape, scale):
        return jax.random.normal(next(ks), shape, jnp.float32) * scale

    def gain(shape):
        return 1.0 + nrm(shape, 0.02)

    H = MLSTM_HEADS
    i_bias = nrm((N_EVEN, H), 0.1)
    f_bias = jnp.linspace(3.0, 6.0, H, dtype=jnp.float32)[None, :] + nrm((N_EVEN, H), 0.1)
    return {
        "x": nrm((BATCH, SEQ, D_MODEL), 1.0),
        "ab_w_in": nrm((N_EVEN, D_MODEL, IN_EVEN), D_MODEL ** -0.5),
        "ab_gate_bias": jnp.concatenate([i_bias, f_bias], axis=-1),
        "a_conv_w": nrm((N_EVEN, CONV_WIDTH, D_CONV), CONV_WIDTH ** -0.5),
        "a_conv_b": nrm((N_EVEN, D_CONV), 0.02),
        "a_norm_g": gain((N_EVEN, D_CONV)),
        "a_norm_b": nrm((N_EVEN, D_CONV), 0.02),
        "b_norm_g": gain((N_EVEN, D_MLSTM)),
        "ab_w_out": nrm((N_EVEN, D_MIX_EVEN, D_MODEL), BETA_INIT * D_MIX_EVEN ** -0.5),
        "ab_ln1_g": gain((N_EVEN, D_MODEL)),
        "ab_ln1_b": nrm((N_EVEN, D_MODEL), 0.02),
        "ffn_w1": nrm((N_EVEN, D_MODEL, D_FF), D_MODEL ** -0.5),
        "ffn_w3": nrm((N_EVEN, D_MODEL, D_FF), D_MODEL ** -0.5),
        "ffn_w2": nrm((N_EVEN, D_FF, D_MODEL), BETA_INIT * D_FF ** -0.5),
        "ab_ln2_g": gain((N_EVEN, D_MODEL)),
        "ab_ln2_b": nrm((N_EVEN, D_MODEL), 0.02),
        "cd_w_in": nrm((N_ODD, D_MODEL, IN_ODD), D_MODEL ** -0.5),
        "c_norm_g": gain((N_ODD, D_GMLP)),
        "c_norm_b": nrm((N_ODD, D_GMLP), 0.02),
        "c_w_s": nrm((N_ODD, GMLP_GROUPS, GMLP_CHUNK, GMLP_CHUNK), GMLP_CHUNK ** -0.5),
        "c_b_s": gain((N_ODD, GMLP_GROUPS, GMLP_CHUNK)),
        "cd_w_out": nrm((N_ODD, D_MIX_ODD, D_MODEL), BETA_INIT * D_MIX_ODD ** -0.5),
        "cd_ln1_g": gain((N_ODD, D_MODEL)),
        "cd_ln1_b": nrm((N_ODD, D_MODEL), 0.02),
        "router_w": nrm((N_ODD, D_MODEL, N_EXPERTS), D_MODEL ** -0.5),
        "router_b": nrm((N_ODD, N_EXPERTS), 0.01),
        "moe_w1": nrm((N_ODD, N_EXPERTS, D_MODEL, D_FF), D_MODEL ** -0.5),
        "moe_w3": nrm((N_ODD, N_EXPERTS, D_MODEL, D_FF), D_MODEL ** -0.5),
        "moe_w2": nrm((N_ODD, N_EXPERTS, D_FF, D_MODEL), BETA_INIT * D_FF ** -0.5),
        "cd_ln2_g": gain((N_ODD, D_MODEL)),
        "cd_ln2_b": nrm((N_ODD, D_MODEL), 0.02),
    }


def reference(x, ab_w_in, ab_gate_bias, a_conv_w, a_conv_b, a_norm_g, a_norm_b, b_norm_g, ab_w_out,
              ab_ln1_g, ab_ln1_b, ffn_w1, ffn_w3, ffn_w2, ab_ln2_g, ab_ln2_b,
              cd_w_in, c_norm_g, c_norm_b, c_w_s, c_b_s, cd_w_out, cd_ln1_g, cd_ln1_b,
              router_w, router_b, moe_w1, moe_w3, moe_w2, cd_ln2_g, cd_ln2_b):
    for layer in range(DEPTH):
        j = layer // 2
        if layer % 2 == 0:
            mix = conv_mlstm_mixer(x, ab_w_in[j], ab_gate_bias[j], a_conv_w[j], a_conv_b[j],
                                   a_norm_g[j], a_norm_b[j], b_norm_g[j], ab_w_out[j])
            x = layer_norm(ALPHA * x + mix, ab_ln1_g[j], ab_ln1_b[j])
            x = layer_norm(ALPHA * x + swiglu(x, ffn_w1[j], ffn_w3[j], ffn_w2[j]), ab_ln2_g[j], ab_ln2_b[j])
        else:
            mix = gmlp_sb_mixer(x, cd_w_in[j], c_norm_g[j], c_norm_b[j], c_w_s[j], c_b_s[j], cd_w_out[j])
            x = layer_norm(ALPHA * x + mix, cd_ln1_g[j], cd_ln1_b[j])
            ffn = moe_swiglu(x, router_w[j], router_b[j], moe_w1[j], moe_w3[j], moe_w2[j])
            x = layer_norm(ALPHA * x + ffn, cd_ln2_g[j], cd_ln2_b[j])
    return x
```

```python
import math
from contextlib import ExitStack
import numpy as np
import concourse.bass as bass
import concourse.mybir as mybir
from concourse.bass_utils import run_bass_kernel_spmd

F32 = mybir.dt.float32
BF16 = mybir.dt.bfloat16
AF = mybir.ActivationFunctionType
ALU = mybir.AluOpType
AX = mybir.AxisListType

D = 1024
DFF = 2816
NFF = 22
NE = 8
DEPTH = 4
ALPHA = (2 * DEPTH) ** 0.25
EPS = 1e-5
NEG = -1.0e30
FGROUPS = [(0, 4), (4, 4), (8, 4), (12, 4), (16, 4), (20, 2)]

ENGS = ["pe", "act", "dve", "pool", "sp"]


class Dep:
    __slots__ = ("name", "lw", "rd", "sem", "dcount", "lastdma", "psum")

    def __init__(self, name):
        self.name = name
        self.psum = False
        self.lw = None
        self.rd = []
        self.sem = None
        self.dcount = 0
        self.lastdma = None


class Op:
    __slots__ = ("id", "eng", "fn", "deps", "dma", "signal", "sigval", "sem", "inc")

    def __init__(self, id, eng, fn, dma, inc):
        self.id = id
        self.eng = eng
        self.fn = fn
        self.deps = set()
        self.dma = dma
        self.signal = False
        self.sigval = 0
        self.sem = None
        self.inc = inc


class Buf:
    __slots__ = ("t", "d")

    def __init__(self, t, d):
        self.t = t
        self.d = d

    def __getitem__(self, k):
        return self.t[k]


SEMPOOL = {}
NDMASEM = 56


def make_sempool(nc, stack):
    pool = {"eng": {}, "dma": []}
    for e in ["pe", "act", "dve", "pool"]:
        pool["eng"][e] = [stack.enter_context(nc.semaphore("s_" + e)), 0]
    for i in range(NDMASEM):
        pool["dma"].append([stack.enter_context(nc.semaphore("d%d" % i)), 0])
    SEMPOOL[id(nc)] = pool


class Phase:
    def __init__(self, nc, name):
        self.nc = nc
        self.name = name
        self.ops = []
        self.by_eng = {e: [] for e in ENGS}
        self.dma_deps = []
        self.all_deps = []
        self.st = ExitStack()
        self.nsb = 0

    def dep(self, name):
        d = Dep(name)
        self.all_deps.append(d)
        return d

    def sb(self, name, shape, dt=F32):
        t = self.st.enter_context(self.nc.sbuf_tensor(self.name + "_" + name, list(shape), dt))
        return Buf(t, self.dep(name))

    def ps(self, name, shape, dt=F32):
        t = self.st.enter_context(self.nc.psum_tensor(self.name + "_" + name, list(shape), dt))
        b = Buf(t, self.dep(name))
        b.d.psum = True
        return b

    def region(self, name):
        return Buf(None, self.dep(name))

    def op(self, eng, fn, R=(), W=(), dma=None, inc=16):
        o = Op(len(self.ops), eng, fn, dma, inc)
        self.ops.append(o)
        self.by_eng[eng].append(o)
        deps = o.deps
        if any(b.d.psum for b in R):
            W = list(W) + [b for b in R if b.d.psum]
            R = [b for b in R if not b.d.psum]
        for b in R:
            t = b.d
            if t.lw is not None:
                deps.add(t.lw)
        for b in W:
            t = b.d
            if t.lw is not None:
                deps.add(t.lw)
            deps.update(t.rd)
        if dma is not None:
            dd = dma.d
            if dd.lastdma is not None:
                deps.add(dd.lastdma)
            if fn is not None:
                dd.lastdma = o.id
            if dd.sem is None:
                dd.sem = True
                self.dma_deps.append(dd)
        deps.discard(o.id)
        best = {}
        for d in deps:
            od = self.ops[d]
            if od.dma is None and od.fn is not None:
                if d > best.get(od.eng, -1):
                    best[od.eng] = d
        for d in list(deps):
            od = self.ops[d]
            if od.dma is None and od.fn is not None and best[od.eng] != d:
                deps.discard(d)
        if eng == "pe":
            for d in list(deps):
                od = self.ops[d]
                if od.eng == "pe" and od.dma is None:
                    deps.discard(d)
        for d in deps:
            self.ops[d].signal = True
        if fn is not None:
            for b in R:
                b.d.rd.append(o.id)
            for b in W:
                b.d.lw = o.id
                b.d.rd = []
        return o

    def pe(self, fn, R=(), W=()):
        return self.op("pe", fn, R, W)

    def act(self, fn, R=(), W=()):
        return self.op("act", fn, R, W)

    def dve(self, fn, R=(), W=()):
        return self.op("dve", fn, R, W)

    def pool(self, fn, R=(), W=()):
        return self.op("pool", fn, R, W)

    def dma(self, q, out_ap, in_ap, R, W, semb):
        return self.op(q, lambda e: e.dma_start(out=out_ap, in_=in_ap), R, W, dma=semb)

    def mm(self, out_ap, lhsT, rhs, start, stop, R, W):
        return self.op("pe", lambda e: e.matmul(out_ap, lhsT=lhsT, rhs=rhs, start=start, stop=stop), R, W)

    def tr(self, out_ap, in_ap, ident_ap, R, W):
        return self.op("pe", lambda e: e.transpose(out=out_ap, in_=in_ap, identity=ident_ap), R, W)

    def barrier(self):
        allb = [Buf(None, d) for d in self.all_deps]
        for e in ENGS:
            self.op(e, None, R=(), W=allb)

    def finish(self):
        nc = self.nc
        st = self.st
        allb = [Buf(None, d) for d in self.all_deps]
        for e in ENGS:
            self.op(e, None, R=(), W=allb)
        pool = SEMPOOL[id(nc)]
        engsem = {}
        cnt = {e: 0 for e in ENGS}
        for e in ["pe", "act", "dve", "pool"]:
            engsem[e] = pool["eng"][e][0]
            cnt[e] = pool["eng"][e][1]
        assert len(self.dma_deps) <= len(pool["dma"]), len(self.dma_deps)
        for i, d in enumerate(self.dma_deps):
            d.sem = pool["dma"][i][0]
            d.dcount = pool["dma"][i][1]
        for o in self.ops:
            if o.fn is None:
                continue
            if o.dma is not None:
                o.dma.d.dcount += o.inc
                o.sigval = o.dma.d.dcount
                o.sem = o.dma.d.sem
            elif o.signal:
                cnt[o.eng] += 1
                o.sigval = cnt[o.eng]
                o.sem = engsem[o.eng]
        ops = self.ops

        def run(eng_name):
            def body(e):
                seen = {}
                for o in self.by_eng[eng_name]:
                    need = {}
                    for d in o.deps:
                        od = ops[d]
                        if od.fn is None:
                            continue
                        key = id(od.sem)
                        if od.sigval > need.get(key, (0, None))[0]:
                            need[key] = (od.sigval, od.sem)
                    for key, (val, sem) in need.items():
                        if seen.get(key, 0) >= val:
                            continue
                        e.wait_ge(sem, val)
                        seen[key] = val
                    if o.fn is None:
                        continue
                    ins = o.fn(e)
                    if o.dma is not None:
                        ins.then_inc(o.sem, o.inc)
                    elif o.signal:
                        ins.then_inc(o.sem, 1)
            return body

        for e in ["pe", "act", "dve", "pool"]:
            pool["eng"][e][1] = cnt[e]
        for i, d in enumerate(self.dma_deps):
            pool["dma"][i][1] = d.dcount
        block = st.enter_context(nc.Block())
        block.tensor(run("pe"))
        block.scalar(run("act"))
        block.vector(run("dve"))
        block.gpsimd(run("pool"))
        block.sync(run("sp"))
        st.close()


class Ctx:
    pass


def load_rows(ph, q, buf, dram_row_ap, n):
    ph.dma(q, buf[:, 0:n], dram_row_ap.partition_broadcast(128), [], [buf], buf)


def ln_rows(ph, s, mv, stats, rstd, nb, width, eps=EPS):
    nchunk = (width + 511) // 512
    for h in range(nchunk):
        lo, hi = h * 512, min(width, (h + 1) * 512)
        ph.dve(lambda e, h=h, lo=lo, hi=hi: e.bn_stats(out=stats[:, h, :], in_=s[:, lo:hi]), [s], [stats])
    ph.dve(lambda e: e.bn_aggr(out=mv[:, :], in_=stats[:, 0:nchunk, :].rearrange("p a b -> p (a b)")), [stats], [mv])
    ph.dve(lambda e: e.tensor_scalar(out=rstd[:, :], in0=mv[:, 1:2], scalar1=eps, scalar2=None, op0=ALU.add), [mv], [rstd])
    ph.act(lambda e: e.activation(out=rstd[:, :], in_=rstd[:, :], func=AF.Sqrt), [rstd], [rstd])
    ph.dve(lambda e: e.reciprocal(out=rstd[:, :], in_=rstd[:, :]), [rstd], [rstd])
    ph.dve(lambda e: e.scalar_tensor_tensor(out=nb[:, :], in0=mv[:, 0:1], scalar=-1.0, in1=rstd[:, :], op0=ALU.mult, op1=ALU.mult),
           [mv, rstd], [nb])


class Epi:
    def __init__(self, ph, cx, g_row, b_row, last, router=None, nbuf=2):
        self.ph = ph
        self.cx = cx
        self.last = last
        self.router = router
        self.grow = ph.sb("e_g", [128, D]); load_rows(ph, "sp", self.grow, g_row, D)
        self.brow = ph.sb("e_b", [128, D]); load_rows(ph, "sp", self.brow, b_row, D)
        self.nbuf = nbuf
        self.s = [ph.sb("e_s%d" % i, [128, D]) for i in range(nbuf)]
        self.y = [ph.sb("e_y%d" % i, [128, D]) for i in range(nbuf)]
        self.yb = [ph.sb("e_yb%d" % i, [128, D], BF16) for i in range(nbuf)]
        self.ya = self.s
        self.stats = ph.sb("e_st", [128, 2, 6]); self.mv = ph.sb("e_mv", [128, 2])
        self.rstd = ph.sb("e_rstd", [128, 1]); self.nb = ph.sb("e_nb", [128, 1])
        self.xTs = [ph.sb("e_xT%d" % i, [128, 8, 512], BF16) for i in range(nbuf)]
        self.k = 0
        if router is not None:
            self.rw = ph.sb("r_w", [128, 8, NE]); ph.dma("sp", self.rw[:, :, :], router["w"].rearrange("(c p) e -> p c e", p=128), [], [self.rw], self.rw)
            self.rb = ph.sb("r_b", [128, NE]); load_rows(ph, "sp", self.rb, router["b"], NE)
            self.yT = ph.sb("r_yT", [128, 8, 128])
            self.lg = ph.sb("r_lg", [128, NE]); self.top = ph.sb("r_top", [128, 8])
            self.g1 = ph.sb("r_g1", [128, 1]); self.g2 = ph.sb("r_g2", [128, 1]); self.dd = ph.sb("r_dd", [128, 1])
            self.c1 = ph.sb("r_c1", [128, NE]); self.c2 = ph.sb("r_c2", [128, NE])

    def run(self, n, res_ap_fn, res_R, idb, idf, trp, trf, rlp):
        ph, cx = self.ph, self.cx
        k = self.k; self.k += 1
        nbuf = self.nbuf
        s = self.s[k % nbuf]; y = self.y[k % nbuf]; yb = self.yb[k % nbuf]; ya = self.ya[k % nbuf]
        xTs = self.xTs[(n // 4) % nbuf]
        accr = cx.acc_dep[n]
        ph.dma("sp", s[:, :], cx.acc[n * 128:(n + 1) * 128, :], [accr], [s], s)
        if res_ap_fn is not None:
            for h in range(2):
                ph.dve(lambda e, h=h: e.tensor_tensor(out=s[:, h * 512:(h + 1) * 512], in0=res_ap_fn(h * 512, (h + 1) * 512),
                                                      in1=s[:, h * 512:(h + 1) * 512], op=ALU.add), [s] + list(res_R), [s])
        ln_rows(ph, s, self.mv, self.stats, self.rstd, self.nb, D)
        ph.act(lambda e: e.activation(out=y[:, :], in_=s[:, :], func=AF.Identity, bias=self.nb[:, :], scale=self.rstd[:, :]),
               [s, self.nb, self.rstd], [y])
        ph.pool(lambda e: e.tensor_tensor(out=y[:, :], in0=y[:, :], in1=self.grow[:, :], op=ALU.mult), [y, self.grow], [y])
        ph.pool(lambda e: e.tensor_tensor(out=y[:, :], in0=y[:, :], in1=self.brow[:, :], op=ALU.add), [y, self.brow], [y])
        if self.last:
            ph.dma("sp", cx.out[n * 128:(n + 1) * 128, :], y[:, :], [y], [cx.out_dep[n]], y)
            return
        ph.act(lambda e: e.activation(out=ya[:, :], in_=y[:, :], func=AF.Copy, scale=ALPHA), [y], [ya])
        ph.dma("sp", cx.acc[n * 128:(n + 1) * 128, :], ya[:, :], [ya], [accr], ya)
        ph.act(lambda e: e.activation(out=yb[:, :], in_=y[:, :], func=AF.Copy), [y], [yb])
        for c in range(8):
            ph.tr(trp[:, c * 128:(c + 1) * 128], yb[:, c * 128:(c + 1) * 128], idb[:, :], [yb, idb], [trp])
        sub = n % 4
        ph.dve(lambda e: e.tensor_copy(out=xTs[:, :, sub * 128:(sub + 1) * 128], in_=trp[:, :].rearrange("p (c t) -> p c t", c=8)),
               [trp], [xTs])
        if sub == 3:
            t0 = (n // 4) * 512
            ph.dma("sp", cx.xT.rearrange("c p s -> p c s")[:, :, t0:t0 + 512], xTs[:, :, :], [xTs], [cx.xT_dep[n // 4]], xTs)
        if self.router is not None:
            for half in range(2):
                tf = trf[half]
                for c in range(4):
                    cc = half * 4 + c
                    ph.tr(tf[:, c * 128:(c + 1) * 128], y[:, cc * 128:(cc + 1) * 128], idf[:, :], [y, idf], [tf])
                ph.act(lambda e, half=half, tf=tf: e.activation(out=self.yT[:, half * 4:(half + 1) * 4, :],
                                                                in_=tf[:, :].rearrange("p (c t) -> p c t", c=4), func=AF.Copy),
                       [tf], [self.yT])
            for c in range(8):
                ph.mm(rlp[:, 0:NE], self.yT[:, c, :], self.rw[:, c, :], c == 0, c == 7, [self.yT, self.rw], [rlp])
            ph.dve(lambda e: e.tensor_tensor(out=self.lg[:, :], in0=rlp[:, 0:NE], in1=self.rb[:, :], op=ALU.add), [rlp, self.rb], [self.lg])
            ph.dve(lambda e: e.max(out=self.top[:, :], in_=self.lg[:, :]), [self.lg], [self.top])
            ph.dve(lambda e: e.tensor_tensor(out=self.dd[:, :], in0=self.top[:, 0:1], in1=self.top[:, 1:2], op=ALU.subtract), [self.top], [self.dd])
            ph.act(lambda e: e.activation(out=self.g1[:, :], in_=self.dd[:, :], func=AF.Sigmoid), [self.dd], [self.g1])
            ph.dve(lambda e: e.tensor_scalar(out=self.g2[:, :], in0=self.g1[:, :], scalar1=-1.0, scalar2=1.0, op0=ALU.mult, op1=ALU.add),
                   [self.g1], [self.g2])
            ph.dve(lambda e: e.tensor_scalar(out=self.c1[:, :], in0=self.lg[:, :], scalar1=self.top[:, 0:1], scalar2=self.g1[:, :],
                                             op0=ALU.is_equal, op1=ALU.mult), [self.lg, self.top, self.g1], [self.c1])
            ph.dve(lambda e: e.tensor_scalar(out=self.c2[:, :], in0=self.lg[:, :], scalar1=self.top[:, 1:2], scalar2=self.g2[:, :],
                                             op0=ALU.is_equal, op1=ALU.mult), [self.lg, self.top, self.g2], [self.c2])
            ph.dve(lambda e: e.tensor_tensor(out=self.c1[:, :], in0=self.c1[:, :], in1=self.c2[:, :], op=ALU.add), [self.c1, self.c2], [self.c1])
            ph.dma("sp", cx.comb[n * 128:(n + 1) * 128, :], self.c1[:, :], [self.c1], [cx.comb_dep], self.c1)


def load_consts(ph, cx, ncols=None):
    ncols = ncols or cx.NCST
    c = ph.sb("cst", [128, ncols])
    ph.dma("sp", c[:, :], cx.cst[:, 0:ncols], [], [c], c)
    idb = ph.sb("idb", [128, 128], BF16)
    ph.dve(lambda e: e.tensor_copy(out=idb[:, :], in_=c[:, 0:128]), [c], [idb])
    return c, idb


def phase_prologue(nc, cx):
    ph = Phase(nc, "p0")
    mk_deps(ph, cx)
    c, idb = load_consts(ph, cx, 128)
    trp = ph.ps("trp", [128, 1024], BF16)
    xs = [ph.sb("x%d" % i, [128, D]) for i in range(2)]
    xa = [ph.sb("xa%d" % i, [128, D]) for i in range(2)]
    xb = [ph.sb("xb%d" % i, [128, D], BF16) for i in range(2)]
    xTs = [ph.sb("xT%d" % i, [128, 8, 512], BF16) for i in range(2)]
    for n in range(cx.NT):
        x = xs[n % 2]; a = xa[n % 2]; b = xb[n % 2]; xt = xTs[(n // 4) % 2]
        ph.dma("sp", x[:, :], cx.x[n * 128:(n + 1) * 128, :], [], [x], x)
        ph.act(lambda e, x=x, a=a: e.activation(out=a[:, :], in_=x[:, :], func=AF.Copy, scale=ALPHA), [x], [a])
        ph.dma("sp", cx.acc[n * 128:(n + 1) * 128, :], a[:, :], [a], [cx.acc_dep[n]], a)
        ph.dve(lambda e, x=x, b=b: e.tensor_copy(out=b[:, :], in_=x[:, :]), [x], [b])
        for cc in range(8):
            ph.tr(trp[:, cc * 128:(cc + 1) * 128], b[:, cc * 128:(cc + 1) * 128], idb[:, :], [b, idb], [trp])
        sub = n % 4
        ph.dve(lambda e, xt=xt, sub=sub: e.tensor_copy(out=xt[:, :, sub * 128:(sub + 1) * 128],
                                                        in_=trp[:, :].rearrange("p (c t) -> p c t", c=8)), [trp], [xt])
        if sub == 3:
            t0 = (n // 4) * 512
            ph.dma("sp", cx.xT.rearrange("c p s -> p c s")[:, :, t0:t0 + 512], xt[:, :, :], [xt], [cx.xT_dep[n // 4]], xt)
    ph.finish()


def phase_ffn(nc, cx, name, w1s, w3s, w2s, ln_g, ln_b, last, moe):
    ph = Phase(nc, name)
    mk_deps(ph, cx)
    c, idb = load_consts(ph, cx, 128)
    idf = Buf(c.t[:, 0:128], c.d)
    NEXP = len(w1s)
    w1g = [ph.sb("w1g%d" % g, [128, 8, n * 128], BF16) for g, (s0, n) in enumerate(FGROUPS)]
    w3g = [ph.sb("w3g%d" % g, [128, 8, n * 128], BF16) for g, (s0, n) in enumerate(FGROUPS)]
    w2g = [ph.sb("w2g%d" % g, [128, n, D], BF16) for g, (s0, n) in enumerate(FGROUPS)]

    import os
    noroll = bool(os.environ.get("NOROLL"))

    def load_w(e):
        xr = [acct[0]] if (noroll and e > 0) else []
        for g, (s0, n) in enumerate(FGROUPS):
            ph.dma("pool", w1g[g][:, :, :], w1s[e][:, s0 * 128:(s0 + n) * 128].rearrange("(c p) n -> p c n", p=128), xr, [w1g[g]], w1g[g])
            ph.dma("pool", w3g[g][:, :, :], w3s[e][:, s0 * 128:(s0 + n) * 128].rearrange("(c p) n -> p c n", p=128), xr, [w3g[g]], w3g[g])
        for g, (s0, n) in enumerate(FGROUPS):
            ph.dma("pool", w2g[g][:, :, :], w2s[e][s0 * 128:(s0 + n) * 128, :].rearrange("(c p) n -> p c n", p=128), xr, [w2g[g]], w2g[g])

    epi = Epi(ph, cx, ln_g, ln_b, last, nbuf=1)
    xTt = [ph.sb("xTt%d" % i, [128, 8, 512], BF16) for i in range(2)]
    gT = ph.sb("gT", [128, NFF, 512], BF16)
    gdeps = [Buf(gT.t, ph.dep("gT%d" % f)) for f in range(NFF)]
    sg = [ph.sb("sg%d" % i, [128, 512]) for i in range(2)]
    acct = [ph.sb("acct%d" % i, [128, D]) for i in range(1)]
    comb = None
    if moe:
        comb = ph.sb("comb", [128, cx.NT, NE])
        ph.dma("sp", comb[:, :, :], cx.comb.rearrange("(n p) e -> p n e", p=128), [cx.comb_dep], [comb], comb)
    hp = [ph.ps("hp%d" % i, [128, 512]) for i in range(4)]
    op = [ph.ps("op%d" % i, [128, 512]) for i in range(2)]
    trp = ph.ps("trp", [128, 1024], BF16)
    k = 0
    for e in range(NEXP):
        load_w(e)
        for tt in range(cx.NTT):
            xt = xTt[k % 2]
            ph.dma("sp", xt[:, :, :], cx.xT.rearrange("c p s -> p c s")[:, :, tt * 512:(tt + 1) * 512], [cx.xT_dep[tt]], [xt], xt)
            for f in range(NFF):
                g = min(f // 4, 5); fo = (f - FGROUPS[g][0]) * 128
                h1 = hp[(2 * f) % 4]; h3 = hp[(2 * f + 1) % 4]
                for kc in range(8):
                    ph.mm(h1[:, :], w1g[g][:, kc, fo:fo + 128], xt[:, kc, :], kc == 0, kc == 7, [w1g[g], xt], [h1])
                for kc in range(8):
                    ph.mm(h3[:, :], w3g[g][:, kc, fo:fo + 128], xt[:, kc, :], kc == 0, kc == 7, [w3g[g], xt], [h3])
                s = sg[f % 2]
                ph.act(lambda e_, h1=h1, s=s: e_.activation(out=s[:, :], in_=h1[:, :], func=AF.Silu), [h1], [s])
                ph.dve(lambda e_, h3=h3, s=s, f=f: e_.tensor_tensor(out=gT[:, f, :], in0=h3[:, :], in1=s[:, :], op=ALU.mult), [h3, s], [gdeps[f]])
            for sub in range(4):
                n = tt * 4 + sub
                for half in range(2):
                    o = op[half]
                    for f in range(NFF):
                        g = min(f // 4, 5); fi = f - FGROUPS[g][0]
                        ph.mm(o[:, :], gT[:, f, sub * 128:(sub + 1) * 128], w2g[g][:, fi, half * 512:(half + 1) * 512], f == 0, f == NFF - 1,
                              [gdeps[f], w2g[g]], [o])
                if e == NEXP - 1 and not moe:
                    epi.run(n, lambda lo, hi: op[lo // 512][:, :], [op[0], op[1]], idb, idf, trp, None, None)
                else:
                    a = acct[0]
                    ph.dma("sp", a[:, :], cx.acc[n * 128:(n + 1) * 128, :], [cx.acc_dep[n]], [a], a)
                    for half in range(2):
                        if moe:
                            ph.dve(lambda e_, a=a, half=half, n=n, e=e: e_.scalar_tensor_tensor(
                                out=a[:, half * 512:(half + 1) * 512], in0=op[half][:, :], scalar=comb[:, n, e:e + 1],
                                in1=a[:, half * 512:(half + 1) * 512], op0=ALU.mult, op1=ALU.add), [op[half], a, comb], [a])
                        else:
                            ph.dve(lambda e_, a=a, half=half: e_.tensor_tensor(out=a[:, half * 512:(half + 1) * 512], in0=op[half][:, :],
                                                                            in1=a[:, half * 512:(half + 1) * 512], op=ALU.add), [op[half], a], [a])
                    if e == NEXP - 1:
                        ph.dma("sp", cx.acc[n * 128:(n + 1) * 128, :], a[:, :], [a], [cx.acc_dep[n]], a)
                        epi.run(n, None, [], idb, idf, trp, None, None)
                    else:
                        ph.dma("sp", cx.acc[n * 128:(n + 1) * 128, :], a[:, :], [a], [cx.acc_dep[n]], a)
            k += 1
    ph.finish()


CST_ID, CST_ONES, CST_MTNEG, CST_M01S, CST_MT01, CST_SEL, CST_SC01, CST_SCNEG, NCST = 0, 128, 256, 384, 512, 640, 1152, 1664, 2176
PE_CW, PE_CB, PE_AG, PE_AB, PE_IB, PE_FB, NPE = 0, 124, 128, 132, 136, 137, 138


def phase_even_mixer(nc, cx, name, j):
    ph = Phase(nc, name)
    mk_deps(ph, cx)
    c, idb = load_consts(ph, cx)
    cd = [c]
    idf = Buf(c.t[:, 0:128], c.d)
    ones = c.t[:, CST_ONES:CST_ONES + 128]
    win = ph.sb("win", [128, 8, 3080], BF16)
    ph.dma("pool", win[:, :, :], cx.ab_w_in[j].rearrange("(c p) n -> p c n", p=128), [], [win], win)
    wout = ph.sb("wout", [128, 8, D], BF16)
    ph.dma("pool", wout[:, :, :], cx.ab_w_out[j].rearrange("(c p) n -> p c n", p=128), [], [wout], wout)
    pp = ph.sb("pp", [128, NPE])
    ph.dma("sp", pp[:, :], cx.pe[j], [], [pp], pp)
    nfb = ph.sb("nfb", [128, 1])
    ph.dve(lambda e: e.tensor_scalar(out=nfb[:, :], in0=pp[:, PE_FB:PE_FB + 1], scalar1=-1.0, scalar2=None, op0=ALU.mult), [pp], [nfb])
    hg = ph.sb("hg", [128, 512]); load_rows(ph, "sp", hg, cx.rows[cx.row_idx[("hg", j)]:cx.row_idx[("hg", j)] + 1, 0:512], 512)
    epi = Epi(ph, cx, cx.rows[cx.row_idx[("eln1g", j)]:cx.row_idx[("eln1g", j)] + 1, :], cx.rows[cx.row_idx[("eln1b", j)]:cx.row_idx[("eln1b", j)] + 1, :], False, nbuf=1)
    pj = [ph.ps("pj%d" % i, [128, 512]) for i in range(2)]
    pm = ph.ps("pm", [128, 512]); pm_st = pm; pm_ub = pm; pm_cols = pm; pm_sc = pm
    pn = ph.ps("pn", [128, 512]); pn_num = pn; pn_int = pn; pn_kv = pn
    pgi = ph.ps("pgi", [128, 512]); pgf = ph.ps("pgf", [128, 512])
    trp = ph.ps("trp", [128, 1024], BF16)
    xTt = [ph.sb("xTt%d" % i, [128, 8, 512], BF16) for i in range(1)]
    glu = ph.sb("glu", [128, 4, 542])
    ph.dve(lambda e: e.memset(glu[:, :, 0:30], 0.0), [], [glu])
    sig = ph.sb("sig", [128, 512])
    cy = ph.sb("cy", [128, 4, 512]); sq = ph.sb("sq", [128, 512])
    mean = ph.sb("mean", [128, 512]); var = ph.sb("var", [128, 512]); tmpa = ph.sb("tmpa", [128, 512])
    mixT = ph.sb("mixT", [128, 8, 512], BF16)
    qT = ph.sb("qT", [128, 4, 512], BF16); kT = ph.sb("kT", [128, 4, 512], BF16)
    ktok = ph.sb("ktok", [128, 4, 512], BF16)
    vext = ph.sb("vext", [128, 4, 4, 132], BF16)
    ph.dve(lambda e: e.memset(vext[:, :, :, 128:129], 1.0), [], [vext])
    osig = ph.sb("osig", [128, 4, 512])
    ig = ph.sb("ig", [128, 512]); sp_ = ph.sb("sp", [128, 512]); Bc = ph.sb("Bc", [128, 512]); lw = ph.sb("lw", [128, 512])
    Mx = ph.sb("Mx", [128, 512]); U = ph.sb("U", [128, 512]); rowsT = ph.sb("rowsT", [128, 512]); tmpr = ph.sb("tmpr", [128, 512])
    ph.dve(lambda e: e.memset(rowsT[:, :], 0.0), [], [rowsT])
    ph.dve(lambda e: e.memset(U[:, :], 0.0), [], [U])
    am = ph.sb("am", [128, 4]); nam = ph.sb("nam", [128, 4]); mp = ph.sb("mp", [128, 5]); sv = ph.sb("sv", [128, 4, 2]); t1 = ph.sb("t1", [128, 1]); t2 = ph.sb("t2", [128, 1])
    ph.dve(lambda e: e.memset(sv[:, :, :], 0.0), [], [sv])
    mcar = ph.sb("mcar", [128, 1])
    ph.dve(lambda e: e.memset(mcar[:, :], 0.0), [], [mcar])
    cols = ph.sb("cols", [128, 4, 128]); scol = ph.sb("scol", [128, 4, 128])
    vt = ph.sb("vt", [128, 512]); rows2 = ph.sb("rows2", [128, 512]); ax = ph.sb("ax", [128, 4]); nax = ph.sb("nax", [128, 4])
    vsc = ph.sb("vsc", [128, 132], BF16)
    ph.dve(lambda e: e.memset(rows2[:, :], 0.0), [], [rows2])
    wgi = ph.sb("wgi", [128, 8, 128], BF16); wgf = ph.sb("wgf", [128, 8, 128], BF16)
    ph.dve(lambda e: e.memset(wgi[:, :, :], 0.0), [], [wgi])
    ph.dve(lambda e: e.memset(wgf[:, :, :], 0.0), [], [wgf])
    for g in range(4):
        ph.dve(lambda e, g=g: e.tensor_copy(out=wgi[:, :, 32 * g:32 * g + 4], in_=win[:, :, 3072:3076]), [win], [wgi])
        ph.dve(lambda e, g=g: e.tensor_copy(out=wgf[:, :, 32 * g:32 * g + 4], in_=win[:, :, 3076:3080]), [win], [wgf])
    Cf = ph.sb("Cf", [128, 4, 132]); Cb = ph.sb("Cb", [128, 4, 132], BF16)
    ph.dve(lambda e: e.memset(Cf[:, :, :], 0.0), [], [Cf])
    ph.dve(lambda e: e.memset(Cb[:, :, :], 0.0), [], [Cb])
    PT = ph.sb("PT", [128, 128], BF16)
    ti = ph.sb("ti", [128, 129]); tot = ph.sb("tot", [128, 129]); dd = ph.sb("dd", [128, 1]); rec = ph.sb("rec", [128, 1])
    kw = ph.sb("kw", [128, 128], BF16)
    hraw = ph.sb("hraw", [128, 512]); hst = ph.sb("hst", [128, 4, 6]); hmv = ph.sb("hmv", [128, 4, 2]); hrs = ph.sb("hrs", [128, 4]); hb = ph.sb("hb", [128, 512], BF16)
    sc01 = c.t[:, CST_SC01:CST_SC01 + 512]; scneg = c.t[:, CST_SCNEG:CST_SCNEG + 512]
    k128 = 128 ** -0.5

    def proj_fm(col0, dst_fn, pjk):
        p = pj[pjk % 2]
        for kc in range(8):
            ph.mm(p[:, :], win[:, kc, col0:col0 + 128], xt[:, kc, :], kc == 0, kc == 7, [win, xt], [p])
        dst_fn(p)

    kpj = [0]
    import os
    emstop = int(os.environ.get("EMSTOP", "99"))
    for tt in range(cx.NTT if emstop > 0 else 0):
        xt = xTt[0]
        ph.dma("sp", xt[:, :, :], cx.xT.rearrange("c p s -> p c s")[:, :, tt * 512:(tt + 1) * 512], [cx.xT_dep[tt]], [xt], xt)
        for ch in range(4):
            p = pj[kpj[0] % 2]; kpj[0] += 1
            for kc in range(8):
                ph.mm(p[:, :], win[:, kc, 512 + ch * 128:512 + (ch + 1) * 128], xt[:, kc, :], kc == 0, kc == 7, [win, xt], [p])
            ph.act(lambda e, p=p: e.activation(out=sig[:, :], in_=p[:, :], func=AF.Sigmoid), [p], [sig])
            p2 = pj[kpj[0] % 2]; kpj[0] += 1
            for kc in range(8):
                ph.mm(p2[:, :], win[:, kc, ch * 128:(ch + 1) * 128], xt[:, kc, :], kc == 0, kc == 7, [win, xt], [p2])
            ph.dve(lambda e, p2=p2, ch=ch: e.tensor_tensor(out=glu[:, ch, 30:542], in0=p2[:, :], in1=sig[:, :], op=ALU.mult), [p2, sig], [glu])
            ph.dve(lambda e, ch=ch: e.tensor_scalar(out=cy[:, ch, :], in0=glu[:, ch, 0:512], scalar1=pp[:, PE_CW + ch * 31:PE_CW + ch * 31 + 1],
                                                    scalar2=pp[:, PE_CB + ch:PE_CB + ch + 1], op0=ALU.mult, op1=ALU.add), [glu, pp], [cy])
            for jj in range(1, 31):
                ph.dve(lambda e, ch=ch, jj=jj: e.scalar_tensor_tensor(out=cy[:, ch, :], in0=glu[:, ch, jj:jj + 512],
                                                                     scalar=pp[:, PE_CW + ch * 31 + jj:PE_CW + ch * 31 + jj + 1],
                                                                     in1=cy[:, ch, :], op0=ALU.mult, op1=ALU.add), [glu, pp, cy], [cy])
            ph.pool(lambda e, ch=ch: e.tensor_copy(out=glu[:, ch, 0:30], in_=glu[:, ch, 512:542]), [glu, cy], [glu])
        if emstop <= 1:
            continue
        for ch in range(4):
            ph.mm(pgi[:, :], ones, cy[:, ch, :], ch == 0, ch == 3, [cy] + cd, [pgi])
        for ch in range(4):
            ph.act(lambda e, ch=ch: e.activation(out=sq[:, :], in_=cy[:, ch, :], func=AF.Square), [cy], [sq])
            ph.mm(pgf[:, :], ones, sq[:, :], ch == 0, ch == 3, [sq] + cd, [pgf])
        ph.act(lambda e: e.activation(out=mean[:, :], in_=pgi[:, :], func=AF.Copy, scale=1.0 / 512), [pgi], [mean])
        ph.dve(lambda e: e.tensor_tensor(out=tmpa[:, :], in0=mean[:, :], in1=mean[:, :], op=ALU.mult), [mean], [tmpa])
        ph.dve(lambda e: e.scalar_tensor_tensor(out=var[:, :], in0=pgf[:, :], scalar=1.0 / 512, in1=tmpa[:, :], op0=ALU.mult, op1=ALU.subtract),
               [pgf, tmpa], [var])
        ph.dve(lambda e: e.tensor_scalar(out=var[:, :], in0=var[:, :], scalar1=EPS, scalar2=None, op0=ALU.add), [var], [var])
        ph.act(lambda e: e.activation(out=var[:, :], in_=var[:, :], func=AF.Sqrt), [var], [var])
        ph.dve(lambda e: e.reciprocal(out=var[:, :], in_=var[:, :]), [var], [var])
        for ch in range(4):
            ph.dve(lambda e, ch=ch: e.tensor_tensor(out=cy[:, ch, :], in0=cy[:, ch, :], in1=mean[:, :], op=ALU.subtract), [cy, mean], [cy])
            ph.dve(lambda e, ch=ch: e.tensor_tensor(out=cy[:, ch, :], in0=cy[:, ch, :], in1=var[:, :], op=ALU.mult), [cy, var], [cy])
            ph.act(lambda e, ch=ch: e.activation(out=mixT[:, ch, :], in_=cy[:, ch, :], func=AF.Silu, bias=pp[:, PE_AB + ch:PE_AB + ch + 1],
                                                 scale=pp[:, PE_AG + ch:PE_AG + ch + 1]), [cy, pp], [mixT])
        if emstop <= 2:
            continue
        for h in range(4):
            p = pj[kpj[0] % 2]; kpj[0] += 1
            for kc in range(8):
                ph.mm(p[:, :], win[:, kc, 1024 + h * 128:1024 + (h + 1) * 128], xt[:, kc, :], kc == 0, kc == 7, [win, xt], [p])
            ph.act(lambda e, p=p, h=h: e.activation(out=qT[:, h, :], in_=p[:, :], func=AF.Copy), [p], [qT])
            p = pj[kpj[0] % 2]; kpj[0] += 1
            for kc in range(8):
                ph.mm(p[:, :], win[:, kc, 1536 + h * 128:1536 + (h + 1) * 128], xt[:, kc, :], kc == 0, kc == 7, [win, xt], [p])
            ph.act(lambda e, p=p, h=h: e.activation(out=kT[:, h, :], in_=p[:, :], func=AF.Copy, scale=k128), [p], [kT])
        for sub in range(4):
            p = pj[kpj[0] % 2]; kpj[0] += 1
            for kc in range(8):
                ph.mm(p[:, :], xt[:, kc, sub * 128:(sub + 1) * 128], win[:, kc, 1536:2048], kc == 0, kc == 7, [win, xt], [p])
            ph.act(lambda e, p=p, sub=sub: e.activation(out=ktok[:, sub, :], in_=p[:, :], func=AF.Copy, scale=k128), [p], [ktok])
            p = pj[kpj[0] % 2]; kpj[0] += 1
            for kc in range(8):
                ph.mm(p[:, :], xt[:, kc, sub * 128:(sub + 1) * 128], win[:, kc, 2048:2560], kc == 0, kc == 7, [win, xt], [p])
            ph.dve(lambda e, p=p, sub=sub: e.tensor_copy(out=vext[:, sub, :, 0:128], in_=p[:, :].rearrange("p (h d) -> p h d", h=4)), [p], [vext])
            p = pj[kpj[0] % 2]; kpj[0] += 1
            for kc in range(8):
                ph.mm(p[:, :], xt[:, kc, sub * 128:(sub + 1) * 128], win[:, kc, 2560:3072], kc == 0, kc == 7, [win, xt], [p])
            ph.act(lambda e, p=p, sub=sub: e.activation(out=osig[:, sub, :], in_=p[:, :], func=AF.Sigmoid), [p], [osig])
        for kc in range(8):
            ph.mm(pgi[:, :], wgi[:, kc, :], xt[:, kc, :], kc == 0, kc == 7, [wgi, xt], [pgi])
        for kc in range(8):
            ph.mm(pgf[:, :], wgf[:, kc, :], xt[:, kc, :], kc == 0, kc == 7, [wgf, xt], [pgf])
        if emstop <= 3:
            continue
        G = [slice(32 * g, 32 * g + 4) for g in range(4)]
        ph.act(lambda e: e.activation(out=ig[:, :], in_=pgi[:, :], func=AF.Identity, bias=pp[:, PE_IB:PE_IB + 1]), [pgi, pp], [ig])
        ph.act(lambda e: e.activation(out=sp_[:, :], in_=pgf[:, :], func=AF.Exp, bias=nfb[:, :], scale=-1.0), [pgf, nfb], [sp_])
        ph.act(lambda e: e.activation(out=sp_[:, :], in_=sp_[:, :], func=AF.Ln, bias=1.0), [sp_], [sp_])
        ph.dve(lambda e: e.tensor_tensor_scan(out=Bc[:, :], data0=sc01, data1=sp_[:, :], initial=0.0, op0=ALU.mult, op1=ALU.add), [sp_] + cd, [Bc])
        ph.dve(lambda e: e.tensor_tensor(out=vt[:, :], in0=ig[:, :], in1=Bc[:, :], op=ALU.add), [ig, Bc], [vt])
        ph.dve(lambda e: e.tensor_tensor_scan(out=Mx[:, :], data0=scneg, data1=vt[:, :], initial=NEG, op0=ALU.add, op1=ALU.max), [vt] + cd, [Mx])
        ph.dve(lambda e: e.tensor_copy(out=ax[:, :], in_=Mx[:, :].rearrange("p (c t) -> p c t", c=4)[:, :, 127]), [Mx], [ax])
        ph.dve(lambda e: e.tensor_scalar(out=nax[:, :], in0=ax[:, :], scalar1=-1.0, scalar2=None, op0=ALU.mult), [ax], [nax])
        for cch in range(4):
            cs = slice(cch * 128, (cch + 1) * 128)
            ph.act(lambda e, cs=cs, cch=cch: e.activation(out=rowsT[G[0], cs], in_=vt[G[0], cs], func=AF.Exp, bias=nax[G[0], cch:cch + 1]), [vt, nax], [rowsT])
        for cch in range(4):
            cs = slice(cch * 128, (cch + 1) * 128)
            ph.dve(lambda e, cs=cs, cch=cch: e.tensor_scalar(out=lw[:, cs], in0=vt[:, cs], scalar1=Bc[:, cch * 128 + 127:cch * 128 + 128], scalar2=None,
                                                            op0=ALU.subtract), [vt, Bc], [lw])
        ph.dve(lambda e: e.tensor_reduce(out=am[:, :], in_=lw[:, :].rearrange("p (c t) -> p c t", c=4), axis=AX.X, op=ALU.max), [lw], [am])
        ph.dve(lambda e: e.tensor_scalar(out=nam[:, :], in0=am[:, :], scalar1=-1.0, scalar2=None, op0=ALU.mult), [am], [nam])
        for cch in range(4):
            cs = slice(cch * 128, (cch + 1) * 128)
            ph.act(lambda e, cs=cs, cch=cch: e.activation(out=rowsT[G[1], cs], in_=lw[G[1], cs], func=AF.Exp, bias=nam[G[1], cch:cch + 1]), [lw, nam], [rowsT])
        ph.dve(lambda e: e.tensor_copy(out=mp[:, 0:1], in_=mcar[:, :]), [mcar], [mp])
        for cch in range(4):
            be = Bc[:, cch * 128 + 127:cch * 128 + 128]
            ph.dve(lambda e, cch=cch, be=be: e.tensor_tensor(out=t1[:, :], in0=mp[:, cch:cch + 1], in1=be, op=ALU.subtract), [mp, Bc], [t1])
            ph.dve(lambda e, cch=cch: e.tensor_tensor(out=mp[:, cch + 1:cch + 2], in0=t1[:, :], in1=am[:, cch:cch + 1], op=ALU.max), [t1, am], [mp])
            ph.dve(lambda e, cch=cch: e.tensor_tensor(out=t1[:, :], in0=t1[:, :], in1=mp[:, cch + 1:cch + 2], op=ALU.subtract), [t1, mp], [t1])
            ph.dve(lambda e, cch=cch: e.tensor_tensor(out=t2[:, :], in0=am[:, cch:cch + 1], in1=mp[:, cch + 1:cch + 2], op=ALU.subtract), [am, mp], [t2])
            ph.act(lambda e, cch=cch: e.activation(out=sv[:, cch, 0:1], in_=t1[:, :], func=AF.Exp), [t1], [sv])
            ph.act(lambda e, cch=cch: e.activation(out=sv[:, cch, 1:2], in_=t2[:, :], func=AF.Exp), [t2], [sv])
        ph.dve(lambda e: e.tensor_copy(out=mcar[:, :], in_=mp[:, 4:5]), [mp], [mcar])
        for cch in range(4):
            cs = slice(cch * 128, (cch + 1) * 128)
            ph.dve(lambda e, cs=cs, cch=cch: e.tensor_scalar(out=U[:, cs], in0=Mx[:, cs], scalar1=mp[:, cch:cch + 1], scalar2=-1.0, op0=ALU.max, op1=ALU.mult),
                   [Mx, mp], [U])
            ph.act(lambda e, cs=cs, cch=cch: e.activation(out=rowsT[G[2], cs], in_=U[G[2], cs], func=AF.Exp, bias=mp[G[2], cch:cch + 1]), [U, mp], [rowsT])
            ph.act(lambda e, cs=cs, cch=cch: e.activation(out=rows2[G[2], cs], in_=U[G[2], cs], func=AF.Exp, bias=ax[G[2], cch:cch + 1]), [U, ax], [rows2])
            ph.dve(lambda e, cs=cs, cch=cch: e.tensor_scalar(out=rows2[G[0], cs], in0=c.t[G[0], CST_ONES:CST_ONES + 128], scalar1=sv[G[0], cch, 0:1], scalar2=None,
                                                            op0=ALU.mult), [sv] + cd, [rows2])
            ph.dve(lambda e, cs=cs, cch=cch: e.tensor_scalar(out=rows2[G[1], cs], in0=c.t[G[1], CST_ONES:CST_ONES + 128], scalar1=sv[G[1], cch, 1:2], scalar2=None,
                                                            op0=ALU.mult), [sv] + cd, [rows2])
        ph.dve(lambda e: e.tensor_tensor(out=tmpr[:, :], in0=Bc[:, :], in1=U[:, :], op=ALU.add), [Bc, U], [tmpr])
        ph.act(lambda e: e.activation(out=rowsT[G[3], :], in_=tmpr[G[3], :], func=AF.Exp), [tmpr], [rowsT])
        if emstop <= 4:
            continue
        for sub in range(4):
            ph.tr(pm[:, 256:384], rowsT[:, sub * 128:(sub + 1) * 128], idf[:, :], [rowsT, idf], [pm_cols])
            ph.act(lambda e, sub=sub: e.activation(out=cols[:, sub, :], in_=pm[:, 256:384], func=AF.Copy), [pm_cols], [cols])
            ph.tr(pm[:, 384:512], rows2[:, sub * 128:(sub + 1) * 128], idf[:, :], [rows2, idf], [pm_sc])
            ph.act(lambda e, sub=sub: e.activation(out=scol[:, sub, :], in_=pm[:, 384:512], func=AF.Copy), [pm_sc], [scol])
        if emstop <= 5:
            continue
        hstop = int(os.environ.get("HSTOP", "99"))
        for sub in range(4):
            cs = slice(sub * 128, (sub + 1) * 128)
            for h in range(4):
                ph.mm(pm[:, 0:128], kT[:, h, cs], qT[:, h, cs], True, True, [kT, qT], [pm_st])
                ph.dve(lambda e: e.tensor_tensor(out=PT[:, :], in0=pm[:, 0:128], in1=c.t[:, CST_MT01:CST_MT01 + 128], op=ALU.mult), [pm_st] + cd, [PT])
                ph.pool(lambda e, sub=sub, h=h: e.tensor_scalar(out=vsc[:, 0:130], in0=vext[:, sub, h, 0:130], scalar1=cols[:, sub, h:h + 1], scalar2=None, op0=ALU.mult),
                        [vext, cols], [vsc])
                if hstop <= 1:
                    continue
                ph.mm(pn[:, 0:129], PT[:, :], vsc[:, 0:129], True, True, [PT, vsc], [pn_num])
                ph.mm(pn[:, 129:258], qT[:, h, cs], Cb[:, h, 0:129], True, True, [qT, Cb], [pn_int])
                ph.act(lambda e, sub=sub, h=h: e.activation(out=ti[:, :], in_=pn[:, 129:258], func=AF.Copy, scale=cols[:, sub, 64 + h:64 + h + 1]), [pn_int, cols], [ti])
                ph.dve(lambda e, sub=sub, h=h: e.scalar_tensor_tensor(out=tot[:, :], in0=pn[:, 0:129], scalar=scol[:, sub, 64 + h:64 + h + 1], in1=ti[:, :],
                                                                      op0=ALU.mult, op1=ALU.add), [pn_num, ti, scol], [tot])
                if hstop <= 2:
                    continue
                ph.act(lambda e: e.activation(out=dd[:, :], in_=tot[:, 128:129], func=AF.Abs), [tot], [dd])
                ph.dve(lambda e, sub=sub, h=h: e.tensor_scalar(out=dd[:, :], in0=dd[:, :], scalar1=cols[:, sub, 96 + h:96 + h + 1], scalar2=None,
                                                               op0=ALU.max), [dd, cols], [dd])
                ph.dve(lambda e: e.reciprocal(out=rec[:, :], in_=dd[:, :]), [dd], [rec])
                ph.dve(lambda e, h=h: e.tensor_scalar(out=hraw[:, h * 128:(h + 1) * 128], in0=tot[:, 0:128], scalar1=rec[:, :], scalar2=None, op0=ALU.mult),
                       [tot, rec], [hraw])
                if hstop <= 3:
                    continue
                ph.dve(lambda e, sub=sub, h=h: e.tensor_scalar(out=kw[:, :], in0=ktok[:, sub, h * 128:(h + 1) * 128], scalar1=cols[:, sub, 32 + h:32 + h + 1],
                                                               scalar2=None, op0=ALU.mult), [ktok, cols], [kw])
                ph.mm(pn[:, 258:387], kw[:, :], vext[:, sub, h, 0:129], True, True, [kw, vext], [pn_kv])
                ph.dve(lambda e, sub=sub, h=h: e.tensor_scalar(out=Cf[:, h, 0:129], in0=Cf[:, h, 0:129], scalar1=scol[:, sub, h:h + 1], scalar2=None, op0=ALU.mult),
                       [Cf, scol], [Cf])
                ph.dve(lambda e, sub=sub, h=h: e.scalar_tensor_tensor(out=Cf[:, h, 0:129], in0=pn[:, 258:387], scalar=scol[:, sub, 32 + h:32 + h + 1], in1=Cf[:, h, 0:129],
                                                                      op0=ALU.mult, op1=ALU.add), [pn_kv, scol, Cf], [Cf])
                ph.act(lambda e, h=h: e.activation(out=Cb[:, h, 0:129], in_=Cf[:, h, 0:129], func=AF.Copy), [Cf], [Cb])
            if hstop <= 4:
                continue
            for h in range(4):
                ph.dve(lambda e, h=h: e.bn_stats(out=hst[:, h, :], in_=hraw[:, h * 128:(h + 1) * 128]), [hraw], [hst])
                ph.dve(lambda e, h=h: e.bn_aggr(out=hmv[:, h, :], in_=hst[:, h, :]), [hst], [hmv])
            ph.dve(lambda e: e.tensor_scalar(out=hrs[:, :], in0=hmv[:, :, 1], scalar1=EPS, scalar2=None, op0=ALU.add), [hmv], [hrs])
            ph.act(lambda e: e.activation(out=hrs[:, :], in_=hrs[:, :], func=AF.Sqrt), [hrs], [hrs])
            ph.dve(lambda e: e.reciprocal(out=hrs[:, :], in_=hrs[:, :]), [hrs], [hrs])
            for h in range(4):
                ph.dve(lambda e, h=h: e.tensor_scalar(out=hraw[:, h * 128:(h + 1) * 128], in0=hraw[:, h * 128:(h + 1) * 128], scalar1=hmv[:, h, 0:1],
                                                      scalar2=hrs[:, h:h + 1], op0=ALU.subtract, op1=ALU.mult), [hraw, hmv, hrs], [hraw])
            ph.pool(lambda e: e.tensor_tensor(out=hraw[:, :], in0=hraw[:, :], in1=hg[:, :], op=ALU.mult), [hraw, hg], [hraw])
            ph.dve(lambda e, sub=sub: e.tensor_tensor(out=hb[:, :], in0=hraw[:, :], in1=osig[:, sub, :], op=ALU.mult), [hraw, osig], [hb])
            for h in range(4):
                ph.tr(trp[:, h * 128:(h + 1) * 128], hb[:, h * 128:(h + 1) * 128], idb[:, :], [hb, idb], [trp])
            ph.act(lambda e, cs=cs: e.activation(out=mixT[:, 4:8, cs], in_=trp[:, 0:512].rearrange("p (c t) -> p c t", c=4), func=AF.Copy), [trp], [mixT])
        if emstop <= 6:
            continue
        for sub in range(4):
            n = tt * 4 + sub
            for half in range(2):
                for kc in range(8):
                    ph.mm(pj[half][:, :], mixT[:, kc, sub * 128:(sub + 1) * 128], wout[:, kc, half * 512:(half + 1) * 512], kc == 0, kc == 7, [mixT, wout], [pj[half]])
            epi.run(n, lambda lo, hi: pj[lo // 512][:, :], [pj[0], pj[1]], idb, idf, trp, None, None)
    ph.finish()


def phase_odd_mixer(nc, cx, name, j):
    ph = Phase(nc, name)
    mk_deps(ph, cx)
    c, idb = load_consts(ph, cx, 640)
    cd = [c]
    idf = Buf(c.t[:, 0:128], c.d)
    S = cx.S
    win = ph.sb("win", [128, 8, 2560], BF16)
    ph.dma("pool", win[:, :, :], cx.cd_w_in[j].rearrange("(c p) n -> p c n", p=128), [], [win], win)
    wout = ph.sb("wout", [128, 8, D], BF16)
    ph.dma("pool", wout[:, :, :], cx.cd_w_out[j].rearrange("(c p) n -> p c n", p=128), [], [wout], wout)
    ri = cx.row_idx
    cng = ph.sb("cng", [128, 512]); load_rows(ph, "sp", cng, cx.rows[ri[("cng", j)]:ri[("cng", j)] + 1, 0:512], 512)
    cnb = ph.sb("cnb", [128, 512]); load_rows(ph, "sp", cnb, cx.rows[ri[("cnb", j)]:ri[("cnb", j)] + 1, 0:512], 512)
    bsb = ph.sb("bsb", [128, 512]); load_rows(ph, "sp", bsb, cx.rows[ri[("bs", j)]:ri[("bs", j)] + 1, 0:512], 512)
    wsf = ph.sb("wsf", [128, 4, 128])
    ph.dma("sp", wsf[:, :, :], cx.wst[j].rearrange("g s t -> s g t"), [], [wsf], wsf)
    WcT = ph.sb("WcT", [128, 4, 128], BF16)
    for g in range(4):
        ph.dve(lambda e, g=g: e.tensor_tensor(out=WcT[:, g, :], in0=wsf[:, g, :], in1=c.t[:, CST_MT01:CST_MT01 + 128], op=ALU.mult), [wsf] + cd, [WcT])
    epi = Epi(ph, cx, cx.rows[ri[("oln1g", j)]:ri[("oln1g", j)] + 1, :], cx.rows[ri[("oln1b", j)]:ri[("oln1b", j)] + 1, :], False,
              router={"w": cx.router_w[j], "b": cx.rows[ri[("rb", j)]:ri[("rb", j)] + 1, 0:NE]}, nbuf=1)
    kTres = ph.sb("kTres", [128, 4, S], BF16)
    Vres = ph.sb("Vres", [128, cx.NT, 512], BF16)
    pj = [ph.ps("pj%d" % i, [128, 512]) for i in range(2)]
    pz = [ph.ps("pz%d" % i, [128, 512]) for i in range(2)]
    pg = ph.ps("pg", [128, 512]); po = ph.ps("po", [128, 512])
    trp = ph.ps("trp", [128, 1024], BF16)
    xt = ph.sb("xTt", [128, 8, 512], BF16)
    uT = ph.sb("uT", [128, 4, 512], BF16)
    zf = ph.sb("zf", [128, 512]); zb = ph.sb("zb", [128, 512], BF16)
    zst = ph.sb("zst", [128, 1, 6]); zmv = ph.sb("zmv", [128, 2]); zrs = ph.sb("zrs", [128, 1]); znb = ph.sb("znb", [128, 1])
    gtmp = ph.sb("gtmp", [128, 512])
    mixT = ph.sb("mixT", [128, 8, 512], BF16)
    qT = ph.sb("qT", [128, 4, 512], BF16)
    NB = 1
    ebuf = [ph.sb("ebuf%d" % i, [128, 512]) for i in range(NB)]
    spb = [ph.sb("spb%d" % i, [128, 513]) for i in range(NB)]
    csb = [ph.sb("csb%d" % i, [128, 513]) for i in range(NB)]
    xb = [ph.sb("xb%d" % i, [128, 512]) for i in range(NB)]
    attb = [ph.sb("attb%d" % i, [128, 512], BF16) for i in range(NB)]
    attT = [ph.sb("attT%d" % i, [128, 4, 128], BF16) for i in range(NB)]
    for i in range(NB):
        ph.dve(lambda e, i=i: e.memset(spb[i][:, 0:1], 0.0), [], [spb[i]])
    car = ph.sb("car", [128, 1]); nbb = [ph.sb("nbb%d" % i, [128, 1]) for i in range(NB)]
    dout = ph.sb("dout", [128, 512], BF16)
    onesr = c.t[:, CST_ONES:CST_ONES + 128]
    ones513 = ph.sb("ones513", [128, 513])
    ph.dve(lambda e: e.memset(ones513[:, :], 1.0), [], [ones513])
    m01s = c.t[:, CST_M01S:CST_M01S + 128]
    kpj = [0]
    kz = [0]
    import os
    for tt in range(int(os.environ.get("OMT", cx.NTT))):
        t0 = tt * 512
        ph.dma("sp", xt[:, :, :], cx.xT.rearrange("c p s -> p c s")[:, :, t0:t0 + 512], [cx.xT_dep[tt]], [xt], xt)
        for ch in range(4):
            p = pj[kpj[0] % 2]; kpj[0] += 1
            for kc in range(8):
                ph.mm(p[:, :], win[:, kc, ch * 128:(ch + 1) * 128], xt[:, kc, :], kc == 0, kc == 7, [win, xt], [p])
            ph.act(lambda e, p=p, ch=ch: e.activation(out=uT[:, ch, :], in_=p[:, :], func=AF.Gelu), [p], [uT])
        for pr in range(4):
            p = pj[kpj[0] % 2]; kpj[0] += 1
            for kc in range(8):
                ph.mm(p[:, :], win[:, kc, 1024 + pr * 128:1024 + (pr + 1) * 128], xt[:, kc, :], kc == 0, kc == 7, [win, xt], [p])
            ph.act(lambda e, p=p, pr=pr: e.activation(out=qT[:, pr, :], in_=p[:, :], func=AF.Copy, scale=0.125), [p], [qT])
            p = pj[kpj[0] % 2]; kpj[0] += 1
            for kc in range(8):
                ph.mm(p[:, :], win[:, kc, 1536 + pr * 128:1536 + (pr + 1) * 128], xt[:, kc, :], kc == 0, kc == 7, [win, xt], [p])
            ph.dve(lambda e, p=p, pr=pr, t0=t0: e.tensor_copy(out=kTres[:, pr, t0:t0 + 512], in_=p[:, :]), [p], [kTres])
        for sub in range(4):
            n = tt * 4 + sub
            cs = slice(sub * 128, (sub + 1) * 128)
            p = pj[kpj[0] % 2]; kpj[0] += 1
            for kc in range(8):
                ph.mm(p[:, :], xt[:, kc, cs], win[:, kc, 2048:2560], kc == 0, kc == 7, [win, xt], [p])
            ph.act(lambda e, p=p, n=n: e.activation(out=Vres[:, n, :], in_=p[:, :], func=AF.Copy), [p], [Vres])
            p = pj[kpj[0] % 2]; kpj[0] += 1
            for kc in range(8):
                ph.mm(p[:, :], xt[:, kc, cs], win[:, kc, 512:1024], kc == 0, kc == 7, [win, xt], [p])
            ph.act(lambda e, p=p: e.activation(out=zf[:, :], in_=p[:, :], func=AF.Gelu), [p], [zf])
            ln_rows(ph, zf, zmv, zst, zrs, znb, 512)
            ph.act(lambda e: e.activation(out=zf[:, :], in_=zf[:, :], func=AF.Identity, bias=znb[:, :], scale=zrs[:, :]), [zf, znb, zrs], [zf])
            ph.pool(lambda e: e.tensor_tensor(out=zf[:, :], in0=zf[:, :], in1=cng[:, :], op=ALU.mult), [zf, cng], [zf])
            ph.pool(lambda e: e.tensor_tensor(out=zb[:, :], in0=zf[:, :], in1=cnb[:, :], op=ALU.add), [zf, cnb], [zb])
            for g in range(4):
                ph.mm(pg[:, g * 128:(g + 1) * 128], zb[:, g * 128:(g + 1) * 128], WcT[:, g, :], True, True, [zb, WcT], [pg])
            ph.dve(lambda e: e.tensor_tensor(out=gtmp[:, :], in0=pg[:, :], in1=bsb[:, :], op=ALU.add), [pg, bsb], [gtmp])
            ph.dve(lambda e, cs=cs: e.tensor_tensor(out=mixT[:, 0:4, cs], in0=gtmp[:, :].rearrange("p (g t) -> p g t", g=4), in1=uT[:, :, cs], op=ALU.mult),
                   [gtmp, uT], [mixT])
            kend = (n + 1) * 128
            KT = (kend + 511) // 512
            for h in range(8):
                pr = h // 2; b0 = (h % 2) * 64
                ph.dve(lambda e: e.memset(car[:, :], 0.0), [], [car])
                first = True
                for kt in reversed(range(KT)):
                    k0 = kt * 512
                    w = min(kend, k0 + 512) - k0
                    nblk = w // 128
                    i = kz[0] % NB; kz[0] += 1
                    z = pz[i]; eb = ebuf[i]; sp_ = spb[i]; cb = csb[i]; x_ = xb[i]; ab = attb[i]; aT = attT[i]; nb_ = nbb[i]
                    ph.mm(z[:, 0:w], qT[b0:b0 + 64, pr, cs], kTres[b0:b0 + 64, pr, k0:k0 + w], True, True, [qT, kTres], [z])
                    ph.act(lambda e, z=z, eb=eb, w=w: e.activation(out=eb[:, 0:w], in_=z[:, 0:w], func=AF.Exp), [z], [eb])
                    ph.act(lambda e, sp_=sp_, eb=eb, w=w: e.activation(out=sp_[:, 1:w + 1], in_=eb[:, 0:w], func=AF.Ln, bias=1.0), [eb], [sp_])
                    if kt == KT - 1:
                        ph.pool(lambda e, sp_=sp_, w=w: e.tensor_tensor(out=sp_[:, 1 + w - 128:1 + w], in0=sp_[:, 1 + w - 128:1 + w], in1=m01s, op=ALU.mult), [sp_] + cd, [sp_])
                        ph.pool(lambda e, eb=eb, w=w: e.tensor_tensor(out=eb[:, w - 128:w], in0=eb[:, w - 128:w], in1=m01s, op=ALU.mult), [eb] + cd, [eb])
                    ph.dve(lambda e, cb=cb, sp_=sp_, w=w: e.tensor_tensor_scan(out=cb[:, 0:w + 1], data0=ones513[:, 0:w + 1], data1=sp_[:, 0:w + 1], initial=0.0,
                                                                              op0=ALU.mult, op1=ALU.add), [sp_, ones513], [cb])
                    ph.dve(lambda e, cb=cb, w=w: e.tensor_tensor(out=car[:, :], in0=cb[:, w:w + 1], in1=car[:, :], op=ALU.add), [cb, car], [car])
                    ph.dve(lambda e, nb_=nb_: e.tensor_scalar(out=nb_[:, :], in0=car[:, :], scalar1=-1.0, scalar2=None, op0=ALU.mult), [car], [nb_])
                    ph.act(lambda e, x_=x_, cb=cb, nb_=nb_, w=w: e.activation(out=x_[:, 0:w], in_=cb[:, 0:w], func=AF.Exp, bias=nb_[:, :]), [cb, nb_], [x_])
                    ph.pool(lambda e, ab=ab, eb=eb, x_=x_, w=w: e.tensor_tensor(out=ab[:, 0:w], in0=eb[:, 0:w], in1=x_[:, 0:w], op=ALU.mult), [eb, x_], [ab])
                    for jb in range(nblk):
                        ph.tr(trp[:, jb * 128:(jb + 1) * 128], ab[:, jb * 128:(jb + 1) * 128], idb[:, :], [ab, idb], [trp])
                    ph.act(lambda e, aT=aT, nblk=nblk: e.activation(out=aT[:, 0:nblk, :], in_=trp[:, 0:nblk * 128].rearrange("p (b t) -> p b t", b=nblk), func=AF.Copy),
                           [trp], [aT])
                    for jb in range(nblk):
                        kb = kt * 4 + jb
                        last = (kt == 0 and jb == nblk - 1)
                        ph.mm(po[:, h * 64:(h + 1) * 64], aT[:, jb, :], Vres[:, kb, h * 64:(h + 1) * 64], first, last, [aT, Vres], [po])
                        first = False
            ph.act(lambda e: e.activation(out=dout[:, :], in_=po[:, :], func=AF.Copy), [po], [dout])
            for cc in range(4):
                ph.tr(trp[:, cc * 128:(cc + 1) * 128], dout[:, cc * 128:(cc + 1) * 128], idb[:, :], [dout, idb], [trp])
            ph.dve(lambda e, cs=cs: e.tensor_copy(out=mixT[:, 4:8, cs], in_=trp[:, 0:512].rearrange("p (c t) -> p c t", c=4)), [trp], [mixT])
        for sub in range(4):
            n = tt * 4 + sub
            for half in range(2):
                for kc in range(8):
                    ph.mm(pj[half][:, :], mixT[:, kc, sub * 128:(sub + 1) * 128], wout[:, kc, half * 512:(half + 1) * 512], kc == 0, kc == 7, [mixT, wout], [pj[half]])
            epi.run(n, lambda lo, hi: pj[lo // 512][:, :], [pj[0], pj[1]], idb, idf, trp, pz, pg)
    ph.finish()


def mk_deps(ph, cx):
    cx.acc_dep = [ph.region("acc%d" % n) for n in range(cx.NT)]
    cx.out_dep = [ph.region("out%d" % n) for n in range(cx.NT)]
    cx.xT_dep = [ph.region("xT%d" % t) for t in range(cx.NTT)]
    cx.comb_dep = ph.region("comb")


ROW_KEYS_EVEN = ["eln1g", "eln1b", "eln2g", "eln2b", "hg"]
ROW_KEYS_ODD = ["oln1g", "oln1b", "oln2g", "oln2b", "cng", "cnb", "bs", "rb"]


def row_index():
    idx = {}
    k = 0
    for j in range(2):
        for key in ROW_KEYS_EVEN + ROW_KEYS_ODD:
            idx[(key, j)] = k
            k += 1
    return idx, k


def build(S, layers):
    nc = bass.Bass("TRN2", target_bir_lowering=False)
    gst = ExitStack()
    make_sempool(nc, gst)
    nc._gst = gst
    cx = Ctx()
    cx.S = S; cx.NT = S // 128; cx.NTT = S // 512; cx.NCST = NCST
    cx.row_idx, nrows = row_index()

    def inp(name, shape):
        return nc.dram_tensor(name, list(shape), F32, kind="ExternalInput").ap()

    cx.x = inp("x", [S, D])
    cx.out = nc.dram_tensor("out", [S, D], F32, kind="ExternalOutput").ap()
    cx.cst = inp("cst", [128, NCST])
    cx.rows = inp("rows", [nrows, D])
    cx.pe = [inp("pe0", [128, NPE]), inp("pe1", [128, NPE])]
    cx.wst = inp("wst", [2, 4, 128, 128])
    cx.wkeys = []
    if any(l % 2 == 0 for l in layers):
        cx.ab_w_in = inp("ab_w_in", [2, D, 3080]); cx.ab_w_out = inp("ab_w_out", [2, D, D])
        cx.ffn_w1 = inp("ffn_w1", [2, D, DFF]); cx.ffn_w3 = inp("ffn_w3", [2, D, DFF]); cx.ffn_w2 = inp("ffn_w2", [2, DFF, D])
        cx.wkeys += ["ab_w_in", "ab_w_out", "ffn_w1", "ffn_w3", "ffn_w2"]
    if any(l % 2 == 1 for l in layers):
        cx.cd_w_in = inp("cd_w_in", [2, D, 2560]); cx.cd_w_out = inp("cd_w_out", [2, D, D])
        cx.router_w = inp("router_w", [2, D, NE])
        cx.moe_w1 = inp("moe_w1", [2, NE, D, DFF]); cx.moe_w3 = inp("moe_w3", [2, NE, D, DFF]); cx.moe_w2 = inp("moe_w2", [2, NE, DFF, D])
        cx.wkeys += ["cd_w_in", "cd_w_out", "router_w", "moe_w1", "moe_w3", "moe_w2"]
    nc._wkeys = cx.wkeys
    cx.acc = nc.dram_tensor("acc", [S, D], F32).ap()
    cx.xT = nc.dram_tensor("xT", [8, 128, S], BF16).ap()
    cx.comb = nc.dram_tensor("comb", [S, NE], F32).ap()
    ri = cx.row_idx
    import os
    kstop = int(os.environ.get("KSTOP", "99"))
    phase_prologue(nc, cx)
    if kstop <= 0:
        return nc
    for li, l in enumerate(layers):
        j = l // 2
        last = (li == len(layers) - 1)
        if l % 2 == 0:
            phase_even_mixer(nc, cx, "em%d" % l, j)
            if kstop <= 1:
                return nc
            phase_ffn(nc, cx, "ef%d" % l, [cx.ffn_w1[j]], [cx.ffn_w3[j]], [cx.ffn_w2[j]],
                      cx.rows[ri[("eln2g", j)]:ri[("eln2g", j)] + 1, :], cx.rows[ri[("eln2b", j)]:ri[("eln2b", j)] + 1, :], last, False)
        else:
            phase_odd_mixer(nc, cx, "om%d" % l, j)
            if kstop <= 1:
                phase_dump(nc, cx)
                return nc
            phase_ffn(nc, cx, "of%d" % l, [cx.moe_w1[j][e] for e in range(NE)], [cx.moe_w3[j][e] for e in range(NE)],
                      [cx.moe_w2[j][e] for e in range(NE)],
                      cx.rows[ri[("oln2g", j)]:ri[("oln2g", j)] + 1, :], cx.rows[ri[("oln2b", j)]:ri[("oln2b", j)] + 1, :], last, True)
    return nc


def host_consts():
    c = np.zeros((128, NCST), np.float32)
    i = np.arange(128)
    c[:, CST_ID:CST_ID + 128] = np.eye(128, dtype=np.float32)
    c[:, CST_ONES:CST_ONES + 128] = 1.0
    c[:, CST_MTNEG:CST_MTNEG + 128] = np.where(i[:, None] <= i[None, :], 0.0, NEG)
    c[:, CST_M01S:CST_M01S + 128] = (i[None, :] < i[:, None]).astype(np.float32)
    c[:, CST_MT01:CST_MT01 + 128] = (i[:, None] <= i[None, :]).astype(np.float32)
    for h in range(4):
        c[h, CST_SEL + h * 128:CST_SEL + (h + 1) * 128] = 1.0
    t = np.arange(512)
    c[:, CST_SC01:CST_SC01 + 512] = (t % 128 != 0).astype(np.float32)[None, :]
    c[:, CST_SCNEG:CST_SCNEG + 512] = np.where(t % 128 == 0, NEG, 0.0)[None, :]
    return c


def host_layout(inp):
    f = lambda a: np.asarray(a, dtype=np.float32)
    idx, nrows = row_index()
    rows = np.zeros((nrows, D), np.float32)

    def put(key, j, v):
        v = f(v).reshape(-1)
        rows[idx[(key, j)], :v.size] = v

    pes = []
    for j in range(2):
        put("eln1g", j, inp["ab_ln1_g"][j]); put("eln1b", j, inp["ab_ln1_b"][j])
        put("eln2g", j, inp["ab_ln2_g"][j]); put("eln2b", j, inp["ab_ln2_b"][j])
        put("hg", j, inp["b_norm_g"][j])
        put("oln1g", j, inp["cd_ln1_g"][j]); put("oln1b", j, inp["cd_ln1_b"][j])
        put("oln2g", j, inp["cd_ln2_g"][j]); put("oln2b", j, inp["cd_ln2_b"][j])
        put("cng", j, inp["c_norm_g"][j]); put("cnb", j, inp["c_norm_b"][j])
        put("bs", j, inp["c_b_s"][j]); put("rb", j, inp["router_b"][j])
        pe = np.zeros((128, NPE), np.float32)
        cw = f(inp["a_conv_w"][j])
        pe[:, PE_CW:PE_CW + 124] = cw.reshape(31, 4, 128).transpose(2, 1, 0).reshape(128, 124)
        pe[:, PE_CB:PE_CB + 4] = f(inp["a_conv_b"][j]).reshape(4, 128).T
        pe[:, PE_AG:PE_AG + 4] = f(inp["a_norm_g"][j]).reshape(4, 128).T
        pe[:, PE_AB:PE_AB + 4] = f(inp["a_norm_b"][j]).reshape(4, 128).T
        gb = f(inp["ab_gate_bias"][j])
        for g in range(4):
            pe[32 * g:32 * g + 4, PE_IB] = gb[0:4]
            pe[32 * g:32 * g + 4, PE_FB] = gb[4:8]
        pes.append(pe)
    wst = np.ascontiguousarray(f(inp["c_w_s"]).transpose(0, 1, 3, 2))
    return rows, pes, wst


WKEYS = ["ab_w_in", "ab_w_out", "ffn_w1", "ffn_w3", "ffn_w2", "cd_w_in", "cd_w_out", "router_w", "moe_w1", "moe_w3", "moe_w2"]
_CACHE = {}


def run(inputs, S, layers, ncores):
    key = (S, tuple(layers))
    if key not in _CACHE:
        _CACHE[key] = build(S, layers)
    nc = _CACHE[key]
    rows, pes, wst = host_layout(inputs)
    cst = host_consts()
    shared = {"cst": cst, "rows": rows, "pe0": pes[0], "pe1": pes[1], "wst": wst}
    for k in nc._wkeys:
        shared[k] = np.ascontiguousarray(np.asarray(inputs[k], dtype=np.float32))
    x = np.asarray(inputs["x"], dtype=np.float32)
    in_maps = []
    for c in range(ncores):
        m = dict(shared)
        m["x"] = np.ascontiguousarray(x[c, :S, :])
        in_maps.append(m)
    res = run_bass_kernel_spmd(nc, in_maps, core_ids=list(range(ncores)))
    return np.stack([res.results[c]["out"] for c in range(ncores)], axis=0)


def kernel(**inputs):
    return run(inputs, 4096, [0, 1, 2, 3], 8)


def phase_dump(nc, cx):
    ph = Phase(nc, "dump")
    mk_deps(ph, cx)
    t = ph.sb("t", [128, D])
    for n in range(cx.NT):
        ph.dma("sp", t[:, :], cx.acc[n * 128:(n + 1) * 128, :], [], [t], t)
        ph.dma("sp", cx.out[n * 128:(n + 1) * 128, :], t[:, :], [t], [cx.out_dep[n]], t)
    ph.finish()
```

```python
import math
from contextlib import ExitStack
import numpy as np
import concourse.bass as bass
import concourse.mybir as mybir
from concourse.bass_utils import run_bass_kernel_spmd

F32 = mybir.dt.float32
BF16 = mybir.dt.bfloat16
AF = mybir.ActivationFunctionType
ALU = mybir.AluOpType
AX = mybir.AxisListType

D = 1024
DFF = 2816
NFF = 22
NE = 8
DEPTH = 4
ALPHA = (2 * DEPTH) ** 0.25
EPS = 1e-5
NEG = -1.0e30
FGROUPS = [(0, 4), (4, 4), (8, 4), (12, 4), (16, 4), (20, 2)]

ENGS = ["pe", "act", "dve", "pool", "sp"]


class Dep:
    __slots__ = ("name", "lw", "rd", "sem", "dcount", "lastdma", "psum")

    def __init__(self, name):
        self.name = name
        self.psum = False
        self.lw = None
        self.rd = []
        self.sem = None
        self.dcount = 0
        self.lastdma = None


class Op:
    __slots__ = ("id", "eng", "fn", "deps", "dma", "signal", "sigval", "sem", "inc")

    def __init__(self, id, eng, fn, dma, inc):
        self.id = id
        self.eng = eng
        self.fn = fn
        self.deps = set()
        self.dma = dma
        self.signal = False
        self.sigval = 0
        self.sem = None
        self.inc = inc


class Buf:
    __slots__ = ("t", "d")

    def __init__(self, t, d):
        self.t = t
        self.d = d

    def __getitem__(self, k):
        return self.t[k]


SEMPOOL = {}
NDMASEM = 56


def make_sempool(nc, stack):
    pool = {"eng": {}, "dma": []}
    for e in ["pe", "act", "dve", "pool"]:
        pool["eng"][e] = [stack.enter_context(nc.semaphore("s_" + e)), 0]
    for i in range(NDMASEM):
        pool["dma"].append([stack.enter_context(nc.semaphore("d%d" % i)), 0])
    SEMPOOL[id(nc)] = pool


class Phase:
    def __init__(self, nc, name):
        self.nc = nc
        self.name = name
        self.ops = []
        self.by_eng = {e: [] for e in ENGS}
        self.dma_deps = []
        self.all_deps = []
        self.st = ExitStack()
        self.nsb = 0

    def dep(self, name):
        d = Dep(name)
        self.all_deps.append(d)
        return d

    def sb(self, name, shape, dt=F32):
        t = self.st.enter_context(self.nc.sbuf_tensor(self.name + "_" + name, list(shape), dt))
        return Buf(t, self.dep(name))

    def ps(self, name, shape, dt=F32):
        t = self.st.enter_context(self.nc.psum_tensor(self.name + "_" + name, list(shape), dt))
        b = Buf(t, self.dep(name))
        b.d.psum = True
        return b

    def region(self, name):
        return Buf(None, self.dep(name))

    def op(self, eng, fn, R=(), W=(), dma=None, inc=16):
        o = Op(len(self.ops), eng, fn, dma, inc)
        self.ops.append(o)
        self.by_eng[eng].append(o)
        deps = o.deps
        if any(b.d.psum for b in R):
            W = list(W) + [b for b in R if b.d.psum]
            R = [b for b in R if not b.d.psum]
        for b in R:
            t = b.d
            if t.lw is not None:
                deps.add(t.lw)
        for b in W:
            t = b.d
            if t.lw is not None:
                deps.add(t.lw)
            deps.update(t.rd)
        if dma is not None:
            dd = dma.d
            if dd.lastdma is not None:
                deps.add(dd.lastdma)
            if fn is not None:
                dd.lastdma = o.id
            if dd.sem is None:
                dd.sem = True
                self.dma_deps.append(dd)
        deps.discard(o.id)
        best = {}
        for d in deps:
            od = self.ops[d]
            if od.dma is None and od.fn is not None:
                if d > best.get(od.eng, -1):
                    best[od.eng] = d
        for d in list(deps):
            od = self.ops[d]
            if od.dma is None and od.fn is not None and best[od.eng] != d:
                deps.discard(d)
        if eng == "pe":
            for d in list(deps):
                od = self.ops[d]
                if od.eng == "pe" and od.dma is None:
                    deps.discard(d)
        for d in deps:
            self.ops[d].signal = True
        if fn is not None:
            for b in R:
                b.d.rd.append(o.id)
            for b in W:
                b.d.lw = o.id
                b.d.rd = []
        return o

    def pe(self, fn, R=(), W=()):
        return self.op("pe", fn, R, W)

    def act(self, fn, R=(), W=()):
        return self.op("act", fn, R, W)

    def dve(self, fn, R=(), W=()):
        return self.op("dve", fn, R, W)

    def pool(self, fn, R=(), W=()):
        return self.op("pool", fn, R, W)

    def dma(self, q, out_ap, in_ap, R, W, semb):
        return self.op(q, lambda e: e.dma_start(out=out_ap, in_=in_ap), R, W, dma=semb)

    def mm(self, out_ap, lhsT, rhs, start, stop, R, W):
        return self.op("pe", lambda e: e.matmul(out_ap, lhsT=lhsT, rhs=rhs, start=start, stop=stop), R, W)

    def tr(self, out_ap, in_ap, ident_ap, R, W):
        return self.op("pe", lambda e: e.transpose(out=out_ap, in_=in_ap, identity=ident_ap), R, W)

    def barrier(self):
        allb = [Buf(None, d) for d in self.all_deps]
        for e in ENGS:
            self.op(e, None, R=(), W=allb)

    def finish(self):
        nc = self.nc
        st = self.st
        allb = [Buf(None, d) for d in self.all_deps]
        for e in ENGS:
            self.op(e, None, R=(), W=allb)
        pool = SEMPOOL[id(nc)]
        engsem = {}
        cnt = {e: 0 for e in ENGS}
        for e in ["pe", "act", "dve", "pool"]:
            engsem[e] = pool["eng"][e][0]
            cnt[e] = pool["eng"][e][1]
        assert len(self.dma_deps) <= len(pool["dma"]), len(self.dma_deps)
        for i, d in enumerate(self.dma_deps):
            d.sem = pool["dma"][i][0]
            d.dcount = pool["dma"][i][1]
        for o in self.ops:
            if o.fn is None:
                continue
            if o.dma is not None:
                o.dma.d.dcount += o.inc
                o.sigval = o.dma.d.dcount
                o.sem = o.dma.d.sem
            elif o.signal:
                cnt[o.eng] += 1
                o.sigval = cnt[o.eng]
                o.sem = engsem[o.eng]
        ops = self.ops

        def run(eng_name):
            def body(e):
                seen = {}
                for o in self.by_eng[eng_name]:
                    need = {}
                    for d in o.deps:
                        od = ops[d]
                        if od.fn is None:
                            continue
                        key = id(od.sem)
                        if od.sigval > need.get(key, (0, None))[0]:
                            need[key] = (od.sigval, od.sem)
                    for key, (val, sem) in need.items():
                        if seen.get(key, 0) >= val:
                            continue
                        e.wait_ge(sem, val)
                        seen[key] = val
                    if o.fn is None:
                        continue
                    ins = o.fn(e)
                    if o.dma is not None:
                        ins.then_inc(o.sem, o.inc)
                    elif o.signal:
                        ins.then_inc(o.sem, 1)
            return body

        for e in ["pe", "act", "dve", "pool"]:
            pool["eng"][e][1] = cnt[e]
        for i, d in enumerate(self.dma_deps):
            pool["dma"][i][1] = d.dcount
        block = st.enter_context(nc.Block())
        block.tensor(run("pe"))
        block.scalar(run("act"))
        block.vector(run("dve"))
        block.gpsimd(run("pool"))
        block.sync(run("sp"))
        st.close()


class Ctx:
    pass


def load_rows(ph, q, buf, dram_row_ap, n):
    ph.dma(q, buf[:, 0:n], dram_row_ap.partition_broadcast(128), [], [buf], buf)


def ln_rows(ph, s, mv, stats, rstd, nb, width, eps=EPS):
    nchunk = (width + 511) // 512
    for h in range(nchunk):
        lo, hi = h * 512, min(width, (h + 1) * 512)
        ph.dve(lambda e, h=h, lo=lo, hi=hi: e.bn_stats(out=stats[:, h, :], in_=s[:, lo:hi]), [s], [stats])
    ph.dve(lambda e: e.bn_aggr(out=mv[:, :], in_=stats[:, 0:nchunk, :].rearrange("p a b -> p (a b)")), [stats], [mv])
    ph.dve(lambda e: e.tensor_scalar(out=rstd[:, :], in0=mv[:, 1:2], scalar1=eps, scalar2=None, op0=ALU.add), [mv], [rstd])
    ph.act(lambda e: e.activation(out=rstd[:, :], in_=rstd[:, :], func=AF.Sqrt), [rstd], [rstd])
    ph.dve(lambda e: e.reciprocal(out=rstd[:, :], in_=rstd[:, :]), [rstd], [rstd])
    ph.dve(lambda e: e.scalar_tensor_tensor(out=nb[:, :], in0=mv[:, 0:1], scalar=-1.0, in1=rstd[:, :], op0=ALU.mult, op1=ALU.mult),
           [mv, rstd], [nb])


class Epi:
    def __init__(self, ph, cx, g_row, b_row, last, router=None, nbuf=2):
        self.ph = ph
        self.cx = cx
        self.last = last
        self.router = router
        self.grow = ph.sb("e_g", [128, D]); load_rows(ph, "sp", self.grow, g_row, D)
        self.brow = ph.sb("e_b", [128, D]); load_rows(ph, "sp", self.brow, b_row, D)
        self.nbuf = nbuf
        self.s = [ph.sb("e_s%d" % i, [128, D]) for i in range(nbuf)]
        self.y = [ph.sb("e_y%d" % i, [128, D]) for i in range(nbuf)]
        self.yb = [ph.sb("e_yb%d" % i, [128, D], BF16) for i in range(nbuf)]
        self.ya = self.s
        self.stats = ph.sb("e_st", [128, 2, 6]); self.mv = ph.sb("e_mv", [128, 2])
        self.rstd = ph.sb("e_rstd", [128, 1]); self.nb = ph.sb("e_nb", [128, 1])
        self.xTs = [ph.sb("e_xT%d" % i, [128, 8, 512], BF16) for i in range(nbuf)]
        self.k = 0
        if router is not None:
            self.rw = ph.sb("r_w", [128, 8, NE]); ph.dma("sp", self.rw[:, :, :], router["w"].rearrange("(c p) e -> p c e", p=128), [], [self.rw], self.rw)
            self.rb = ph.sb("r_b", [128, NE]); load_rows(ph, "sp", self.rb, router["b"], NE)
            self.yT = ph.sb("r_yT", [128, 8, 128])
            self.lg = ph.sb("r_lg", [128, NE]); self.top = ph.sb("r_top", [128, 8])
            self.g1 = ph.sb("r_g1", [128, 1]); self.g2 = ph.sb("r_g2", [128, 1]); self.dd = ph.sb("r_dd", [128, 1])
            self.c1 = ph.sb("r_c1", [128, NE]); self.c2 = ph.sb("r_c2", [128, NE])

    def run(self, n, res_ap_fn, res_R, idb, idf, trp, trf, rlp):
        ph, cx = self.ph, self.cx
        k = self.k; self.k += 1
        nbuf = self.nbuf
        s = self.s[k % nbuf]; y = self.y[k % nbuf]; yb = self.yb[k % nbuf]; ya = self.ya[k % nbuf]
        xTs = self.xTs[(n // 4) % nbuf]
        accr = cx.acc_dep[n]
        ph.dma("sp", s[:, :], cx.acc[n * 128:(n + 1) * 128, :], [accr], [s], s)
        if res_ap_fn is not None:
            for h in range(2):
                ph.dve(lambda e, h=h: e.tensor_tensor(out=s[:, h * 512:(h + 1) * 512], in0=res_ap_fn(h * 512, (h + 1) * 512),
                                                      in1=s[:, h * 512:(h + 1) * 512], op=ALU.add), [s] + list(res_R), [s])
        ln_rows(ph, s, self.mv, self.stats, self.rstd, self.nb, D)
        ph.act(lambda e: e.activation(out=y[:, :], in_=s[:, :], func=AF.Identity, bias=self.nb[:, :], scale=self.rstd[:, :]),
               [s, self.nb, self.rstd], [y])
        ph.pool(lambda e: e.tensor_tensor(out=y[:, :], in0=y[:, :], in1=self.grow[:, :], op=ALU.mult), [y, self.grow], [y])
        ph.pool(lambda e: e.tensor_tensor(out=y[:, :], in0=y[:, :], in1=self.brow[:, :], op=ALU.add), [y, self.brow], [y])
        if self.last:
            ph.dma("sp", cx.out[n * 128:(n + 1) * 128, :], y[:, :], [y], [cx.out_dep[n]], y)
            return
        ph.act(lambda e: e.activation(out=ya[:, :], in_=y[:, :], func=AF.Copy, scale=ALPHA), [y], [ya])
        ph.dma("sp", cx.acc[n * 128:(n + 1) * 128, :], ya[:, :], [ya], [accr], ya)
        ph.act(lambda e: e.activation(out=yb[:, :], in_=y[:, :], func=AF.Copy), [y], [yb])
        for c in range(8):
            ph.tr(trp[:, c * 128:(c + 1) * 128], yb[:, c * 128:(c + 1) * 128], idb[:, :], [yb, idb], [trp])
        sub = n % 4
        ph.dve(lambda e: e.tensor_copy(out=xTs[:, :, sub * 128:(sub + 1) * 128], in_=trp[:, :].rearrange("p (c t) -> p c t", c=8)),
               [trp], [xTs])
        if sub == 3:
            t0 = (n // 4) * 512
            ph.dma("sp", cx.xT.rearrange("c p s -> p c s")[:, :, t0:t0 + 512], xTs[:, :, :], [xTs], [cx.xT_dep[n // 4]], xTs)
        if self.router is not None:
            for half in range(2):
                tf = trf[half]
                for c in range(4):
                    cc = half * 4 + c
                    ph.tr(tf[:, c * 128:(c + 1) * 128], y[:, cc * 128:(cc + 1) * 128], idf[:, :], [y, idf], [tf])
                ph.act(lambda e, half=half, tf=tf: e.activation(out=self.yT[:, half * 4:(half + 1) * 4, :],
                                                                in_=tf[:, :].rearrange("p (c t) -> p c t", c=4), func=AF.Copy),
                       [tf], [self.yT])
            for c in range(8):
                ph.mm(rlp[:, 0:NE], self.yT[:, c, :], self.rw[:, c, :], c == 0, c == 7, [self.yT, self.rw], [rlp])
            ph.dve(lambda e: e.tensor_tensor(out=self.lg[:, :], in0=rlp[:, 0:NE], in1=self.rb[:, :], op=ALU.add), [rlp, self.rb], [self.lg])
            ph.dve(lambda e: e.max(out=self.top[:, :], in_=self.lg[:, :]), [self.lg], [self.top])
            ph.dve(lambda e: e.tensor_tensor(out=self.dd[:, :], in0=self.top[:, 0:1], in1=self.top[:, 1:2], op=ALU.subtract), [self.top], [self.dd])
            ph.act(lambda e: e.activation(out=self.g1[:, :], in_=self.dd[:, :], func=AF.Sigmoid), [self.dd], [self.g1])
            ph.dve(lambda e: e.tensor_scalar(out=self.g2[:, :], in0=self.g1[:, :], scalar1=-1.0, scalar2=1.0, op0=ALU.mult, op1=ALU.add),
                   [self.g1], [self.g2])
            ph.dve(lambda e: e.tensor_scalar(out=self.c1[:, :], in0=self.lg[:, :], scalar1=self.top[:, 0:1], scalar2=self.g1[:, :],
                                             op0=ALU.is_equal, op1=ALU.mult), [self.lg, self.top, self.g1], [self.c1])
            ph.dve(lambda e: e.tensor_scalar(out=self.c2[:, :], in0=self.lg[:, :], scalar1=self.top[:, 1:2], scalar2=self.g2[:, :],
                                             op0=ALU.is_equal, op1=ALU.mult), [self.lg, self.top, self.g2], [self.c2])
            ph.dve(lambda e: e.tensor_tensor(out=self.c1[:, :], in0=self.c1[:, :], in1=self.c2[:, :], op=ALU.add), [self.c1, self.c2], [self.c1])
            ph.dma("sp", cx.comb[n * 128:(n + 1) * 128, :], self.c1[:, :], [self.c1], [cx.comb_dep], self.c1)


def load_consts(ph, cx, ncols=None):
    ncols = ncols or cx.NCST
    c = ph.sb("cst", [128, ncols])
    ph.dma("sp", c[:, :], cx.cst[:, 0:ncols], [], [c], c)
    idb = ph.sb("idb", [128, 128], BF16)
    ph.dve(lambda e: e.tensor_copy(out=idb[:, :], in_=c[:, 0:128]), [c], [idb])
    return c, idb


def phase_prologue(nc, cx):
    ph = Phase(nc, "p0")
    mk_deps(ph, cx)
    c, idb = load_consts(ph, cx, 128)
    trp = ph.ps("trp", [128, 1024], BF16)
    xs = [ph.sb("x%d" % i, [128, D]) for i in range(2)]
    xa = [ph.sb("xa%d" % i, [128, D]) for i in range(2)]
    xb = [ph.sb("xb%d" % i, [128, D], BF16) for i in range(2)]
    xTs = [ph.sb("xT%d" % i, [128, 8, 512], BF16) for i in range(2)]
    for n in range(cx.NT):
        x = xs[n % 2]; a = xa[n % 2]; b = xb[n % 2]; xt = xTs[(n // 4) % 2]
        ph.dma("sp", x[:, :], cx.x[n * 128:(n + 1) * 128, :], [], [x], x)
        ph.act(lambda e, x=x, a=a: e.activation(out=a[:, :], in_=x[:, :], func=AF.Copy, scale=ALPHA), [x], [a])
        ph.dma("sp", cx.acc[n * 128:(n + 1) * 128, :], a[:, :], [a], [cx.acc_dep[n]], a)
        ph.dve(lambda e, x=x, b=b: e.tensor_copy(out=b[:, :], in_=x[:, :]), [x], [b])
        for cc in range(8):
            ph.tr(trp[:, cc * 128:(cc + 1) * 128], b[:, cc * 128:(cc + 1) * 128], idb[:, :], [b, idb], [trp])
        sub = n % 4
        ph.dve(lambda e, xt=xt, sub=sub: e.tensor_copy(out=xt[:, :, sub * 128:(sub + 1) * 128],
                                                        in_=trp[:, :].rearrange("p (c t) -> p c t", c=8)), [trp], [xt])
        if sub == 3:
            t0 = (n // 4) * 512
            ph.dma("sp", cx.xT.rearrange("c p s -> p c s")[:, :, t0:t0 + 512], xt[:, :, :], [xt], [cx.xT_dep[n // 4]], xt)
    ph.finish()


def phase_ffn(nc, cx, name, w1s, w3s, w2s, ln_g, ln_b, last, moe):
    ph = Phase(nc, name)
    mk_deps(ph, cx)
    c, idb = load_consts(ph, cx, 128)
    idf = Buf(c.t[:, 0:128], c.d)
    NEXP = len(w1s)
    w1g = [ph.sb("w1g%d" % g, [128, 8, n * 128], BF16) for g, (s0, n) in enumerate(FGROUPS)]
    w3g = [ph.sb("w3g%d" % g, [128, 8, n * 128], BF16) for g, (s0, n) in enumerate(FGROUPS)]
    w2g = [ph.sb("w2g%d" % g, [128, n, D], BF16) for g, (s0, n) in enumerate(FGROUPS)]

    import os
    noroll = bool(os.environ.get("NOROLL"))

    def load_w(e):
        xr = [acct[0]] if (noroll and e > 0) else []
        for g, (s0, n) in enumerate(FGROUPS):
            ph.dma("pool", w1g[g][:, :, :], w1s[e][:, s0 * 128:(s0 + n) * 128].rearrange("(c p) n -> p c n", p=128), xr, [w1g[g]], w1g[g])
            ph.dma("pool", w3g[g][:, :, :], w3s[e][:, s0 * 128:(s0 + n) * 128].rearrange("(c p) n -> p c n", p=128), xr, [w3g[g]], w3g[g])
        for g, (s0, n) in enumerate(FGROUPS):
            ph.dma("pool", w2g[g][:, :, :], w2s[e][s0 * 128:(s0 + n) * 128, :].rearrange("(c p) n -> p c n", p=128), xr, [w2g[g]], w2g[g])

    epi = Epi(ph, cx, ln_g, ln_b, last, nbuf=1)
    xTt = [ph.sb("xTt%d" % i, [128, 8, 512], BF16) for i in range(2)]
    gT = ph.sb("gT", [128, NFF, 512], BF16)
    gdeps = [Buf(gT.t, ph.dep("gT%d" % f)) for f in range(NFF)]
    sg = [ph.sb("sg%d" % i, [128, 512]) for i in range(2)]
    acct = [ph.sb("acct%d" % i, [128, D]) for i in range(1)]
    comb = None
    if moe:
        comb = ph.sb("comb", [128, cx.NT, NE])
        ph.dma("sp", comb[:, :, :], cx.comb.rearrange("(n p) e -> p n e", p=128), [cx.comb_dep], [comb], comb)
    hp = [ph.ps("hp%d" % i, [128, 512]) for i in range(4)]
    op = [ph.ps("op%d" % i, [128, 512]) for i in range(2)]
    trp = ph.ps("trp", [128, 1024], BF16)
    k = 0
    for e in range(NEXP):
        load_w(e)
        for tt in range(cx.NTT):
            xt = xTt[k % 2]
            ph.dma("sp", xt[:, :, :], cx.xT.rearrange("c p s -> p c s")[:, :, tt * 512:(tt + 1) * 512], [cx.xT_dep[tt]], [xt], xt)
            for f in range(NFF):
                g = min(f // 4, 5); fo = (f - FGROUPS[g][0]) * 128
                h1 = hp[(2 * f) % 4]; h3 = hp[(2 * f + 1) % 4]
                for kc in range(8):
                    ph.mm(h1[:, :], w1g[g][:, kc, fo:fo + 128], xt[:, kc, :], kc == 0, kc == 7, [w1g[g], xt], [h1])
                for kc in range(8):
                    ph.mm(h3[:, :], w3g[g][:, kc, fo:fo + 128], xt[:, kc, :], kc == 0, kc == 7, [w3g[g], xt], [h3])
                s = sg[f % 2]
                ph.act(lambda e_, h1=h1, s=s: e_.activation(out=s[:, :], in_=h1[:, :], func=AF.Silu), [h1], [s])
                ph.dve(lambda e_, h3=h3, s=s, f=f: e_.tensor_tensor(out=gT[:, f, :], in0=h3[:, :], in1=s[:, :], op=ALU.mult), [h3, s], [gdeps[f]])
            for sub in range(4):
                n = tt * 4 + sub
                for half in range(2):
                    o = op[half]
                    for f in range(NFF):
                        g = min(f // 4, 5); fi = f - FGROUPS[g][0]
                        ph.mm(o[:, :], gT[:, f, sub * 128:(sub + 1) * 128], w2g[g][:, fi, half * 512:(half + 1) * 512], f == 0, f == NFF - 1,
                              [gdeps[f], w2g[g]], [o])
                if e == NEXP - 1 and not moe:
                    epi.run(n, lambda lo, hi: op[lo // 512][:, :], [op[0], op[1]], idb, idf, trp, None, None)
                else:
                    a = acct[0]
                    ph.dma("sp", a[:, :], cx.acc[n * 128:(n + 1) * 128, :], [cx.acc_dep[n]], [a], a)
                    for half in range(2):
                        if moe:
                            ph.dve(lambda e_, a=a, half=half, n=n, e=e: e_.scalar_tensor_tensor(
                                out=a[:, half * 512:(half + 1) * 512], in0=op[half][:, :], scalar=comb[:, n, e:e + 1],
                                in1=a[:, half * 512:(half + 1) * 512], op0=ALU.mult, op1=ALU.add), [op[half], a, comb], [a])
                        else:
                            ph.dve(lambda e_, a=a, half=half: e_.tensor_tensor(out=a[:, half * 512:(half + 1) * 512], in0=op[half][:, :],
                                                                            in1=a[:, half * 512:(half + 1) * 512], op=ALU.add), [op[half], a], [a])
                    if e == NEXP - 1:
                        ph.dma("sp", cx.acc[n * 128:(n + 1) * 128, :], a[:, :], [a], [cx.acc_dep[n]], a)
                        epi.run(n, None, [], idb, idf, trp, None, None)
                    else:
                        ph.dma("sp", cx.acc[n * 128:(n + 1) * 128, :], a[:, :], [a], [cx.acc_dep[n]], a)
            k += 1
    ph.finish()


CST_ID, CST_ONES, CST_MTNEG, CST_M01S, CST_MT01, CST_SEL, CST_SC01, CST_SCNEG, NCST = 0, 128, 256, 384, 512, 640, 1152, 1664, 2176
PE_CW, PE_CB, PE_AG, PE_AB, PE_IB, PE_FB, NPE = 0, 124, 128, 132, 136, 137, 138


def phase_even_mixer(nc, cx, name, j):
    ph = Phase(nc, name)
    mk_deps(ph, cx)
    c, idb = load_consts(ph, cx)
    cd = [c]
    idf = Buf(c.t[:, 0:128], c.d)
    ones = c.t[:, CST_ONES:CST_ONES + 128]
    win = ph.sb("win", [128, 8, 3080], BF16)
    ph.dma("pool", win[:, :, :], cx.ab_w_in[j].rearrange("(c p) n -> p c n", p=128), [], [win], win)
    wout = ph.sb("wout", [128, 8, D], BF16)
    ph.dma("pool", wout[:, :, :], cx.ab_w_out[j].rearrange("(c p) n -> p c n", p=128), [], [wout], wout)
    pp = ph.sb("pp", [128, NPE])
    ph.dma("sp", pp[:, :], cx.pe[j], [], [pp], pp)
    nfb = ph.sb("nfb", [128, 1])
    ph.dve(lambda e: e.tensor_scalar(out=nfb[:, :], in0=pp[:, PE_FB:PE_FB + 1], scalar1=-1.0, scalar2=None, op0=ALU.mult), [pp], [nfb])
    hg = ph.sb("hg", [128, 512]); load_rows(ph, "sp", hg, cx.rows[cx.row_idx[("hg", j)]:cx.row_idx[("hg", j)] + 1, 0:512], 512)
    epi = Epi(ph, cx, cx.rows[cx.row_idx[("eln1g", j)]:cx.row_idx[("eln1g", j)] + 1, :], cx.rows[cx.row_idx[("eln1b", j)]:cx.row_idx[("eln1b", j)] + 1, :], False, nbuf=1)
    pj = [ph.ps("pj%d" % i, [128, 512]) for i in range(2)]
    pm = ph.ps("pm", [128, 512]); pm_st = pm; pm_ub = pm; pm_cols = pm; pm_sc = pm
    pn = ph.ps("pn", [128, 512]); pn_num = pn; pn_int = pn; pn_kv = pn
    pgi = ph.ps("pgi", [128, 512]); pgf = ph.ps("pgf", [128, 512])
    trp = ph.ps("trp", [128, 1024], BF16)
    xTt = [ph.sb("xTt%d" % i, [128, 8, 512], BF16) for i in range(1)]
    glu = ph.sb("glu", [128, 4, 542])
    ph.dve(lambda e: e.memset(glu[:, :, 0:30], 0.0), [], [glu])
    sig = ph.sb("sig", [128, 512])
    cy = ph.sb("cy", [128, 4, 512]); sq = ph.sb("sq", [128, 512])
    mean = ph.sb("mean", [128, 512]); var = ph.sb("var", [128, 512]); tmpa = ph.sb("tmpa", [128, 512])
    mixT = ph.sb("mixT", [128, 8, 512], BF16)
    qT = ph.sb("qT", [128, 4, 512], BF16); kT = ph.sb("kT", [128, 4, 512], BF16)
    ktok = ph.sb("ktok", [128, 4, 512], BF16)
    vext = ph.sb("vext", [128, 4, 4, 132], BF16)
    ph.dve(lambda e: e.memset(vext[:, :, :, 128:129], 1.0), [], [vext])
    osig = ph.sb("osig", [128, 4, 512])
    ig = ph.sb("ig", [128, 512]); sp_ = ph.sb("sp", [128, 512]); Bc = ph.sb("Bc", [128, 512]); lw = ph.sb("lw", [128, 512])
    Mx = ph.sb("Mx", [128, 512]); U = ph.sb("U", [128, 512]); rowsT = ph.sb("rowsT", [128, 512]); tmpr = ph.sb("tmpr", [128, 512])
    ph.dve(lambda e: e.memset(rowsT[:, :], 0.0), [], [rowsT])
    ph.dve(lambda e: e.memset(U[:, :], 0.0), [], [U])
    am = ph.sb("am", [128, 4]); nam = ph.sb("nam", [128, 4]); mp = ph.sb("mp", [128, 5]); sv = ph.sb("sv", [128, 4, 2]); t1 = ph.sb("t1", [128, 1]); t2 = ph.sb("t2", [128, 1])
    ph.dve(lambda e: e.memset(sv[:, :, :], 0.0), [], [sv])
    mcar = ph.sb("mcar", [128, 1])
    ph.dve(lambda e: e.memset(mcar[:, :], 0.0), [], [mcar])
    cols = ph.sb("cols", [128, 4, 128]); scol = ph.sb("scol", [128, 4, 128])
    vt = ph.sb("vt", [128, 512]); rows2 = ph.sb("rows2", [128, 512]); ax = ph.sb("ax", [128, 4]); nax = ph.sb("nax", [128, 4])
    vsc = ph.sb("vsc", [128, 132], BF16)
    ph.dve(lambda e: e.memset(rows2[:, :], 0.0), [], [rows2])
    wgi = ph.sb("wgi", [128, 8, 128], BF16); wgf = ph.sb("wgf", [128, 8, 128], BF16)
    ph.dve(lambda e: e.memset(wgi[:, :, :], 0.0), [], [wgi])
    ph.dve(lambda e: e.memset(wgf[:, :, :], 0.0), [], [wgf])
    for g in range(4):
        ph.dve(lambda e, g=g: e.tensor_copy(out=wgi[:, :, 32 * g:32 * g + 4], in_=win[:, :, 3072:3076]), [win], [wgi])
        ph.dve(lambda e, g=g: e.tensor_copy(out=wgf[:, :, 32 * g:32 * g + 4], in_=win[:, :, 3076:3080]), [win], [wgf])
    Cf = ph.sb("Cf", [128, 4, 132]); Cb = ph.sb("Cb", [128, 4, 132], BF16)
    ph.dve(lambda e: e.memset(Cf[:, :, :], 0.0), [], [Cf])
    ph.dve(lambda e: e.memset(Cb[:, :, :], 0.0), [], [Cb])
    PT = ph.sb("PT", [128, 128], BF16)
    ti = ph.sb("ti", [128, 129]); tot = ph.sb("tot", [128, 129]); dd = ph.sb("dd", [128, 1]); rec = ph.sb("rec", [128, 1])
    kw = ph.sb("kw", [128, 128], BF16)
    hraw = ph.sb("hraw", [128, 512]); hst = ph.sb("hst", [128, 4, 6]); hmv = ph.sb("hmv", [128, 4, 2]); hrs = ph.sb("hrs", [128, 4]); hb = ph.sb("hb", [128, 512], BF16)
    sc01 = c.t[:, CST_SC01:CST_SC01 + 512]; scneg = c.t[:, CST_SCNEG:CST_SCNEG + 512]
    k128 = 128 ** -0.5

    def proj_fm(col0, dst_fn, pjk):
        p = pj[pjk % 2]
        for kc in range(8):
            ph.mm(p[:, :], win[:, kc, col0:col0 + 128], xt[:, kc, :], kc == 0, kc == 7, [win, xt], [p])
        dst_fn(p)

    kpj = [0]
    import os
    emstop = int(os.environ.get("EMSTOP", "99"))
    for tt in range(cx.NTT if emstop > 0 else 0):
        xt = xTt[0]
        ph.dma("sp", xt[:, :, :], cx.xT.rearrange("c p s -> p c s")[:, :, tt * 512:(tt + 1) * 512], [cx.xT_dep[tt]], [xt], xt)
        for ch in range(4):
            p = pj[kpj[0] % 2]; kpj[0] += 1
            for kc in range(8):
                ph.mm(p[:, :], win[:, kc, 512 + ch * 128:512 + (ch + 1) * 128], xt[:, kc, :], kc == 0, kc == 7, [win, xt], [p])
            ph.act(lambda e, p=p: e.activation(out=sig[:, :], in_=p[:, :], func=AF.Sigmoid), [p], [sig])
            p2 = pj[kpj[0] % 2]; kpj[0] += 1
            for kc in range(8):
                ph.mm(p2[:, :], win[:, kc, ch * 128:(ch + 1) * 128], xt[:, kc, :], kc == 0, kc == 7, [win, xt], [p2])
            ph.dve(lambda e, p2=p2, ch=ch: e.tensor_tensor(out=glu[:, ch, 30:542], in0=p2[:, :], in1=sig[:, :], op=ALU.mult), [p2, sig], [glu])
            ph.dve(lambda e, ch=ch: e.tensor_scalar(out=cy[:, ch, :], in0=glu[:, ch, 0:512], scalar1=pp[:, PE_CW + ch * 31:PE_CW + ch * 31 + 1],
                                                    scalar2=pp[:, PE_CB + ch:PE_CB + ch + 1], op0=ALU.mult, op1=ALU.add), [glu, pp], [cy])
            for jj in range(1, 31):
                ph.dve(lambda e, ch=ch, jj=jj: e.scalar_tensor_tensor(out=cy[:, ch, :], in0=glu[:, ch, jj:jj + 512],
                                                                     scalar=pp[:, PE_CW + ch * 31 + jj:PE_CW + ch * 31 + jj + 1],
                                                                     in1=cy[:, ch, :], op0=ALU.mult, op1=ALU.add), [glu, pp, cy], [cy])
            ph.pool(lambda e, ch=ch: e.tensor_copy(out=glu[:, ch, 0:30], in_=glu[:, ch, 512:542]), [glu, cy], [glu])
        if emstop <= 1:
            continue
        for ch in range(4):
            ph.mm(pgi[:, :], ones, cy[:, ch, :], ch == 0, ch == 3, [cy] + cd, [pgi])
        for ch in range(4):
            ph.act(lambda e, ch=ch: e.activation(out=sq[:, :], in_=cy[:, ch, :], func=AF.Square), [cy], [sq])
            ph.mm(pgf[:, :], ones, sq[:, :], ch == 0, ch == 3, [sq] + cd, [pgf])
        ph.act(lambda e: e.activation(out=mean[:, :], in_=pgi[:, :], func=AF.Copy, scale=1.0 / 512), [pgi], [mean])
        ph.dve(lambda e: e.tensor_tensor(out=tmpa[:, :], in0=mean[:, :], in1=mean[:, :], op=ALU.mult), [mean], [tmpa])
        ph.dve(lambda e: e.scalar_tensor_tensor(out=var[:, :], in0=pgf[:, :], scalar=1.0 / 512, in1=tmpa[:, :], op0=ALU.mult, op1=ALU.subtract),
               [pgf, tmpa], [var])
        ph.dve(lambda e: e.tensor_scalar(out=var[:, :], in0=var[:, :], scalar1=EPS, scalar2=None, op0=ALU.add), [var], [var])
        ph.act(lambda e: e.activation(out=var[:, :], in_=var[:, :], func=AF.Sqrt), [var], [var])
        ph.dve(lambda e: e.reciprocal(out=var[:, :], in_=var[:, :]), [var], [var])
        for ch in range(4):
            ph.dve(lambda e, ch=ch: e.tensor_tensor(out=cy[:, ch, :], in0=cy[:, ch, :], in1=mean[:, :], op=ALU.subtract), [cy, mean], [cy])
            ph.dve(lambda e, ch=ch: e.tensor_tensor(out=cy[:, ch, :], in0=cy[:, ch, :], in1=var[:, :], op=ALU.mult), [cy, var], [cy])
            ph.act(lambda e, ch=ch: e.activation(out=mixT[:, ch, :], in_=cy[:, ch, :], func=AF.Silu, bias=pp[:, PE_AB + ch:PE_AB + ch + 1],
                                                 scale=pp[:, PE_AG + ch:PE_AG + ch + 1]), [cy, pp], [mixT])
        if emstop <= 2:
            continue
        for h in range(4):
            p = pj[kpj[0] % 2]; kpj[0] += 1
            for kc in range(8):
                ph.mm(p[:, :], win[:, kc, 1024 + h * 128:1024 + (h + 1) * 128], xt[:, kc, :], kc == 0, kc == 7, [win, xt], [p])
            ph.act(lambda e, p=p, h=h: e.activation(out=qT[:, h, :], in_=p[:, :], func=AF.Copy), [p], [qT])
            p = pj[kpj[0] % 2]; kpj[0] += 1
            for kc in range(8):
                ph.mm(p[:, :], win[:, kc, 1536 + h * 128:1536 + (h + 1) * 128], xt[:, kc, :], kc == 0, kc == 7, [win, xt], [p])
            ph.act(lambda e, p=p, h=h: e.activation(out=kT[:, h, :], in_=p[:, :], func=AF.Copy, scale=k128), [p], [kT])
        for sub in range(4):
            p = pj[kpj[0] % 2]; kpj[0] += 1
            for kc in range(8):
                ph.mm(p[:, :], xt[:, kc, sub * 128:(sub + 1) * 128], win[:, kc, 1536:2048], kc == 0, kc == 7, [win, xt], [p])
            ph.act(lambda e, p=p, sub=sub: e.activation(out=ktok[:, sub, :], in_=p[:, :], func=AF.Copy, scale=k128), [p], [ktok])
            p = pj[kpj[0] % 2]; kpj[0] += 1
            for kc in range(8):
                ph.mm(p[:, :], xt[:, kc, sub * 128:(sub + 1) * 128], win[:, kc, 2048:2560], kc == 0, kc == 7, [win, xt], [p])
            ph.dve(lambda e, p=p, sub=sub: e.tensor_copy(out=vext[:, sub, :, 0:128], in_=p[:, :].rearrange("p (h d) -> p h d", h=4)), [p], [vext])
            p = pj[kpj[0] % 2]; kpj[0] += 1
            for kc in range(8):
                ph.mm(p[:, :], xt[:, kc, sub * 128:(sub + 1) * 128], win[:, kc, 2560:3072], kc == 0, kc == 7, [win, xt], [p])
            ph.act(lambda e, p=p, sub=sub: e.activation(out=osig[:, sub, :], in_=p[:, :], func=AF.Sigmoid), [p], [osig])
        for kc in range(8):
            ph.mm(pgi[:, :], wgi[:, kc, :], xt[:, kc, :], kc == 0, kc == 7, [wgi, xt], [pgi])
        for kc in range(8):
            ph.mm(pgf[:, :], wgf[:, kc, :], xt[:, kc, :], kc == 0, kc == 7, [wgf, xt], [pgf])
        if emstop <= 3:
            continue
        G = [slice(32 * g, 32 * g + 4) for g in range(4)]
        ph.act(lambda e: e.activation(out=ig[:, :], in_=pgi[:, :], func=AF.Identity, bias=pp[:, PE_IB:PE_IB + 1]), [pgi, pp], [ig])
        ph.act(lambda e: e.activation(out=sp_[:, :], in_=pgf[:, :], func=AF.Exp, bias=nfb[:, :], scale=-1.0), [pgf, nfb], [sp_])
        ph.act(lambda e: e.activation(out=sp_[:, :], in_=sp_[:, :], func=AF.Ln, bias=1.0), [sp_], [sp_])
        ph.dve(lambda e: e.tensor_tensor_scan(out=Bc[:, :], data0=sc01, data1=sp_[:, :], initial=0.0, op0=ALU.mult, op1=ALU.add), [sp_] + cd, [Bc])
        ph.dve(lambda e: e.tensor_tensor(out=vt[:, :], in0=ig[:, :], in1=Bc[:, :], op=ALU.add), [ig, Bc], [vt])
        ph.dve(lambda e: e.tensor_tensor_scan(out=Mx[:, :], data0=scneg, data1=vt[:, :], initial=NEG, op0=ALU.add, op1=ALU.max), [vt] + cd, [Mx])
        ph.dve(lambda e: e.tensor_copy(out=ax[:, :], in_=Mx[:, :].rearrange("p (c t) -> p c t", c=4)[:, :, 127]), [Mx], [ax])
        ph.dve(lambda e: e.tensor_scalar(out=nax[:, :], in0=ax[:, :], scalar1=-1.0, scalar2=None, op0=ALU.mult), [ax], [nax])
        for cch in range(4):
            cs = slice(cch * 128, (cch + 1) * 128)
            ph.act(lambda e, cs=cs, cch=cch: e.activation(out=rowsT[G[0], cs], in_=vt[G[0], cs], func=AF.Exp, bias=nax[G[0], cch:cch + 1]), [vt, nax], [rowsT])
        for cch in range(4):
            cs = slice(cch * 128, (cch + 1) * 128)
            ph.dve(lambda e, cs=cs, cch=cch: e.tensor_scalar(out=lw[:, cs], in0=vt[:, cs], scalar1=Bc[:, cch * 128 + 127:cch * 128 + 128], scalar2=None,
                                                            op0=ALU.subtract), [vt, Bc], [lw])
        ph.dve(lambda e: e.tensor_reduce(out=am[:, :], in_=lw[:, :].rearrange("p (c t) -> p c t", c=4), axis=AX.X, op=ALU.max), [lw], [am])
        ph.dve(lambda e: e.tensor_scalar(out=nam[:, :], in0=am[:, :], scalar1=-1.0, scalar2=None, op0=ALU.mult), [am], [nam])
        for cch in range(4):
            cs = slice(cch * 128, (cch + 1) * 128)
            ph.act(lambda e, cs=cs, cch=cch: e.activation(out=rowsT[G[1], cs], in_=lw[G[1], cs], func=AF.Exp, bias=nam[G[1], cch:cch + 1]), [lw, nam], [rowsT])
        ph.dve(lambda e: e.tensor_copy(out=mp[:, 0:1], in_=mcar[:, :]), [mcar], [mp])
        for cch in range(4):
            be = Bc[:, cch * 128 + 127:cch * 128 + 128]
            ph.dve(lambda e, cch=cch, be=be: e.tensor_tensor(out=t1[:, :], in0=mp[:, cch:cch + 1], in1=be, op=ALU.subtract), [mp, Bc], [t1])
            ph.dve(lambda e, cch=cch: e.tensor_tensor(out=mp[:, cch + 1:cch + 2], in0=t1[:, :], in1=am[:, cch:cch + 1], op=ALU.max), [t1, am], [mp])
            ph.dve(lambda e, cch=cch: e.tensor_tensor(out=t1[:, :], in0=t1[:, :], in1=mp[:, cch + 1:cch + 2], op=ALU.subtract), [t1, mp], [t1])
            ph.dve(lambda e, cch=cch: e.tensor_tensor(out=t2[:, :], in0=am[:, cch:cch + 1], in1=mp[:, cch + 1:cch + 2], op=ALU.subtract), [am, mp], [t2])
            ph.act(lambda e, cch=cch: e.activation(out=sv[:, cch, 0:1], in_=t1[:, :], func=AF.Exp), [t1], [sv])
            ph.act(lambda e, cch=cch: e.activation(out=sv[:, cch, 1:2], in_=t2[:, :], func=AF.Exp), [t2], [sv])
        ph.dve(lambda e: e.tensor_copy(out=mcar[:, :], in_=mp[:, 4:5]), [mp], [mcar])
        for cch in range(4):
            cs = slice(cch * 128, (cch + 1) * 128)
            ph.dve(lambda e, cs=cs, cch=cch: e.tensor_scalar(out=U[:, cs], in0=Mx[:, cs], scalar1=mp[:, cch:cch + 1], scalar2=-1.0, op0=ALU.max, op1=ALU.mult),
                   [Mx, mp], [U])
            ph.act(lambda e, cs=cs, cch=cch: e.activation(out=rowsT[G[2], cs], in_=U[G[2], cs], func=AF.Exp, bias=mp[G[2], cch:cch + 1]), [U, mp], [rowsT])
            ph.act(lambda e, cs=cs, cch=cch: e.activation(out=rows2[G[2], cs], in_=U[G[2], cs], func=AF.Exp, bias=ax[G[2], cch:cch + 1]), [U, ax], [rows2])
            ph.dve(lambda e, cs=cs, cch=cch: e.tensor_scalar(out=rows2[G[0], cs], in0=c.t[G[0], CST_ONES:CST_ONES + 128], scalar1=sv[G[0], cch, 0:1], scalar2=None,
                                                            op0=ALU.mult), [sv] + cd, [rows2])
            ph.dve(lambda e, cs=cs, cch=cch: e.tensor_scalar(out=rows2[G[1], cs], in0=c.t[G[1], CST_ONES:CST_ONES + 128], scalar1=sv[G[1], cch, 1:2], scalar2=None,
                                                            op0=ALU.mult), [sv] + cd, [rows2])
        ph.dve(lambda e: e.tensor_tensor(out=tmpr[:, :], in0=Bc[:, :], in1=U[:, :], op=ALU.add), [Bc, U], [tmpr])
        ph.act(lambda e: e.activation(out=rowsT[G[3], :], in_=tmpr[G[3], :], func=AF.Exp), [tmpr], [rowsT])
        if emstop <= 4:
            continue
        for sub in range(4):
            ph.tr(pm[:, 256:384], rowsT[:, sub * 128:(sub + 1) * 128], idf[:, :], [rowsT, idf], [pm_cols])
            ph.act(lambda e, sub=sub: e.activation(out=cols[:, sub, :], in_=pm[:, 256:384], func=AF.Copy), [pm_cols], [cols])
            ph.tr(pm[:, 384:512], rows2[:, sub * 128:(sub + 1) * 128], idf[:, :], [rows2, idf], [pm_sc])
            ph.act(lambda e, sub=sub: e.activation(out=scol[:, sub, :], in_=pm[:, 384:512], func=AF.Copy), [pm_sc], [scol])
        if emstop <= 5:
            continue
        hstop = int(os.environ.get("HSTOP", "99"))
        for sub in range(4):
            cs = slice(sub * 128, (sub + 1) * 128)
            for h in range(4):
                ph.mm(pm[:, 0:128], kT[:, h, cs], qT[:, h, cs], True, True, [kT, qT], [pm_st])
                ph.dve(lambda e: e.tensor_tensor(out=PT[:, :], in0=pm[:, 0:128], in1=c.t[:, CST_MT01:CST_MT01 + 128], op=ALU.mult), [pm_st] + cd, [PT])
                ph.pool(lambda e, sub=sub, h=h: e.tensor_scalar(out=vsc[:, 0:130], in0=vext[:, sub, h, 0:130], scalar1=cols[:, sub, h:h + 1], scalar2=None, op0=ALU.mult),
                        [vext, cols], [vsc])
                if hstop <= 1:
                    continue
                ph.mm(pn[:, 0:129], PT[:, :], vsc[:, 0:129], True, True, [PT, vsc], [pn_num])
                ph.mm(pn[:, 129:258], qT[:, h, cs], Cb[:, h, 0:129], True, True, [qT, Cb], [pn_int])
                ph.act(lambda e, sub=sub, h=h: e.activation(out=ti[:, :], in_=pn[:, 129:258], func=AF.Copy, scale=cols[:, sub, 64 + h:64 + h + 1]), [pn_int, cols], [ti])
                ph.dve(lambda e, sub=sub, h=h: e.scalar_tensor_tensor(out=tot[:, :], in0=pn[:, 0:129], scalar=scol[:, sub, 64 + h:64 + h + 1], in1=ti[:, :],
                                                                      op0=ALU.mult, op1=ALU.add), [pn_num, ti, scol], [tot])
                if hstop <= 2:
                    continue
                ph.act(lambda e: e.activation(out=dd[:, :], in_=tot[:, 128:129], func=AF.Abs), [tot], [dd])
                ph.dve(lambda e, sub=sub, h=h: e.tensor_scalar(out=dd[:, :], in0=dd[:, :], scalar1=cols[:, sub, 96 + h:96 + h + 1], scalar2=None,
                                                               op0=ALU.max), [dd, cols], [dd])
                ph.dve(lambda e: e.reciprocal(out=rec[:, :], in_=dd[:, :]), [dd], [rec])
                ph.dve(lambda e, h=h: e.tensor_scalar(out=hraw[:, h * 128:(h + 1) * 128], in0=tot[:, 0:128], scalar1=rec[:, :], scalar2=None, op0=ALU.mult),
                       [tot, rec], [hraw])
                if hstop <= 3:
                    continue
                ph.dve(lambda e, sub=sub, h=h: e.tensor_scalar(out=kw[:, :], in0=ktok[:, sub, h * 128:(h + 1) * 128], scalar1=cols[:, sub, 32 + h:32 + h + 1],
                                                               scalar2=None, op0=ALU.mult), [ktok, cols], [kw])
                ph.mm(pn[:, 258:387], kw[:, :], vext[:, sub, h, 0:129], True, True, [kw, vext], [pn_kv])
                ph.dve(lambda e, sub=sub, h=h: e.tensor_scalar(out=Cf[:, h, 0:129], in0=Cf[:, h, 0:129], scalar1=scol[:, sub, h:h + 1], scalar2=None, op0=ALU.mult),
                       [Cf, scol], [Cf])
                ph.dve(lambda e, sub=sub, h=h: e.scalar_tensor_tensor(out=Cf[:, h, 0:129], in0=pn[:, 258:387], scalar=scol[:, sub, 32 + h:32 + h + 1], in1=Cf[:, h, 0:129],
                                                                      op0=ALU.mult, op1=ALU.add), [pn_kv, scol, Cf], [Cf])
                ph.act(lambda e, h=h: e.activation(out=Cb[:, h, 0:129], in_=Cf[:, h, 0:129], func=AF.Copy), [Cf], [Cb])
            if hstop <= 4:
                continue
            for h in range(4):
                ph.dve(lambda e, h=h: e.bn_stats(out=hst[:, h, :], in_=hraw[:, h * 128:(h + 1) * 128]), [hraw], [hst])
                ph.dve(lambda e, h=h: e.bn_aggr(out=hmv[:, h, :], in_=hst[:, h, :]), [hst], [hmv])
            ph.dve(lambda e: e.tensor_scalar(out=hrs[:, :], in0=hmv[:, :, 1], scalar1=EPS, scalar2=None, op0=ALU.add), [hmv], [hrs])
            ph.act(lambda e: e.activation(out=hrs[:, :], in_=hrs[:, :], func=AF.Sqrt), [hrs], [hrs])
            ph.dve(lambda e: e.reciprocal(out=hrs[:, :], in_=hrs[:, :]), [hrs], [hrs])
            for h in range(4):
                ph.dve(lambda e, h=h: e.tensor_scalar(out=hraw[:, h * 128:(h + 1) * 128], in0=hraw[:, h * 128:(h + 1) * 128], scalar1=hmv[:, h, 0:1],
                                                      scalar2=hrs[:, h:h + 1], op0=ALU.subtract, op1=ALU.mult), [hraw, hmv, hrs], [hraw])
            ph.pool(lambda e: e.tensor_tensor(out=hraw[:, :], in0=hraw[:, :], in1=hg[:, :], op=ALU.mult), [hraw, hg], [hraw])
            ph.dve(lambda e, sub=sub: e.tensor_tensor(out=hb[:, :], in0=hraw[:, :], in1=osig[:, sub, :], op=ALU.mult), [hraw, osig], [hb])
            for h in range(4):
                ph.tr(trp[:, h * 128:(h + 1) * 128], hb[:, h * 128:(h + 1) * 128], idb[:, :], [hb, idb], [trp])
            ph.act(lambda e, cs=cs: e.activation(out=mixT[:, 4:8, cs], in_=trp[:, 0:512].rearrange("p (c t) -> p c t", c=4), func=AF.Copy), [trp], [mixT])
        if emstop <= 6:
            continue
        for sub in range(4):
            n = tt * 4 + sub
            for half in range(2):
                for kc in range(8):
                    ph.mm(pj[half][:, :], mixT[:, kc, sub * 128:(sub + 1) * 128], wout[:, kc, half * 512:(half + 1) * 512], kc == 0, kc == 7, [mixT, wout], [pj[half]])
            epi.run(n, lambda lo, hi: pj[lo // 512][:, :], [pj[0], pj[1]], idb, idf, trp, None, None)
    ph.finish()


def phase_odd_mixer(nc, cx, name, j):
    ph = Phase(nc, name)
    mk_deps(ph, cx)
    c, idb = load_consts(ph, cx, 640)
    cd = [c]
    idf = Buf(c.t[:, 0:128], c.d)
    S = cx.S
    win = ph.sb("win", [128, 8, 2560], BF16)
    ph.dma("pool", win[:, :, :], cx.cd_w_in[j].rearrange("(c p) n -> p c n", p=128), [], [win], win)
    wout = ph.sb("wout", [128, 8, D], BF16)
    ph.dma("pool", wout[:, :, :], cx.cd_w_out[j].rearrange("(c p) n -> p c n", p=128), [], [wout], wout)
    ri = cx.row_idx
    cng = ph.sb("cng", [128, 512]); load_rows(ph, "sp", cng, cx.rows[ri[("cng", j)]:ri[("cng", j)] + 1, 0:512], 512)
    cnb = ph.sb("cnb", [128, 512]); load_rows(ph, "sp", cnb, cx.rows[ri[("cnb", j)]:ri[("cnb", j)] + 1, 0:512], 512)
    bsb = ph.sb("bsb", [128, 512]); load_rows(ph, "sp", bsb, cx.rows[ri[("bs", j)]:ri[("bs", j)] + 1, 0:512], 512)
    wsf = ph.sb("wsf", [128, 4, 128])
    ph.dma("sp", wsf[:, :, :], cx.wst[j].rearrange("g s t -> s g t"), [], [wsf], wsf)
    WcT = ph.sb("WcT", [128, 4, 128], BF16)
    for g in range(4):
        ph.dve(lambda e, g=g: e.tensor_tensor(out=WcT[:, g, :], in0=wsf[:, g, :], in1=c.t[:, CST_MT01:CST_MT01 + 128], op=ALU.mult), [wsf] + cd, [WcT])
    epi = Epi(ph, cx, cx.rows[ri[("oln1g", j)]:ri[("oln1g", j)] + 1, :], cx.rows[ri[("oln1b", j)]:ri[("oln1b", j)] + 1, :], False,
              router={"w": cx.router_w[j], "b": cx.rows[ri[("rb", j)]:ri[("rb", j)] + 1, 0:NE]}, nbuf=1)
    kTres = ph.sb("kTres", [128, 4, S], BF16)
    Vres = ph.sb("Vres", [128, cx.NT, 512], BF16)
    pj = [ph.ps("pj%d" % i, [128, 512]) for i in range(2)]
    pz = [ph.ps("pz%d" % i, [128, 512]) for i in range(2)]
    pg = ph.ps("pg", [128, 512]); pos = [ph.ps("po%d" % i, [128, 512]) for i in range(2)]
    trp = ph.ps("trp", [128, 1024], BF16)
    xt = ph.sb("xTt", [128, 8, 512], BF16)
    uT = ph.sb("uT", [128, 4, 512], BF16)
    zf = ph.sb("zf", [128, 512]); zb = ph.sb("zb", [128, 512], BF16)
    zst = ph.sb("zst", [128, 1, 6]); zmv = ph.sb("zmv", [128, 2]); zrs = ph.sb("zrs", [128, 1]); znb = ph.sb("znb", [128, 1])
    gtmp = zf
    mixT = ph.sb("mixT", [128, 8, 512], BF16)
    qT = ph.sb("qT", [128, 4, 512], BF16)
    NB = 2
    ebuf = [ph.sb("ebuf%d" % i, [128, 512]) for i in range(NB)]
    spb = [ph.sb("spb%d" % i, [128, 513]) for i in range(NB)]
    csb = [ph.sb("csb%d" % i, [128, 513]) for i in range(NB)]
    attb = [ph.sb("attb%d" % i, [128, 512], BF16) for i in range(NB)]
    attT = ph.sb("attT", [128, 2, 4, 128], BF16)
    for i in range(NB):
        ph.dve(lambda e, i=i: e.memset(spb[i][:, 0:1], 0.0), [], [spb[i]])
    cars = [ph.sb("car%d" % i, [128, 1]) for i in range(NB)]
    nbb = [ph.sb("nbb%d" % i, [128, 1]) for i in range(NB)]
    dout = ph.sb("dout", [128, 512], BF16)
    onesr = c.t[:, CST_ONES:CST_ONES + 128]
    ones513 = ph.sb("ones513", [128, 513], BF16)
    ph.dve(lambda e: e.memset(ones513[:, :], 1.0), [], [ones513])
    m01s = c.t[:, CST_M01S:CST_M01S + 128]
    kpj = [0]
    kz = [0]
    import os
    for tt in range(int(os.environ.get("OMT", cx.NTT))):
        t0 = tt * 512
        ph.dma("sp", xt[:, :, :], cx.xT.rearrange("c p s -> p c s")[:, :, t0:t0 + 512], [cx.xT_dep[tt]], [xt], xt)
        for ch in range(4):
            p = pj[kpj[0] % 2]; kpj[0] += 1
            for kc in range(8):
                ph.mm(p[:, :], win[:, kc, ch * 128:(ch + 1) * 128], xt[:, kc, :], kc == 0, kc == 7, [win, xt], [p])
            ph.act(lambda e, p=p, ch=ch: e.activation(out=uT[:, ch, :], in_=p[:, :], func=AF.Gelu), [p], [uT])
        for pr in range(4):
            p = pj[kpj[0] % 2]; kpj[0] += 1
            for kc in range(8):
                ph.mm(p[:, :], win[:, kc, 1024 + pr * 128:1024 + (pr + 1) * 128], xt[:, kc, :], kc == 0, kc == 7, [win, xt], [p])
            ph.act(lambda e, p=p, pr=pr: e.activation(out=qT[:, pr, :], in_=p[:, :], func=AF.Copy, scale=0.125), [p], [qT])
            p = pj[kpj[0] % 2]; kpj[0] += 1
            for kc in range(8):
                ph.mm(p[:, :], win[:, kc, 1536 + pr * 128:1536 + (pr + 1) * 128], xt[:, kc, :], kc == 0, kc == 7, [win, xt], [p])
            ph.dve(lambda e, p=p, pr=pr, t0=t0: e.tensor_copy(out=kTres[:, pr, t0:t0 + 512], in_=p[:, :]), [p], [kTres])
        for sub in range(4):
            n = tt * 4 + sub
            cs = slice(sub * 128, (sub + 1) * 128)
            p = pj[kpj[0] % 2]; kpj[0] += 1
            for kc in range(8):
                ph.mm(p[:, :], xt[:, kc, cs], win[:, kc, 2048:2560], kc == 0, kc == 7, [win, xt], [p])
            ph.act(lambda e, p=p, n=n: e.activation(out=Vres[:, n, :], in_=p[:, :], func=AF.Copy), [p], [Vres])
            p = pj[kpj[0] % 2]; kpj[0] += 1
            for kc in range(8):
                ph.mm(p[:, :], xt[:, kc, cs], win[:, kc, 512:1024], kc == 0, kc == 7, [win, xt], [p])
            ph.act(lambda e, p=p: e.activation(out=zf[:, :], in_=p[:, :], func=AF.Gelu), [p], [zf])
            ln_rows(ph, zf, zmv, zst, zrs, znb, 512)
            ph.act(lambda e: e.activation(out=zf[:, :], in_=zf[:, :], func=AF.Identity, bias=znb[:, :], scale=zrs[:, :]), [zf, znb, zrs], [zf])
            ph.pool(lambda e: e.tensor_tensor(out=zf[:, :], in0=zf[:, :], in1=cng[:, :], op=ALU.mult), [zf, cng], [zf])
            ph.pool(lambda e: e.tensor_tensor(out=zb[:, :], in0=zf[:, :], in1=cnb[:, :], op=ALU.add), [zf, cnb], [zb])
            for g in range(4):
                ph.mm(pg[:, g * 128:(g + 1) * 128], zb[:, g * 128:(g + 1) * 128], WcT[:, g, :], True, True, [zb, WcT], [pg])
            ph.dve(lambda e: e.tensor_tensor(out=gtmp[:, :], in0=pg[:, :], in1=bsb[:, :], op=ALU.add), [pg, bsb], [gtmp])
            ph.dve(lambda e, cs=cs: e.tensor_tensor(out=mixT[:, 0:4, cs], in0=gtmp[:, :].rearrange("p (g t) -> p g t", g=4), in1=uT[:, :, cs], op=ALU.mult),
                   [gtmp, uT], [mixT])
            kend = (n + 1) * 128
            KT = (kend + 511) // 512
            for pr in range(4):
                J = (0, 1)
                for j in J:
                    ph.dve(lambda e, j=j: e.memset(cars[j][:, :], 0.0), [], [cars[j]])
                first = [True, True]
                for kt in reversed(range(KT)):
                    k0 = kt * 512
                    w = min(kend, k0 + 512) - k0
                    nblk = w // 128
                    for j in J:
                        b0 = j * 64
                        ph.mm(pz[j][:, 0:w], qT[b0:b0 + 64, pr, cs], kTres[b0:b0 + 64, pr, k0:k0 + w], True, True, [qT, kTres], [pz[j]])
                    for j in J:
                        ph.act(lambda e, j=j, w=w: e.activation(out=ebuf[j][:, 0:w], in_=pz[j][:, 0:w], func=AF.Exp), [pz[j]], [ebuf[j]])
                    for j in J:
                        ph.act(lambda e, j=j, w=w: e.activation(out=spb[j][:, 1:w + 1], in_=ebuf[j][:, 0:w], func=AF.Ln, bias=1.0), [ebuf[j]], [spb[j]])
                    if kt == KT - 1:
                        for j in J:
                            ph.pool(lambda e, j=j, w=w: e.tensor_tensor(out=spb[j][:, 1 + w - 128:1 + w], in0=spb[j][:, 1 + w - 128:1 + w], in1=m01s, op=ALU.mult),
                                    [spb[j]] + cd, [spb[j]])
                            ph.pool(lambda e, j=j, w=w: e.tensor_tensor(out=ebuf[j][:, w - 128:w], in0=ebuf[j][:, w - 128:w], in1=m01s, op=ALU.mult),
                                    [ebuf[j]] + cd, [ebuf[j]])
                    for j in J:
                        ph.dve(lambda e, j=j, w=w: e.tensor_tensor_scan(out=csb[j][:, 0:w + 1], data0=ones513[:, 0:w + 1], data1=spb[j][:, 0:w + 1], initial=0.0,
                                                                       op0=ALU.mult, op1=ALU.add), [spb[j], ones513], [csb[j]])
                    for j in J:
                        ph.dve(lambda e, j=j, w=w: e.tensor_tensor(out=cars[j][:, :], in0=csb[j][:, w:w + 1], in1=cars[j][:, :], op=ALU.add), [csb[j], cars[j]], [cars[j]])
                        ph.dve(lambda e, j=j: e.tensor_scalar(out=nbb[j][:, :], in0=cars[j][:, :], scalar1=-1.0, scalar2=None, op0=ALU.mult), [cars[j]], [nbb[j]])
                    for j in J:
                        ph.act(lambda e, j=j, w=w: e.activation(out=csb[j][:, 0:w], in_=csb[j][:, 0:w], func=AF.Exp, bias=nbb[j][:, :]), [csb[j], nbb[j]], [csb[j]])
                    for j in J:
                        ph.pool(lambda e, j=j, w=w: e.tensor_tensor(out=attb[j][:, 0:w], in0=ebuf[j][:, 0:w], in1=csb[j][:, 0:w], op=ALU.mult), [ebuf[j], csb[j]], [attb[j]])
                    for j in J:
                        for jb in range(nblk):
                            ph.tr(trp[:, j * 512 + jb * 128:j * 512 + (jb + 1) * 128], attb[j][:, jb * 128:(jb + 1) * 128], idb[:, :], [attb[j], idb], [trp])
                    ph.dve(lambda e, nblk=nblk: e.tensor_copy(out=attT[:, :, 0:nblk, :], in_=trp[:, :].rearrange("p (j b t) -> p j b t", j=2, b=4)[:, :, 0:nblk, :]),
                           [trp], [attT])
                    for j in J:
                        h = 2 * pr + j
                        for jb in range(nblk):
                            kb = kt * 4 + jb
                            last = (kt == 0 and jb == nblk - 1)
                            ph.mm(pos[j][:, h * 64:(h + 1) * 64], attT[:, j, jb, :], Vres[:, kb, h * 64:(h + 1) * 64], first[j], last, [attT, Vres], [pos[j]])
                            first[j] = False
            for j in (0, 1):
                ph.act(lambda e, j=j: e.activation(out=dout[:, :].rearrange("p (r j d) -> p r j d", r=4, j=2)[:, :, j, :],
                                                   in_=pos[j][:, :].rearrange("p (r j d) -> p r j d", r=4, j=2)[:, :, j, :], func=AF.Copy), [pos[j]], [dout])
            for cc in range(4):
                ph.tr(trp[:, cc * 128:(cc + 1) * 128], dout[:, cc * 128:(cc + 1) * 128], idb[:, :], [dout, idb], [trp])
            ph.dve(lambda e, cs=cs: e.tensor_copy(out=mixT[:, 4:8, cs], in_=trp[:, 0:512].rearrange("p (c t) -> p c t", c=4)), [trp], [mixT])
        for sub in range(4):
            n = tt * 4 + sub
            for half in range(2):
                for kc in range(8):
                    ph.mm(pj[half][:, :], mixT[:, kc, sub * 128:(sub + 1) * 128], wout[:, kc, half * 512:(half + 1) * 512], kc == 0, kc == 7, [mixT, wout], [pj[half]])
            epi.run(n, lambda lo, hi: pj[lo // 512][:, :], [pj[0], pj[1]], idb, idf, trp, pz, pg)
    ph.finish()


def mk_deps(ph, cx):
    cx.acc_dep = [ph.region("acc%d" % n) for n in range(cx.NT)]
    cx.out_dep = [ph.region("out%d" % n) for n in range(cx.NT)]
    cx.xT_dep = [ph.region("xT%d" % t) for t in range(cx.NTT)]
    cx.comb_dep = ph.region("comb")


ROW_KEYS_EVEN = ["eln1g", "eln1b", "eln2g", "eln2b", "hg"]
ROW_KEYS_ODD = ["oln1g", "oln1b", "oln2g", "oln2b", "cng", "cnb", "bs", "rb"]


def row_index():
    idx = {}
    k = 0
    for j in range(2):
        for key in ROW_KEYS_EVEN + ROW_KEYS_ODD:
            idx[(key, j)] = k
            k += 1
    return idx, k


def build(S, layers):
    nc = bass.Bass("TRN2", target_bir_lowering=False)
    gst = ExitStack()
    make_sempool(nc, gst)
    nc._gst = gst
    cx = Ctx()
    cx.S = S; cx.NT = S // 128; cx.NTT = S // 512; cx.NCST = NCST
    cx.row_idx, nrows = row_index()

    def inp(name, shape):
        return nc.dram_tensor(name, list(shape), F32, kind="ExternalInput").ap()

    cx.x = inp("x", [S, D])
    cx.out = nc.dram_tensor("out", [S, D], F32, kind="ExternalOutput").ap()
    cx.cst = inp("cst", [128, NCST])
    cx.rows = inp("rows", [nrows, D])
    cx.pe = [inp("pe0", [128, NPE]), inp("pe1", [128, NPE])]
    cx.wst = inp("wst", [2, 4, 128, 128])
    cx.wkeys = []
    if any(l % 2 == 0 for l in layers):
        cx.ab_w_in = inp("ab_w_in", [2, D, 3080]); cx.ab_w_out = inp("ab_w_out", [2, D, D])
        cx.ffn_w1 = inp("ffn_w1", [2, D, DFF]); cx.ffn_w3 = inp("ffn_w3", [2, D, DFF]); cx.ffn_w2 = inp("ffn_w2", [2, DFF, D])
        cx.wkeys += ["ab_w_in", "ab_w_out", "ffn_w1", "ffn_w3", "ffn_w2"]
    if any(l % 2 == 1 for l in layers):
        cx.cd_w_in = inp("cd_w_in", [2, D, 2560]); cx.cd_w_out = inp("cd_w_out", [2, D, D])
        cx.router_w = inp("router_w", [2, D, NE])
        cx.moe_w1 = inp("moe_w1", [2, NE, D, DFF]); cx.moe_w3 = inp("moe_w3", [2, NE, D, DFF]); cx.moe_w2 = inp("moe_w2", [2, NE, DFF, D])
        cx.wkeys += ["cd_w_in", "cd_w_out", "router_w", "moe_w1", "moe_w3", "moe_w2"]
    nc._wkeys = cx.wkeys
    cx.acc = nc.dram_tensor("acc", [S, D], F32).ap()
    cx.xT = nc.dram_tensor("xT", [8, 128, S], BF16).ap()
    cx.comb = nc.dram_tensor("comb", [S, NE], F32).ap()
    ri = cx.row_idx
    import os
    kstop = int(os.environ.get("KSTOP", "99"))
    phase_prologue(nc, cx)
    if kstop <= 0:
        return nc
    for li, l in enumerate(layers):
        j = l // 2
        last = (li == len(layers) - 1)
        if l % 2 == 0:
            phase_even_mixer(nc, cx, "em%d" % l, j)
            if kstop <= 1:
                return nc
            phase_ffn(nc, cx, "ef%d" % l, [cx.ffn_w1[j]], [cx.ffn_w3[j]], [cx.ffn_w2[j]],
                      cx.rows[ri[("eln2g", j)]:ri[("eln2g", j)] + 1, :], cx.rows[ri[("eln2b", j)]:ri[("eln2b", j)] + 1, :], last, False)
        else:
            phase_odd_mixer(nc, cx, "om%d" % l, j)
            if kstop <= 1:
                phase_dump(nc, cx)
                return nc
            phase_ffn(nc, cx, "of%d" % l, [cx.moe_w1[j][e] for e in range(NE)], [cx.moe_w3[j][e] for e in range(NE)],
                      [cx.moe_w2[j][e] for e in range(NE)],
                      cx.rows[ri[("oln2g", j)]:ri[("oln2g", j)] + 1, :], cx.rows[ri[("oln2b", j)]:ri[("oln2b", j)] + 1, :], last, True)
    return nc


def host_consts():
    c = np.zeros((128, NCST), np.float32)
    i = np.arange(128)
    c[:, CST_ID:CST_ID + 128] = np.eye(128, dtype=np.float32)
    c[:, CST_ONES:CST_ONES + 128] = 1.0
    c[:, CST_MTNEG:CST_MTNEG + 128] = np.where(i[:, None] <= i[None, :], 0.0, NEG)
    c[:, CST_M01S:CST_M01S + 128] = (i[None, :] < i[:, None]).astype(np.float32)
    c[:, CST_MT01:CST_MT01 + 128] = (i[:, None] <= i[None, :]).astype(np.float32)
    for h in range(4):
        c[h, CST_SEL + h * 128:CST_SEL + (h + 1) * 128] = 1.0
    t = np.arange(512)
    c[:, CST_SC01:CST_SC01 + 512] = (t % 128 != 0).astype(np.float32)[None, :]
    c[:, CST_SCNEG:CST_SCNEG + 512] = np.where(t % 128 == 0, NEG, 0.0)[None, :]
    return c


def host_layout(inp):
    f = lambda a: np.asarray(a, dtype=np.float32)
    idx, nrows = row_index()
    rows = np.zeros((nrows, D), np.float32)

    def put(key, j, v):
        v = f(v).reshape(-1)
        rows[idx[(key, j)], :v.size] = v

    pes = []
    for j in range(2):
        put("eln1g", j, inp["ab_ln1_g"][j]); put("eln1b", j, inp["ab_ln1_b"][j])
        put("eln2g", j, inp["ab_ln2_g"][j]); put("eln2b", j, inp["ab_ln2_b"][j])
        put("hg", j, inp["b_norm_g"][j])
        put("oln1g", j, inp["cd_ln1_g"][j]); put("oln1b", j, inp["cd_ln1_b"][j])
        put("oln2g", j, inp["cd_ln2_g"][j]); put("oln2b", j, inp["cd_ln2_b"][j])
        put("cng", j, inp["c_norm_g"][j]); put("cnb", j, inp["c_norm_b"][j])
        put("bs", j, inp["c_b_s"][j]); put("rb", j, inp["router_b"][j])
        pe = np.zeros((128, NPE), np.float32)
        cw = f(inp["a_conv_w"][j])
        pe[:, PE_CW:PE_CW + 124] = cw.reshape(31, 4, 128).transpose(2, 1, 0).reshape(128, 124)
        pe[:, PE_CB:PE_CB + 4] = f(inp["a_conv_b"][j]).reshape(4, 128).T
        pe[:, PE_AG:PE_AG + 4] = f(inp["a_norm_g"][j]).reshape(4, 128).T
        pe[:, PE_AB:PE_AB + 4] = f(inp["a_norm_b"][j]).reshape(4, 128).T
        gb = f(inp["ab_gate_bias"][j])
        for g in range(4):
            pe[32 * g:32 * g + 4, PE_IB] = gb[0:4]
            pe[32 * g:32 * g + 4, PE_FB] = gb[4:8]
        pes.append(pe)
    wst = np.ascontiguousarray(f(inp["c_w_s"]).transpose(0, 1, 3, 2))
    return rows, pes, wst


WKEYS = ["ab_w_in", "ab_w_out", "ffn_w1", "ffn_w3", "ffn_w2", "cd_w_in", "cd_w_out", "router_w", "moe_w1", "moe_w3", "moe_w2"]
_CACHE = {}


def run(inputs, S, layers, ncores):
    key = (S, tuple(layers))
    if key not in _CACHE:
        _CACHE[key] = build(S, layers)
    nc = _CACHE[key]
    rows, pes, wst = host_layout(inputs)
    cst = host_consts()
    shared = {"cst": cst, "rows": rows, "pe0": pes[0], "pe1": pes[1], "wst": wst}
    for k in nc._wkeys:
        shared[k] = np.ascontiguousarray(np.asarray(inputs[k], dtype=np.float32))
    x = np.asarray(inputs["x"], dtype=np.float32)
    in_maps = []
    for c in range(ncores):
        m = dict(shared)
        m["x"] = np.ascontiguousarray(x[c, :S, :])
        in_maps.append(m)
    res = run_bass_kernel_spmd(nc, in_maps, core_ids=list(range(ncores)))
    return np.stack([res.results[c]["out"] for c in range(ncores)], axis=0)


def kernel(**inputs):
    return run(inputs, 4096, [0, 1, 2, 3], 8)


def phase_dump(nc, cx):
    ph = Phase(nc, "dump")
    mk_deps(ph, cx)
    t = ph.sb("t", [128, D])
    for n in range(cx.NT):
        ph.dma("sp", t[:, :], cx.acc[n * 128:(n + 1) * 128, :], [], [t], t)
        ph.dma("sp", cx.out[n * 128:(n + 1) * 128, :], t[:, :], [t], [cx.out_dep[n]], t)
    ph.finish()
```

```python
import math
from contextlib import ExitStack
import numpy as np
import concourse.bass as bass
import concourse.mybir as mybir
from concourse.bass_utils import run_bass_kernel_spmd

F32 = mybir.dt.float32
BF16 = mybir.dt.bfloat16
AF = mybir.ActivationFunctionType
ALU = mybir.AluOpType
AX = mybir.AxisListType

D = 1024
DFF = 2816
NFF = 22
NE = 8
DEPTH = 4
ALPHA = (2 * DEPTH) ** 0.25
EPS = 1e-5
NEG = -1.0e30
FGROUPS = [(0, 4), (4, 4), (8, 4), (12, 4), (16, 4), (20, 2)]

ENGS = ["pe", "act", "dve", "pool", "sp"]


class Dep:
    __slots__ = ("name", "lw", "rd", "sem", "dcount", "lastdma", "psum")

    def __init__(self, name):
        self.name = name
        self.psum = False
        self.lw = None
        self.rd = []
        self.sem = None
        self.dcount = 0
        self.lastdma = None


class Op:
    __slots__ = ("id", "eng", "fn", "deps", "dma", "signal", "sigval", "sem", "inc")

    def __init__(self, id, eng, fn, dma, inc):
        self.id = id
        self.eng = eng
        self.fn = fn
        self.deps = set()
        self.dma = dma
        self.signal = False
        self.sigval = 0
        self.sem = None
        self.inc = inc


class Buf:
    __slots__ = ("t", "d")

    def __init__(self, t, d):
        self.t = t
        self.d = d

    def __getitem__(self, k):
        return self.t[k]


SEMPOOL = {}
NDMASEM = 56


def make_sempool(nc, stack):
    pool = {"eng": {}, "dma": []}
    for e in ["pe", "act", "dve", "pool"]:
        pool["eng"][e] = [stack.enter_context(nc.semaphore("s_" + e)), 0]
    for i in range(NDMASEM):
        pool["dma"].append([stack.enter_context(nc.semaphore("d%d" % i)), 0])
    SEMPOOL[id(nc)] = pool


class Phase:
    def __init__(self, nc, name):
        self.nc = nc
        self.name = name
        self.ops = []
        self.by_eng = {e: [] for e in ENGS}
        self.dma_deps = []
        self.all_deps = []
        self.st = ExitStack()
        self.nsb = 0

    def dep(self, name):
        d = Dep(name)
        self.all_deps.append(d)
        return d

    def sb(self, name, shape, dt=F32):
        t = self.st.enter_context(self.nc.sbuf_tensor(self.name + "_" + name, list(shape), dt))
        return Buf(t, self.dep(name))

    def ps(self, name, shape, dt=F32):
        t = self.st.enter_context(self.nc.psum_tensor(self.name + "_" + name, list(shape), dt))
        b = Buf(t, self.dep(name))
        b.d.psum = True
        return b

    def region(self, name):
        return Buf(None, self.dep(name))

    def op(self, eng, fn, R=(), W=(), dma=None, inc=16):
        o = Op(len(self.ops), eng, fn, dma, inc)
        self.ops.append(o)
        self.by_eng[eng].append(o)
        deps = o.deps
        if any(b.d.psum for b in R):
            W = list(W) + [b for b in R if b.d.psum]
            R = [b for b in R if not b.d.psum]
        for b in R:
            t = b.d
            if t.lw is not None:
                deps.add(t.lw)
        for b in W:
            t = b.d
            if t.lw is not None:
                deps.add(t.lw)
            deps.update(t.rd)
        if dma is not None:
            dd = dma.d
            if dd.lastdma is not None:
                deps.add(dd.lastdma)
            if fn is not None:
                dd.lastdma = o.id
            if dd.sem is None:
                dd.sem = True
                self.dma_deps.append(dd)
        deps.discard(o.id)
        best = {}
        for d in deps:
            od = self.ops[d]
            if od.dma is None and od.fn is not None:
                if d > best.get(od.eng, -1):
                    best[od.eng] = d
        for d in list(deps):
            od = self.ops[d]
            if od.dma is None and od.fn is not None and best[od.eng] != d:
                deps.discard(d)
        if eng == "pe":
            for d in list(deps):
                od = self.ops[d]
                if od.eng == "pe" and od.dma is None:
                    deps.discard(d)
        for d in deps:
            self.ops[d].signal = True
        if fn is not None:
            for b in R:
                b.d.rd.append(o.id)
            for b in W:
                b.d.lw = o.id
                b.d.rd = []
        return o

    def pe(self, fn, R=(), W=()):
        return self.op("pe", fn, R, W)

    def act(self, fn, R=(), W=()):
        return self.op("act", fn, R, W)

    def dve(self, fn, R=(), W=()):
        return self.op("dve", fn, R, W)

    def pool(self, fn, R=(), W=()):
        return self.op("pool", fn, R, W)

    def dma(self, q, out_ap, in_ap, R, W, semb):
        return self.op(q, lambda e: e.dma_start(out=out_ap, in_=in_ap), R, W, dma=semb)

    def mm(self, out_ap, lhsT, rhs, start, stop, R, W):
        return self.op("pe", lambda e: e.matmul(out_ap, lhsT=lhsT, rhs=rhs, start=start, stop=stop), R, W)

    def tr(self, out_ap, in_ap, ident_ap, R, W):
        return self.op("pe", lambda e: e.transpose(out=out_ap, in_=in_ap, identity=ident_ap), R, W)

    def barrier(self):
        allb = [Buf(None, d) for d in self.all_deps]
        for e in ENGS:
            self.op(e, None, R=(), W=allb)

    def finish(self):
        nc = self.nc
        st = self.st
        allb = [Buf(None, d) for d in self.all_deps]
        for e in ENGS:
            self.op(e, None, R=(), W=allb)
        pool = SEMPOOL[id(nc)]
        engsem = {}
        cnt = {e: 0 for e in ENGS}
        for e in ["pe", "act", "dve", "pool"]:
            engsem[e] = pool["eng"][e][0]
            cnt[e] = pool["eng"][e][1]
        assert len(self.dma_deps) <= len(pool["dma"]), len(self.dma_deps)
        for i, d in enumerate(self.dma_deps):
            d.sem = pool["dma"][i][0]
            d.dcount = pool["dma"][i][1]
        for o in self.ops:
            if o.fn is None:
                continue
            if o.dma is not None:
                o.dma.d.dcount += o.inc
                o.sigval = o.dma.d.dcount
                o.sem = o.dma.d.sem
            elif o.signal:
                cnt[o.eng] += 1
                o.sigval = cnt[o.eng]
                o.sem = engsem[o.eng]
        ops = self.ops

        def run(eng_name):
            def body(e):
                seen = {}
                for o in self.by_eng[eng_name]:
                    need = {}
                    for d in o.deps:
                        od = ops[d]
                        if od.fn is None:
                            continue
                        key = id(od.sem)
                        if od.sigval > need.get(key, (0, None))[0]:
                            need[key] = (od.sigval, od.sem)
                    for key, (val, sem) in need.items():
                        if seen.get(key, 0) >= val:
                            continue
                        e.wait_ge(sem, val)
                        seen[key] = val
                    if o.fn is None:
                        continue
                    ins = o.fn(e)
                    if o.dma is not None:
                        ins.then_inc(o.sem, o.inc)
                    elif o.signal:
                        ins.then_inc(o.sem, 1)
            return body

        for e in ["pe", "act", "dve", "pool"]:
            pool["eng"][e][1] = cnt[e]
        for i, d in enumerate(self.dma_deps):
            pool["dma"][i][1] = d.dcount
        block = st.enter_context(nc.Block())
        block.tensor(run("pe"))
        block.scalar(run("act"))
        block.vector(run("dve"))
        block.gpsimd(run("pool"))
        block.sync(run("sp"))
        st.close()


class Ctx:
    pass


def load_rows(ph, q, buf, dram_row_ap, n):
    ph.dma(q, buf[:, 0:n], dram_row_ap.partition_broadcast(128), [], [buf], buf)


def ln_rows(ph, s, mv, stats, rstd, nb, width, eps=EPS):
    nchunk = (width + 511) // 512
    for h in range(nchunk):
        lo, hi = h * 512, min(width, (h + 1) * 512)
        ph.dve(lambda e, h=h, lo=lo, hi=hi: e.bn_stats(out=stats[:, h, :], in_=s[:, lo:hi]), [s], [stats])
    ph.dve(lambda e: e.bn_aggr(out=mv[:, :], in_=stats[:, 0:nchunk, :].rearrange("p a b -> p (a b)")), [stats], [mv])
    ph.dve(lambda e: e.tensor_scalar(out=rstd[:, :], in0=mv[:, 1:2], scalar1=eps, scalar2=None, op0=ALU.add), [mv], [rstd])
    ph.act(lambda e: e.activation(out=rstd[:, :], in_=rstd[:, :], func=AF.Sqrt), [rstd], [rstd])
    ph.dve(lambda e: e.reciprocal(out=rstd[:, :], in_=rstd[:, :]), [rstd], [rstd])
    ph.dve(lambda e: e.scalar_tensor_tensor(out=nb[:, :], in0=mv[:, 0:1], scalar=-1.0, in1=rstd[:, :], op0=ALU.mult, op1=ALU.mult),
           [mv, rstd], [nb])


class Epi:
    def __init__(self, ph, cx, g_row, b_row, last, router=None, nbuf=2):
        self.ph = ph
        self.cx = cx
        self.last = last
        self.router = router
        self.grow = ph.sb("e_g", [128, D]); load_rows(ph, "sp", self.grow, g_row, D)
        self.brow = ph.sb("e_b", [128, D]); load_rows(ph, "sp", self.brow, b_row, D)
        self.nbuf = nbuf
        self.s = [ph.sb("e_s%d" % i, [128, D]) for i in range(nbuf)]
        self.y = [ph.sb("e_y%d" % i, [128, D]) for i in range(nbuf)]
        self.yb = [ph.sb("e_yb%d" % i, [128, D], BF16) for i in range(nbuf)]
        self.ya = self.s
        self.stats = ph.sb("e_st", [128, 2, 6]); self.mv = ph.sb("e_mv", [128, 2])
        self.rstd = ph.sb("e_rstd", [128, 1]); self.nb = ph.sb("e_nb", [128, 1])
        self.xTs = [ph.sb("e_xT%d" % i, [128, 8, 512], BF16) for i in range(nbuf)]
        self.k = 0
        if router is not None:
            self.rw = ph.sb("r_w", [128, 8, NE]); ph.dma("sp", self.rw[:, :, :], router["w"].rearrange("(c p) e -> p c e", p=128), [], [self.rw], self.rw)
            self.rb = ph.sb("r_b", [128, NE]); load_rows(ph, "sp", self.rb, router["b"], NE)
            self.yT = ph.sb("r_yT", [128, 8, 128])
            self.lg = ph.sb("r_lg", [128, NE]); self.top = ph.sb("r_top", [128, 8])
            self.g1 = ph.sb("r_g1", [128, 1]); self.g2 = ph.sb("r_g2", [128, 1]); self.dd = ph.sb("r_dd", [128, 1])
            self.c1 = ph.sb("r_c1", [128, NE]); self.c2 = ph.sb("r_c2", [128, NE])

    def run(self, n, res_ap_fn, res_R, idb, idf, trp, trf, rlp):
        ph, cx = self.ph, self.cx
        k = self.k; self.k += 1
        nbuf = self.nbuf
        s = self.s[k % nbuf]; y = self.y[k % nbuf]; yb = self.yb[k % nbuf]; ya = self.ya[k % nbuf]
        xTs = self.xTs[(n // 4) % nbuf]
        accr = cx.acc_dep[n]
        ph.dma("sp", s[:, :], cx.acc[n * 128:(n + 1) * 128, :], [accr], [s], s)
        if res_ap_fn is not None:
            for h in range(2):
                ph.dve(lambda e, h=h: e.tensor_tensor(out=s[:, h * 512:(h + 1) * 512], in0=res_ap_fn(h * 512, (h + 1) * 512),
                                                      in1=s[:, h * 512:(h + 1) * 512], op=ALU.add), [s] + list(res_R), [s])
        ln_rows(ph, s, self.mv, self.stats, self.rstd, self.nb, D)
        ph.act(lambda e: e.activation(out=y[:, :], in_=s[:, :], func=AF.Identity, bias=self.nb[:, :], scale=self.rstd[:, :]),
               [s, self.nb, self.rstd], [y])
        ph.pool(lambda e: e.tensor_tensor(out=y[:, :], in0=y[:, :], in1=self.grow[:, :], op=ALU.mult), [y, self.grow], [y])
        ph.pool(lambda e: e.tensor_tensor(out=y[:, :], in0=y[:, :], in1=self.brow[:, :], op=ALU.add), [y, self.brow], [y])
        if self.last:
            ph.dma("sp", cx.out[n * 128:(n + 1) * 128, :], y[:, :], [y], [cx.out_dep[n]], y)
            return
        ph.act(lambda e: e.activation(out=ya[:, :], in_=y[:, :], func=AF.Copy, scale=ALPHA), [y], [ya])
        ph.dma("sp", cx.acc[n * 128:(n + 1) * 128, :], ya[:, :], [ya], [accr], ya)
        ph.act(lambda e: e.activation(out=yb[:, :], in_=y[:, :], func=AF.Copy), [y], [yb])
        for c in range(8):
            ph.tr(trp[:, c * 128:(c + 1) * 128], yb[:, c * 128:(c + 1) * 128], idb[:, :], [yb, idb], [trp])
        sub = n % 4
        ph.dve(lambda e: e.tensor_copy(out=xTs[:, :, sub * 128:(sub + 1) * 128], in_=trp[:, :].rearrange("p (c t) -> p c t", c=8)),
               [trp], [xTs])
        if sub == 3:
            t0 = (n // 4) * 512
            ph.dma("sp", cx.xT.rearrange("c p s -> p c s")[:, :, t0:t0 + 512], xTs[:, :, :], [xTs], [cx.xT_dep[n // 4]], xTs)
        if self.router is not None:
            for half in range(2):
                tf = trf[half]
                for c in range(4):
                    cc = half * 4 + c
                    ph.tr(tf[:, c * 128:(c + 1) * 128], y[:, cc * 128:(cc + 1) * 128], idf[:, :], [y, idf], [tf])
                ph.act(lambda e, half=half, tf=tf: e.activation(out=self.yT[:, half * 4:(half + 1) * 4, :],
                                                                in_=tf[:, :].rearrange("p (c t) -> p c t", c=4), func=AF.Copy),
                       [tf], [self.yT])
            for c in range(8):
                ph.mm(rlp[:, 0:NE], self.yT[:, c, :], self.rw[:, c, :], c == 0, c == 7, [self.yT, self.rw], [rlp])
            ph.dve(lambda e: e.tensor_tensor(out=self.lg[:, :], in0=rlp[:, 0:NE], in1=self.rb[:, :], op=ALU.add), [rlp, self.rb], [self.lg])
            ph.dve(lambda e: e.max(out=self.top[:, :], in_=self.lg[:, :]), [self.lg], [self.top])
            ph.dve(lambda e: e.tensor_tensor(out=self.dd[:, :], in0=self.top[:, 0:1], in1=self.top[:, 1:2], op=ALU.subtract), [self.top], [self.dd])
            ph.act(lambda e: e.activation(out=self.g1[:, :], in_=self.dd[:, :], func=AF.Sigmoid), [self.dd], [self.g1])
            ph.dve(lambda e: e.tensor_scalar(out=self.g2[:, :], in0=self.g1[:, :], scalar1=-1.0, scalar2=1.0, op0=ALU.mult, op1=ALU.add),
                   [self.g1], [self.g2])
            ph.dve(lambda e: e.tensor_scalar(out=self.c1[:, :], in0=self.lg[:, :], scalar1=self.top[:, 0:1], scalar2=self.g1[:, :],
                                             op0=ALU.is_equal, op1=ALU.mult), [self.lg, self.top, self.g1], [self.c1])
            ph.dve(lambda e: e.tensor_scalar(out=self.c2[:, :], in0=self.lg[:, :], scalar1=self.top[:, 1:2], scalar2=self.g2[:, :],
                                             op0=ALU.is_equal, op1=ALU.mult), [self.lg, self.top, self.g2], [self.c2])
            ph.dve(lambda e: e.tensor_tensor(out=self.c1[:, :], in0=self.c1[:, :], in1=self.c2[:, :], op=ALU.add), [self.c1, self.c2], [self.c1])
            ph.dma("sp", cx.comb[n * 128:(n + 1) * 128, :], self.c1[:, :], [self.c1], [cx.comb_dep], self.c1)


def load_consts(ph, cx, ncols=None):
    ncols = ncols or cx.NCST
    c = ph.sb("cst", [128, ncols])
    ph.dma("sp", c[:, :], cx.cst[:, 0:ncols], [], [c], c)
    idb = ph.sb("idb", [128, 128], BF16)
    ph.dve(lambda e: e.tensor_copy(out=idb[:, :], in_=c[:, 0:128]), [c], [idb])
    return c, idb


def phase_prologue(nc, cx):
    ph = Phase(nc, "p0")
    mk_deps(ph, cx)
    c, idb = load_consts(ph, cx, 128)
    trp = ph.ps("trp", [128, 1024], BF16)
    xs = [ph.sb("x%d" % i, [128, D]) for i in range(2)]
    xa = [ph.sb("xa%d" % i, [128, D]) for i in range(2)]
    xb = [ph.sb("xb%d" % i, [128, D], BF16) for i in range(2)]
    xTs = [ph.sb("xT%d" % i, [128, 8, 512], BF16) for i in range(2)]
    for n in range(cx.NT):
        x = xs[n % 2]; a = xa[n % 2]; b = xb[n % 2]; xt = xTs[(n // 4) % 2]
        ph.dma("sp", x[:, :], cx.x[n * 128:(n + 1) * 128, :], [], [x], x)
        ph.act(lambda e, x=x, a=a: e.activation(out=a[:, :], in_=x[:, :], func=AF.Copy, scale=ALPHA), [x], [a])
        ph.dma("sp", cx.acc[n * 128:(n + 1) * 128, :], a[:, :], [a], [cx.acc_dep[n]], a)
        ph.dve(lambda e, x=x, b=b: e.tensor_copy(out=b[:, :], in_=x[:, :]), [x], [b])
        for cc in range(8):
            ph.tr(trp[:, cc * 128:(cc + 1) * 128], b[:, cc * 128:(cc + 1) * 128], idb[:, :], [b, idb], [trp])
        sub = n % 4
        ph.dve(lambda e, xt=xt, sub=sub: e.tensor_copy(out=xt[:, :, sub * 128:(sub + 1) * 128],
                                                        in_=trp[:, :].rearrange("p (c t) -> p c t", c=8)), [trp], [xt])
        if sub == 3:
            t0 = (n // 4) * 512
            ph.dma("sp", cx.xT.rearrange("c p s -> p c s")[:, :, t0:t0 + 512], xt[:, :, :], [xt], [cx.xT_dep[n // 4]], xt)
    ph.finish()


def phase_ffn(nc, cx, name, w1s, w3s, w2s, ln_g, ln_b, last, moe):
    ph = Phase(nc, name)
    mk_deps(ph, cx)
    c, idb = load_consts(ph, cx, 128)
    idf = Buf(c.t[:, 0:128], c.d)
    NEXP = len(w1s)
    w1g = [ph.sb("w1g%d" % g, [128, 8, n * 128], BF16) for g, (s0, n) in enumerate(FGROUPS)]
    w3g = [ph.sb("w3g%d" % g, [128, 8, n * 128], BF16) for g, (s0, n) in enumerate(FGROUPS)]
    w2g = [ph.sb("w2g%d" % g, [128, n, D], BF16) for g, (s0, n) in enumerate(FGROUPS)]

    import os
    noroll = bool(os.environ.get("NOROLL"))

    def load_w(e):
        xr = [acct[0]] if (noroll and e > 0) else []
        for g, (s0, n) in enumerate(FGROUPS):
            ph.dma("pool", w1g[g][:, :, :], w1s[e][:, s0 * 128:(s0 + n) * 128].rearrange("(c p) n -> p c n", p=128), xr, [w1g[g]], w1g[g])
            ph.dma("pool", w3g[g][:, :, :], w3s[e][:, s0 * 128:(s0 + n) * 128].rearrange("(c p) n -> p c n", p=128), xr, [w3g[g]], w3g[g])
        for g, (s0, n) in enumerate(FGROUPS):
            ph.dma("pool", w2g[g][:, :, :], w2s[e][s0 * 128:(s0 + n) * 128, :].rearrange("(c p) n -> p c n", p=128), xr, [w2g[g]], w2g[g])

    epi = Epi(ph, cx, ln_g, ln_b, last, nbuf=1)
    xTt = [ph.sb("xTt%d" % i, [128, 8, 512], BF16) for i in range(2)]
    gT = ph.sb("gT", [128, NFF, 512], BF16)
    gdeps = [Buf(gT.t, ph.dep("gT%d" % f)) for f in range(NFF)]
    sg = [ph.sb("sg%d" % i, [128, 512]) for i in range(2)]
    acct = [ph.sb("acct%d" % i, [128, D]) for i in range(1)]
    comb = None
    if moe:
        comb = ph.sb("comb", [128, cx.NT, NE])
        ph.dma("sp", comb[:, :, :], cx.comb.rearrange("(n p) e -> p n e", p=128), [cx.comb_dep], [comb], comb)
    hp = [ph.ps("hp%d" % i, [128, 512]) for i in range(4)]
    op = [ph.ps("op%d" % i, [128, 512]) for i in range(2)]
    trp = ph.ps("trp", [128, 1024], BF16)
    k = 0
    for e in range(NEXP):
        load_w(e)
        for tt in range(cx.NTT):
            xt = xTt[k % 2]
            ph.dma("sp", xt[:, :, :], cx.xT.rearrange("c p s -> p c s")[:, :, tt * 512:(tt + 1) * 512], [cx.xT_dep[tt]], [xt], xt)
            for f in range(NFF):
                g = min(f // 4, 5); fo = (f - FGROUPS[g][0]) * 128
                h1 = hp[(2 * f) % 4]; h3 = hp[(2 * f + 1) % 4]
                for kc in range(8):
                    ph.mm(h1[:, :], w1g[g][:, kc, fo:fo + 128], xt[:, kc, :], kc == 0, kc == 7, [w1g[g], xt], [h1])
                for kc in range(8):
                    ph.mm(h3[:, :], w3g[g][:, kc, fo:fo + 128], xt[:, kc, :], kc == 0, kc == 7, [w3g[g], xt], [h3])
                s = sg[f % 2]
                ph.act(lambda e_, h1=h1, s=s: e_.activation(out=s[:, :], in_=h1[:, :], func=AF.Silu), [h1], [s])
                ph.dve(lambda e_, h3=h3, s=s, f=f: e_.tensor_tensor(out=gT[:, f, :], in0=h3[:, :], in1=s[:, :], op=ALU.mult), [h3, s], [gdeps[f]])
            for sub in range(4):
                n = tt * 4 + sub
                for half in range(2):
                    o = op[half]
                    for f in range(NFF):
                        g = min(f // 4, 5); fi = f - FGROUPS[g][0]
                        ph.mm(o[:, :], gT[:, f, sub * 128:(sub + 1) * 128], w2g[g][:, fi, half * 512:(half + 1) * 512], f == 0, f == NFF - 1,
                              [gdeps[f], w2g[g]], [o])
                if e == NEXP - 1 and not moe:
                    epi.run(n, lambda lo, hi: op[lo // 512][:, :], [op[0], op[1]], idb, idf, trp, None, None)
                else:
                    a = acct[0]
                    ph.dma("sp", a[:, :], cx.acc[n * 128:(n + 1) * 128, :], [cx.acc_dep[n]], [a], a)
                    for half in range(2):
                        if moe:
                            ph.dve(lambda e_, a=a, half=half, n=n, e=e: e_.scalar_tensor_tensor(
                                out=a[:, half * 512:(half + 1) * 512], in0=op[half][:, :], scalar=comb[:, n, e:e + 1],
                                in1=a[:, half * 512:(half + 1) * 512], op0=ALU.mult, op1=ALU.add), [op[half], a, comb], [a])
                        else:
                            ph.dve(lambda e_, a=a, half=half: e_.tensor_tensor(out=a[:, half * 512:(half + 1) * 512], in0=op[half][:, :],
                                                                            in1=a[:, half * 512:(half + 1) * 512], op=ALU.add), [op[half], a], [a])
                    if e == NEXP - 1:
                        ph.dma("sp", cx.acc[n * 128:(n + 1) * 128, :], a[:, :], [a], [cx.acc_dep[n]], a)
                        epi.run(n, None, [], idb, idf, trp, None, None)
                    else:
                        ph.dma("sp", cx.acc[n * 128:(n + 1) * 128, :], a[:, :], [a], [cx.acc_dep[n]], a)
            k += 1
    ph.finish()


CST_ID, CST_ONES, CST_MTNEG, CST_M01S, CST_MT01, CST_SEL, CST_SC01, CST_SCNEG, NCST = 0, 128, 256, 384, 512, 640, 1152, 1664, 2176
PE_CW, PE_CB, PE_AG, PE_AB, PE_IB, PE_FB, NPE = 0, 124, 128, 132, 136, 137, 138


def phase_even_mixer(nc, cx, name, j):
    ph = Phase(nc, name)
    mk_deps(ph, cx)
    c, idb = load_consts(ph, cx)
    cd = [c]
    idf = Buf(c.t[:, 0:128], c.d)
    ones = c.t[:, CST_ONES:CST_ONES + 128]
    win = ph.sb("win", [128, 8, 3080], BF16)
    ph.dma("pool", win[:, :, :], cx.ab_w_in[j].rearrange("(c p) n -> p c n", p=128), [], [win], win)
    wout = ph.sb("wout", [128, 8, D], BF16)
    ph.dma("pool", wout[:, :, :], cx.ab_w_out[j].rearrange("(c p) n -> p c n", p=128), [], [wout], wout)
    pp = ph.sb("pp", [128, NPE])
    ph.dma("sp", pp[:, :], cx.pe[j], [], [pp], pp)
    nfb = ph.sb("nfb", [128, 1])
    ph.dve(lambda e: e.tensor_scalar(out=nfb[:, :], in0=pp[:, PE_FB:PE_FB + 1], scalar1=-1.0, scalar2=None, op0=ALU.mult), [pp], [nfb])
    hg = ph.sb("hg", [128, 512]); load_rows(ph, "sp", hg, cx.rows[cx.row_idx[("hg", j)]:cx.row_idx[("hg", j)] + 1, 0:512], 512)
    epi = Epi(ph, cx, cx.rows[cx.row_idx[("eln1g", j)]:cx.row_idx[("eln1g", j)] + 1, :], cx.rows[cx.row_idx[("eln1b", j)]:cx.row_idx[("eln1b", j)] + 1, :], False, nbuf=1)
    pj = [ph.ps("pj%d" % i, [128, 512]) for i in range(2)]
    pm = ph.ps("pm", [128, 512]); pm_st = pm; pm_ub = pm; pm_cols = pm; pm_sc = pm
    pn = ph.ps("pn", [128, 512]); pn_num = pn; pn_int = pn; pn_kv = pn
    pgi = ph.ps("pgi", [128, 512]); pgf = ph.ps("pgf", [128, 512])
    trp = ph.ps("trp", [128, 1024], BF16)
    xTt = [ph.sb("xTt%d" % i, [128, 8, 512], BF16) for i in range(1)]
    glu = ph.sb("glu", [128, 4, 542])
    ph.dve(lambda e: e.memset(glu[:, :, 0:30], 0.0), [], [glu])
    sig = ph.sb("sig", [128, 512])
    cy = ph.sb("cy", [128, 4, 512]); sq = ph.sb("sq", [128, 512])
    mean = ph.sb("mean", [128, 512]); var = ph.sb("var", [128, 512]); tmpa = ph.sb("tmpa", [128, 512])
    mixT = ph.sb("mixT", [128, 8, 512], BF16)
    qT = ph.sb("qT", [128, 4, 512], BF16); kT = ph.sb("kT", [128, 4, 512], BF16)
    ktok = ph.sb("ktok", [128, 4, 512], BF16)
    vext = ph.sb("vext", [128, 4, 4, 132], BF16)
    ph.dve(lambda e: e.memset(vext[:, :, :, 128:129], 1.0), [], [vext])
    osig = ph.sb("osig", [128, 4, 512])
    ig = ph.sb("ig", [128, 512]); sp_ = ph.sb("sp", [128, 512]); Bc = ph.sb("Bc", [128, 512]); lw = ph.sb("lw", [128, 512])
    Mx = ph.sb("Mx", [128, 512]); U = ph.sb("U", [128, 512]); rowsT = ph.sb("rowsT", [128, 512]); tmpr = ph.sb("tmpr", [128, 512])
    ph.dve(lambda e: e.memset(rowsT[:, :], 0.0), [], [rowsT])
    ph.dve(lambda e: e.memset(U[:, :], 0.0), [], [U])
    am = ph.sb("am", [128, 4]); nam = ph.sb("nam", [128, 4]); mp = ph.sb("mp", [128, 5]); sv = ph.sb("sv", [128, 4, 2]); t1 = ph.sb("t1", [128, 1]); t2 = ph.sb("t2", [128, 1])
    ph.dve(lambda e: e.memset(sv[:, :, :], 0.0), [], [sv])
    mcar = ph.sb("mcar", [128, 1])
    ph.dve(lambda e: e.memset(mcar[:, :], 0.0), [], [mcar])
    cols = ph.sb("cols", [128, 4, 128]); scol = ph.sb("scol", [128, 4, 128])
    vt = ph.sb("vt", [128, 512]); rows2 = ph.sb("rows2", [128, 512]); ax = ph.sb("ax", [128, 4]); nax = ph.sb("nax", [128, 4])
    vsc = ph.sb("vsc", [128, 132], BF16)
    ph.dve(lambda e: e.memset(rows2[:, :], 0.0), [], [rows2])
    wgi = ph.sb("wgi", [128, 8, 128], BF16); wgf = ph.sb("wgf", [128, 8, 128], BF16)
    ph.dve(lambda e: e.memset(wgi[:, :, :], 0.0), [], [wgi])
    ph.dve(lambda e: e.memset(wgf[:, :, :], 0.0), [], [wgf])
    for g in range(4):
        ph.dve(lambda e, g=g: e.tensor_copy(out=wgi[:, :, 32 * g:32 * g + 4], in_=win[:, :, 3072:3076]), [win], [wgi])
        ph.dve(lambda e, g=g: e.tensor_copy(out=wgf[:, :, 32 * g:32 * g + 4], in_=win[:, :, 3076:3080]), [win], [wgf])
    Cf = ph.sb("Cf", [128, 4, 132]); Cb = ph.sb("Cb", [128, 4, 132], BF16)
    ph.dve(lambda e: e.memset(Cf[:, :, :], 0.0), [], [Cf])
    ph.dve(lambda e: e.memset(Cb[:, :, :], 0.0), [], [Cb])
    PT = ph.sb("PT", [128, 128], BF16)
    ti = ph.sb("ti", [128, 129]); tot = ph.sb("tot", [128, 129]); dd = ph.sb("dd", [128, 1]); rec = ph.sb("rec", [128, 1])
    kw = ph.sb("kw", [128, 128], BF16)
    hraw = ph.sb("hraw", [128, 512]); hst = ph.sb("hst", [128, 4, 6]); hmv = ph.sb("hmv", [128, 4, 2]); hrs = ph.sb("hrs", [128, 4]); hb = ph.sb("hb", [128, 512], BF16)
    sc01 = c.t[:, CST_SC01:CST_SC01 + 512]; scneg = c.t[:, CST_SCNEG:CST_SCNEG + 512]
    k128 = 128 ** -0.5

    def proj_fm(col0, dst_fn, pjk):
        p = pj[pjk % 2]
        for kc in range(8):
            ph.mm(p[:, :], win[:, kc, col0:col0 + 128], xt[:, kc, :], kc == 0, kc == 7, [win, xt], [p])
        dst_fn(p)

    kpj = [0]
    import os
    emstop = int(os.environ.get("EMSTOP", "99"))
    for tt in range(cx.NTT if emstop > 0 else 0):
        xt = xTt[0]
        ph.dma("sp", xt[:, :, :], cx.xT.rearrange("c p s -> p c s")[:, :, tt * 512:(tt + 1) * 512], [cx.xT_dep[tt]], [xt], xt)
        for ch in range(4):
            p = pj[kpj[0] % 2]; kpj[0] += 1
            for kc in range(8):
                ph.mm(p[:, :], win[:, kc, 512 + ch * 128:512 + (ch + 1) * 128], xt[:, kc, :], kc == 0, kc == 7, [win, xt], [p])
            ph.act(lambda e, p=p: e.activation(out=sig[:, :], in_=p[:, :], func=AF.Sigmoid), [p], [sig])
            p2 = pj[kpj[0] % 2]; kpj[0] += 1
            for kc in range(8):
                ph.mm(p2[:, :], win[:, kc, ch * 128:(ch + 1) * 128], xt[:, kc, :], kc == 0, kc == 7, [win, xt], [p2])
            ph.dve(lambda e, p2=p2, ch=ch: e.tensor_tensor(out=glu[:, ch, 30:542], in0=p2[:, :], in1=sig[:, :], op=ALU.mult), [p2, sig], [glu])
            ph.dve(lambda e, ch=ch: e.tensor_scalar(out=cy[:, ch, :], in0=glu[:, ch, 0:512], scalar1=pp[:, PE_CW + ch * 31:PE_CW + ch * 31 + 1],
                                                    scalar2=pp[:, PE_CB + ch:PE_CB + ch + 1], op0=ALU.mult, op1=ALU.add), [glu, pp], [cy])
            for jj in range(1, 31):
                ph.dve(lambda e, ch=ch, jj=jj: e.scalar_tensor_tensor(out=cy[:, ch, :], in0=glu[:, ch, jj:jj + 512],
                                                                     scalar=pp[:, PE_CW + ch * 31 + jj:PE_CW + ch * 31 + jj + 1],
                                                                     in1=cy[:, ch, :], op0=ALU.mult, op1=ALU.add), [glu, pp, cy], [cy])
            ph.pool(lambda e, ch=ch: e.tensor_copy(out=glu[:, ch, 0:30], in_=glu[:, ch, 512:542]), [glu, cy], [glu])
        if emstop <= 1:
            continue
        for ch in range(4):
            ph.mm(pgi[:, :], ones, cy[:, ch, :], ch == 0, ch == 3, [cy] + cd, [pgi])
        for ch in range(4):
            ph.act(lambda e, ch=ch: e.activation(out=sq[:, :], in_=cy[:, ch, :], func=AF.Square), [cy], [sq])
            ph.mm(pgf[:, :], ones, sq[:, :], ch == 0, ch == 3, [sq] + cd, [pgf])
        ph.act(lambda e: e.activation(out=mean[:, :], in_=pgi[:, :], func=AF.Copy, scale=1.0 / 512), [pgi], [mean])
        ph.dve(lambda e: e.tensor_tensor(out=tmpa[:, :], in0=mean[:, :], in1=mean[:, :], op=ALU.mult), [mean], [tmpa])
        ph.dve(lambda e: e.scalar_tensor_tensor(out=var[:, :], in0=pgf[:, :], scalar=1.0 / 512, in1=tmpa[:, :], op0=ALU.mult, op1=ALU.subtract),
               [pgf, tmpa], [var])
        ph.dve(lambda e: e.tensor_scalar(out=var[:, :], in0=var[:, :], scalar1=EPS, scalar2=None, op0=ALU.add), [var], [var])
        ph.act(lambda e: e.activation(out=var[:, :], in_=var[:, :], func=AF.Sqrt), [var], [var])
        ph.dve(lambda e: e.reciprocal(out=var[:, :], in_=var[:, :]), [var], [var])
        for ch in range(4):
            ph.dve(lambda e, ch=ch: e.tensor_tensor(out=cy[:, ch, :], in0=cy[:, ch, :], in1=mean[:, :], op=ALU.subtract), [cy, mean], [cy])
            ph.dve(lambda e, ch=ch: e.tensor_tensor(out=cy[:, ch, :], in0=cy[:, ch, :], in1=var[:, :], op=ALU.mult), [cy, var], [cy])
            ph.act(lambda e, ch=ch: e.activation(out=mixT[:, ch, :], in_=cy[:, ch, :], func=AF.Silu, bias=pp[:, PE_AB + ch:PE_AB + ch + 1],
                                                 scale=pp[:, PE_AG + ch:PE_AG + ch + 1]), [cy, pp], [mixT])
        if emstop <= 2:
            continue
        for h in range(4):
            p = pj[kpj[0] % 2]; kpj[0] += 1
            for kc in range(8):
                ph.mm(p[:, :], win[:, kc, 1024 + h * 128:1024 + (h + 1) * 128], xt[:, kc, :], kc == 0, kc == 7, [win, xt], [p])
            ph.act(lambda e, p=p, h=h: e.activation(out=qT[:, h, :], in_=p[:, :], func=AF.Copy), [p], [qT])
            p = pj[kpj[0] % 2]; kpj[0] += 1
            for kc in range(8):
                ph.mm(p[:, :], win[:, kc, 1536 + h * 128:1536 + (h + 1) * 128], xt[:, kc, :], kc == 0, kc == 7, [win, xt], [p])
            ph.act(lambda e, p=p, h=h: e.activation(out=kT[:, h, :], in_=p[:, :], func=AF.Copy, scale=k128), [p], [kT])
        for sub in range(4):
            p = pj[kpj[0] % 2]; kpj[0] += 1
            for kc in range(8):
                ph.mm(p[:, :], xt[:, kc, sub * 128:(sub + 1) * 128], win[:, kc, 1536:2048], kc == 0, kc == 7, [win, xt], [p])
            ph.act(lambda e, p=p, sub=sub: e.activation(out=ktok[:, sub, :], in_=p[:, :], func=AF.Copy, scale=k128), [p], [ktok])
            p = pj[kpj[0] % 2]; kpj[0] += 1
            for kc in range(8):
                ph.mm(p[:, :], xt[:, kc, sub * 128:(sub + 1) * 128], win[:, kc, 2048:2560], kc == 0, kc == 7, [win, xt], [p])
            ph.dve(lambda e, p=p, sub=sub: e.tensor_copy(out=vext[:, sub, :, 0:128], in_=p[:, :].rearrange("p (h d) -> p h d", h=4)), [p], [vext])
            p = pj[kpj[0] % 2]; kpj[0] += 1
            for kc in range(8):
                ph.mm(p[:, :], xt[:, kc, sub * 128:(sub + 1) * 128], win[:, kc, 2560:3072], kc == 0, kc == 7, [win, xt], [p])
            ph.act(lambda e, p=p, sub=sub: e.activation(out=osig[:, sub, :], in_=p[:, :], func=AF.Sigmoid), [p], [osig])
        for kc in range(8):
            ph.mm(pgi[:, :], wgi[:, kc, :], xt[:, kc, :], kc == 0, kc == 7, [wgi, xt], [pgi])
        for kc in range(8):
            ph.mm(pgf[:, :], wgf[:, kc, :], xt[:, kc, :], kc == 0, kc == 7, [wgf, xt], [pgf])
        if emstop <= 3:
            continue
        G = [slice(32 * g, 32 * g + 4) for g in range(4)]
        ph.act(lambda e: e.activation(out=ig[:, :], in_=pgi[:, :], func=AF.Identity, bias=pp[:, PE_IB:PE_IB + 1]), [pgi, pp], [ig])
        ph.act(lambda e: e.activation(out=sp_[:, :], in_=pgf[:, :], func=AF.Exp, bias=nfb[:, :], scale=-1.0), [pgf, nfb], [sp_])
        ph.act(lambda e: e.activation(out=sp_[:, :], in_=sp_[:, :], func=AF.Ln, bias=1.0), [sp_], [sp_])
        ph.dve(lambda e: e.tensor_tensor_scan(out=Bc[:, :], data0=sc01, data1=sp_[:, :], initial=0.0, op0=ALU.mult, op1=ALU.add), [sp_] + cd, [Bc])
        ph.dve(lambda e: e.tensor_tensor(out=vt[:, :], in0=ig[:, :], in1=Bc[:, :], op=ALU.add), [ig, Bc], [vt])
        ph.dve(lambda e: e.tensor_tensor_scan(out=Mx[:, :], data0=scneg, data1=vt[:, :], initial=NEG, op0=ALU.add, op1=ALU.max), [vt] + cd, [Mx])
        ph.dve(lambda e: e.tensor_copy(out=ax[:, :], in_=Mx[:, :].rearrange("p (c t) -> p c t", c=4)[:, :, 127]), [Mx], [ax])
        ph.dve(lambda e: e.tensor_scalar(out=nax[:, :], in0=ax[:, :], scalar1=-1.0, scalar2=None, op0=ALU.mult), [ax], [nax])
        for cch in range(4):
            cs = slice(cch * 128, (cch + 1) * 128)
            ph.act(lambda e, cs=cs, cch=cch: e.activation(out=rowsT[G[0], cs], in_=vt[G[0], cs], func=AF.Exp, bias=nax[G[0], cch:cch + 1]), [vt, nax], [rowsT])
        for cch in range(4):
            cs = slice(cch * 128, (cch + 1) * 128)
            ph.dve(lambda e, cs=cs, cch=cch: e.tensor_scalar(out=lw[:, cs], in0=vt[:, cs], scalar1=Bc[:, cch * 128 + 127:cch * 128 + 128], scalar2=None,
                                                            op0=ALU.subtract), [vt, Bc], [lw])
        ph.dve(lambda e: e.tensor_reduce(out=am[:, :], in_=lw[:, :].rearrange("p (c t) -> p c t", c=4), axis=AX.X, op=ALU.max), [lw], [am])
        ph.dve(lambda e: e.tensor_scalar(out=nam[:, :], in0=am[:, :], scalar1=-1.0, scalar2=None, op0=ALU.mult), [am], [nam])
        for cch in range(4):
            cs = slice(cch * 128, (cch + 1) * 128)
            ph.act(lambda e, cs=cs, cch=cch: e.activation(out=rowsT[G[1], cs], in_=lw[G[1], cs], func=AF.Exp, bias=nam[G[1], cch:cch + 1]), [lw, nam], [rowsT])
        ph.dve(lambda e: e.tensor_copy(out=mp[:, 0:1], in_=mcar[:, :]), [mcar], [mp])
        for cch in range(4):
            be = Bc[:, cch * 128 + 127:cch * 128 + 128]
            ph.dve(lambda e, cch=cch, be=be: e.tensor_tensor(out=t1[:, :], in0=mp[:, cch:cch + 1], in1=be, op=ALU.subtract), [mp, Bc], [t1])
            ph.dve(lambda e, cch=cch: e.tensor_tensor(out=mp[:, cch + 1:cch + 2], in0=t1[:, :], in1=am[:, cch:cch + 1], op=ALU.max), [t1, am], [mp])
            ph.dve(lambda e, cch=cch: e.tensor_tensor(out=t1[:, :], in0=t1[:, :], in1=mp[:, cch + 1:cch + 2], op=ALU.subtract), [t1, mp], [t1])
            ph.dve(lambda e, cch=cch: e.tensor_tensor(out=t2[:, :], in0=am[:, cch:cch + 1], in1=mp[:, cch + 1:cch + 2], op=ALU.subtract), [am, mp], [t2])
            ph.act(lambda e, cch=cch: e.activation(out=sv[:, cch, 0:1], in_=t1[:, :], func=AF.Exp), [t1], [sv])
            ph.act(lambda e, cch=cch: e.activation(out=sv[:, cch, 1:2], in_=t2[:, :], func=AF.Exp), [t2], [sv])
        ph.dve(lambda e: e.tensor_copy(out=mcar[:, :], in_=mp[:, 4:5]), [mp], [mcar])
        for cch in range(4):
            cs = slice(cch * 128, (cch + 1) * 128)
            ph.dve(lambda e, cs=cs, cch=cch: e.tensor_scalar(out=U[:, cs], in0=Mx[:, cs], scalar1=mp[:, cch:cch + 1], scalar2=-1.0, op0=ALU.max, op1=ALU.mult),
                   [Mx, mp], [U])
            ph.act(lambda e, cs=cs, cch=cch: e.activation(out=rowsT[G[2], cs], in_=U[G[2], cs], func=AF.Exp, bias=mp[G[2], cch:cch + 1]), [U, mp], [rowsT])
            ph.act(lambda e, cs=cs, cch=cch: e.activation(out=rows2[G[2], cs], in_=U[G[2], cs], func=AF.Exp, bias=ax[G[2], cch:cch + 1]), [U, ax], [rows2])
            ph.dve(lambda e, cs=cs, cch=cch: e.tensor_scalar(out=rows2[G[0], cs], in0=c.t[G[0], CST_ONES:CST_ONES + 128], scalar1=sv[G[0], cch, 0:1], scalar2=None,
                                                            op0=ALU.mult), [sv] + cd, [rows2])
            ph.dve(lambda e, cs=cs, cch=cch: e.tensor_scalar(out=rows2[G[1], cs], in0=c.t[G[1], CST_ONES:CST_ONES + 128], scalar1=sv[G[1], cch, 1:2], scalar2=None,
                                                            op0=ALU.mult), [sv] + cd, [rows2])
        ph.dve(lambda e: e.tensor_tensor(out=tmpr[:, :], in0=Bc[:, :], in1=U[:, :], op=ALU.add), [Bc, U], [tmpr])
        ph.act(lambda e: e.activation(out=rowsT[G[3], :], in_=tmpr[G[3], :], func=AF.Exp), [tmpr], [rowsT])
        if emstop <= 4:
            continue
        for sub in range(4):
            ph.tr(pm[:, 256:384], rowsT[:, sub * 128:(sub + 1) * 128], idf[:, :], [rowsT, idf], [pm_cols])
            ph.act(lambda e, sub=sub: e.activation(out=cols[:, sub, :], in_=pm[:, 256:384], func=AF.Copy), [pm_cols], [cols])
            ph.tr(pm[:, 384:512], rows2[:, sub * 128:(sub + 1) * 128], idf[:, :], [rows2, idf], [pm_sc])
            ph.act(lambda e, sub=sub: e.activation(out=scol[:, sub, :], in_=pm[:, 384:512], func=AF.Copy), [pm_sc], [scol])
        if emstop <= 5:
            continue
        hstop = int(os.environ.get("HSTOP", "99"))
        for sub in range(4):
            cs = slice(sub * 128, (sub + 1) * 128)
            for h in range(4):
                ph.mm(pm[:, 0:128], kT[:, h, cs], qT[:, h, cs], True, True, [kT, qT], [pm_st])
                ph.dve(lambda e: e.tensor_tensor(out=PT[:, :], in0=pm[:, 0:128], in1=c.t[:, CST_MT01:CST_MT01 + 128], op=ALU.mult), [pm_st] + cd, [PT])
                ph.pool(lambda e, sub=sub, h=h: e.tensor_scalar(out=vsc[:, 0:130], in0=vext[:, sub, h, 0:130], scalar1=cols[:, sub, h:h + 1], scalar2=None, op0=ALU.mult),
                        [vext, cols], [vsc])
                if hstop <= 1:
                    continue
                ph.mm(pn[:, 0:129], PT[:, :], vsc[:, 0:129], True, True, [PT, vsc], [pn_num])
                ph.mm(pn[:, 129:258], qT[:, h, cs], Cb[:, h, 0:129], True, True, [qT, Cb], [pn_int])
                ph.act(lambda e, sub=sub, h=h: e.activation(out=ti[:, :], in_=pn[:, 129:258], func=AF.Copy, scale=cols[:, sub, 64 + h:64 + h + 1]), [pn_int, cols], [ti])
                ph.dve(lambda e, sub=sub, h=h: e.scalar_tensor_tensor(out=tot[:, :], in0=pn[:, 0:129], scalar=scol[:, sub, 64 + h:64 + h + 1], in1=ti[:, :],
                                                                      op0=ALU.mult, op1=ALU.add), [pn_num, ti, scol], [tot])
                if hstop <= 2:
                    continue
                ph.act(lambda e: e.activation(out=dd[:, :], in_=tot[:, 128:129], func=AF.Abs), [tot], [dd])
                ph.dve(lambda e, sub=sub, h=h: e.tensor_scalar(out=dd[:, :], in0=dd[:, :], scalar1=cols[:, sub, 96 + h:96 + h + 1], scalar2=None,
                                                               op0=ALU.max), [dd, cols], [dd])
                ph.dve(lambda e: e.reciprocal(out=rec[:, :], in_=dd[:, :]), [dd], [rec])
                ph.dve(lambda e, h=h: e.tensor_scalar(out=hraw[:, h * 128:(h + 1) * 128], in0=tot[:, 0:128], scalar1=rec[:, :], scalar2=None, op0=ALU.mult),
                       [tot, rec], [hraw])
                if hstop <= 3:
                    continue
                ph.dve(lambda e, sub=sub, h=h: e.tensor_scalar(out=kw[:, :], in0=ktok[:, sub, h * 128:(h + 1) * 128], scalar1=cols[:, sub, 32 + h:32 + h + 1],
                                                               scalar2=None, op0=ALU.mult), [ktok, cols], [kw])
                ph.mm(pn[:, 258:387], kw[:, :], vext[:, sub, h, 0:129], True, True, [kw, vext], [pn_kv])
                ph.dve(lambda e, sub=sub, h=h: e.tensor_scalar(out=Cf[:, h, 0:129], in0=Cf[:, h, 0:129], scalar1=scol[:, sub, h:h + 1], scalar2=None, op0=ALU.mult),
                       [Cf, scol], [Cf])
                ph.dve(lambda e, sub=sub, h=h: e.scalar_tensor_tensor(out=Cf[:, h, 0:129], in0=pn[:, 258:387], scalar=scol[:, sub, 32 + h:32 + h + 1], in1=Cf[:, h, 0:129],
                                                                      op0=ALU.mult, op1=ALU.add), [pn_kv, scol, Cf], [Cf])
                ph.act(lambda e, h=h: e.activation(out=Cb[:, h, 0:129], in_=Cf[:, h, 0:129], func=AF.Copy), [Cf], [Cb])
            if hstop <= 4:
                continue
            for h in range(4):
                ph.dve(lambda e, h=h: e.bn_stats(out=hst[:, h, :], in_=hraw[:, h * 128:(h + 1) * 128]), [hraw], [hst])
                ph.dve(lambda e, h=h: e.bn_aggr(out=hmv[:, h, :], in_=hst[:, h, :]), [hst], [hmv])
            ph.dve(lambda e: e.tensor_scalar(out=hrs[:, :], in0=hmv[:, :, 1], scalar1=EPS, scalar2=None, op0=ALU.add), [hmv], [hrs])
            ph.act(lambda e: e.activation(out=hrs[:, :], in_=hrs[:, :], func=AF.Sqrt), [hrs], [hrs])
            ph.dve(lambda e: e.reciprocal(out=hrs[:, :], in_=hrs[:, :]), [hrs], [hrs])
            for h in range(4):
                ph.dve(lambda e, h=h: e.tensor_scalar(out=hraw[:, h * 128:(h + 1) * 128], in0=hraw[:, h * 128:(h + 1) * 128], scalar1=hmv[:, h, 0:1],
                                                      scalar2=hrs[:, h:h + 1], op0=ALU.subtract, op1=ALU.mult), [hraw, hmv, hrs], [hraw])
            ph.pool(lambda e: e.tensor_tensor(out=hraw[:, :], in0=hraw[:, :], in1=hg[:, :], op=ALU.mult), [hraw, hg], [hraw])
            ph.dve(lambda e, sub=sub: e.tensor_tensor(out=hb[:, :], in0=hraw[:, :], in1=osig[:, sub, :], op=ALU.mult), [hraw, osig], [hb])
            for h in range(4):
                ph.tr(trp[:, h * 128:(h + 1) * 128], hb[:, h * 128:(h + 1) * 128], idb[:, :], [hb, idb], [trp])
            ph.act(lambda e, cs=cs: e.activation(out=mixT[:, 4:8, cs], in_=trp[:, 0:512].rearrange("p (c t) -> p c t", c=4), func=AF.Copy), [trp], [mixT])
        if emstop <= 6:
            continue
        for sub in range(4):
            n = tt * 4 + sub
            for half in range(2):
                for kc in range(8):
                    ph.mm(pj[half][:, :], mixT[:, kc, sub * 128:(sub + 1) * 128], wout[:, kc, half * 512:(half + 1) * 512], kc == 0, kc == 7, [mixT, wout], [pj[half]])
            epi.run(n, lambda lo, hi: pj[lo // 512][:, :], [pj[0], pj[1]], idb, idf, trp, None, None)
    ph.finish()


def phase_odd_mixer(nc, cx, name, j):
    ph = Phase(nc, name)
    mk_deps(ph, cx)
    c, idb = load_consts(ph, cx, 640)
    cd = [c]
    idf = Buf(c.t[:, 0:128], c.d)
    S = cx.S
    win = ph.sb("win", [128, 8, 2560], BF16)
    ph.dma("pool", win[:, :, :], cx.cd_w_in[j].rearrange("(c p) n -> p c n", p=128), [], [win], win)
    wout = ph.sb("wout", [128, 8, D], BF16)
    ph.dma("pool", wout[:, :, :], cx.cd_w_out[j].rearrange("(c p) n -> p c n", p=128), [], [wout], wout)
    ri = cx.row_idx
    cng = ph.sb("cng", [128, 512]); load_rows(ph, "sp", cng, cx.rows[ri[("cng", j)]:ri[("cng", j)] + 1, 0:512], 512)
    cnb = ph.sb("cnb", [128, 512]); load_rows(ph, "sp", cnb, cx.rows[ri[("cnb", j)]:ri[("cnb", j)] + 1, 0:512], 512)
    bsb = ph.sb("bsb", [128, 512]); load_rows(ph, "sp", bsb, cx.rows[ri[("bs", j)]:ri[("bs", j)] + 1, 0:512], 512)
    wsf = ph.sb("wsf", [128, 4, 128])
    ph.dma("sp", wsf[:, :, :], cx.wst[j].rearrange("g s t -> s g t"), [], [wsf], wsf)
    WcT = ph.sb("WcT", [128, 4, 128], BF16)
    for g in range(4):
        ph.dve(lambda e, g=g: e.tensor_tensor(out=WcT[:, g, :], in0=wsf[:, g, :], in1=c.t[:, CST_MT01:CST_MT01 + 128], op=ALU.mult), [wsf] + cd, [WcT])
    epi = Epi(ph, cx, cx.rows[ri[("oln1g", j)]:ri[("oln1g", j)] + 1, :], cx.rows[ri[("oln1b", j)]:ri[("oln1b", j)] + 1, :], False,
              router={"w": cx.router_w[j], "b": cx.rows[ri[("rb", j)]:ri[("rb", j)] + 1, 0:NE]}, nbuf=1)
    kTres = ph.sb("kTres", [128, 4, S], BF16)
    Vres = ph.sb("Vres", [128, cx.NT, 512], BF16)
    pj = [ph.ps("pj%d" % i, [128, 512]) for i in range(2)]
    pz = [ph.ps("pz%d" % i, [128, 512]) for i in range(2)]
    pg = ph.ps("pg", [128, 512]); pos = [ph.ps("po%d" % i, [128, 512]) for i in range(2)]
    trp = ph.ps("trp", [128, 1024], BF16)
    xt = ph.sb("xTt", [128, 8, 512], BF16)
    uT = ph.sb("uT", [128, 4, 512], BF16)
    zf = ph.sb("zf", [128, 512]); zb = ph.sb("zb", [128, 512], BF16)
    zst = ph.sb("zst", [128, 1, 6]); zmv = ph.sb("zmv", [128, 2]); zrs = ph.sb("zrs", [128, 1]); znb = ph.sb("znb", [128, 1])
    gtmp = zf
    mixT = ph.sb("mixT", [128, 8, 512], BF16)
    qT = ph.sb("qT", [128, 4, 512], BF16)
    NB = 2
    ebuf = [ph.sb("ebuf%d" % i, [128, 512]) for i in range(NB)]
    spb = [ph.sb("spb%d" % i, [128, 513]) for i in range(NB)]
    csb = [ph.sb("csb%d" % i, [128, 513]) for i in range(NB)]
    attb = [ph.sb("attb%d" % i, [128, 512], BF16) for i in range(NB)]
    attT = ph.sb("attT", [128, 2, 4, 128], BF16)
    for i in range(NB):
        ph.dve(lambda e, i=i: e.memset(spb[i][:, 0:1], 0.0), [], [spb[i]])
    cars = [ph.sb("car%d" % i, [128, 1]) for i in range(NB)]
    nbb = [ph.sb("nbb%d" % i, [128, 1]) for i in range(NB)]
    dout = ph.sb("dout", [128, 512], BF16)
    onesr = c.t[:, CST_ONES:CST_ONES + 128]
    ones513 = ph.sb("ones513", [128, 513], BF16)
    ph.dve(lambda e: e.memset(ones513[:, :], 1.0), [], [ones513])
    m01s = c.t[:, CST_M01S:CST_M01S + 128]
    kpj = [0]
    kz = [0]
    import os
    for tt in range(int(os.environ.get("OMT", cx.NTT))):
        t0 = tt * 512
        ph.dma("sp", xt[:, :, :], cx.xT.rearrange("c p s -> p c s")[:, :, t0:t0 + 512], [cx.xT_dep[tt]], [xt], xt)
        for ch in range(4):
            p = pj[kpj[0] % 2]; kpj[0] += 1
            for kc in range(8):
                ph.mm(p[:, :], win[:, kc, ch * 128:(ch + 1) * 128], xt[:, kc, :], kc == 0, kc == 7, [win, xt], [p])
            ph.act(lambda e, p=p, ch=ch: e.activation(out=uT[:, ch, :], in_=p[:, :], func=AF.Gelu), [p], [uT])
        for pr in range(4):
            p = pj[kpj[0] % 2]; kpj[0] += 1
            for kc in range(8):
                ph.mm(p[:, :], win[:, kc, 1024 + pr * 128:1024 + (pr + 1) * 128], xt[:, kc, :], kc == 0, kc == 7, [win, xt], [p])
            ph.act(lambda e, p=p, pr=pr: e.activation(out=qT[:, pr, :], in_=p[:, :], func=AF.Copy, scale=0.125), [p], [qT])
            p = pj[kpj[0] % 2]; kpj[0] += 1
            for kc in range(8):
                ph.mm(p[:, :], win[:, kc, 1536 + pr * 128:1536 + (pr + 1) * 128], xt[:, kc, :], kc == 0, kc == 7, [win, xt], [p])
            ph.dve(lambda e, p=p, pr=pr, t0=t0: e.tensor_copy(out=kTres[:, pr, t0:t0 + 512], in_=p[:, :]), [p], [kTres])
        for sub in range(4):
            n = tt * 4 + sub
            cs = slice(sub * 128, (sub + 1) * 128)
            p = pj[kpj[0] % 2]; kpj[0] += 1
            for kc in range(8):
                ph.mm(p[:, :], xt[:, kc, cs], win[:, kc, 2048:2560], kc == 0, kc == 7, [win, xt], [p])
            ph.act(lambda e, p=p, n=n: e.activation(out=Vres[:, n, :], in_=p[:, :], func=AF.Copy), [p], [Vres])
            p = pj[kpj[0] % 2]; kpj[0] += 1
            for kc in range(8):
                ph.mm(p[:, :], xt[:, kc, cs], win[:, kc, 512:1024], kc == 0, kc == 7, [win, xt], [p])
            ph.act(lambda e, p=p: e.activation(out=zf[:, :], in_=p[:, :], func=AF.Gelu), [p], [zf])
            ln_rows(ph, zf, zmv, zst, zrs, znb, 512)
            ph.act(lambda e: e.activation(out=zf[:, :], in_=zf[:, :], func=AF.Identity, bias=znb[:, :], scale=zrs[:, :]), [zf, znb, zrs], [zf])
            ph.pool(lambda e: e.tensor_tensor(out=zf[:, :], in0=zf[:, :], in1=cng[:, :], op=ALU.mult), [zf, cng], [zf])
            ph.pool(lambda e: e.tensor_tensor(out=zb[:, :], in0=zf[:, :], in1=cnb[:, :], op=ALU.add), [zf, cnb], [zb])
            for g in range(4):
                ph.mm(pg[:, g * 128:(g + 1) * 128], zb[:, g * 128:(g + 1) * 128], WcT[:, g, :], True, True, [zb, WcT], [pg])
            ph.dve(lambda e: e.tensor_tensor(out=gtmp[:, :], in0=pg[:, :], in1=bsb[:, :], op=ALU.add), [pg, bsb], [gtmp])
            ph.dve(lambda e, cs=cs: e.tensor_tensor(out=mixT[:, 0:4, cs], in0=gtmp[:, :].rearrange("p (g t) -> p g t", g=4), in1=uT[:, :, cs], op=ALU.mult),
                   [gtmp, uT], [mixT])
            kend = (n + 1) * 128
            KT = (kend + 511) // 512
            for pr in range(4):
                J = (0, 1)
                for j in J:
                    ph.dve(lambda e, j=j: e.memset(cars[j][:, :], 0.0), [], [cars[j]])
                first = [True, True]
                steps = []
                for kt in reversed(range(KT)):
                    k0 = kt * 512
                    w = min(kend, k0 + 512) - k0
                    steps.append((kt, k0, w, w // 128))

                def emit_z(st):
                    kt_, k0_, w_, _ = st
                    for j in J:
                        b0 = j * 64
                        ph.mm(pz[j][:, 0:w_], qT[b0:b0 + 64, pr, cs], kTres[b0:b0 + 64, pr, k0_:k0_ + w_], True, True, [qT, kTres], [pz[j]])

                emit_z(steps[0])
                for si, (kt, k0, w, nblk) in enumerate(steps):
                    for j in J:
                        ph.act(lambda e, j=j, w=w: e.activation(out=ebuf[j][:, 0:w], in_=pz[j][:, 0:w], func=AF.Exp), [pz[j]], [ebuf[j]])
                    if si + 1 < len(steps):
                        emit_z(steps[si + 1])
                    for j in J:
                        ph.act(lambda e, j=j, w=w: e.activation(out=spb[j][:, 1:w + 1], in_=ebuf[j][:, 0:w], func=AF.Ln, bias=1.0), [ebuf[j]], [spb[j]])
                    if kt == KT - 1:
                        for j in J:
                            ph.pool(lambda e, j=j, w=w: e.tensor_tensor(out=spb[j][:, 1 + w - 128:1 + w], in0=spb[j][:, 1 + w - 128:1 + w], in1=m01s, op=ALU.mult),
                                    [spb[j]] + cd, [spb[j]])
                            ph.pool(lambda e, j=j, w=w: e.tensor_tensor(out=ebuf[j][:, w - 128:w], in0=ebuf[j][:, w - 128:w], in1=m01s, op=ALU.mult),
                                    [ebuf[j]] + cd, [ebuf[j]])
                    for j in J:
                        ph.dve(lambda e, j=j, w=w: e.tensor_tensor_scan(out=csb[j][:, 0:w + 1], data0=ones513[:, 0:w + 1], data1=spb[j][:, 0:w + 1], initial=0.0,
                                                                       op0=ALU.mult, op1=ALU.add), [spb[j], ones513], [csb[j]])
                    for j in J:
                        ph.dve(lambda e, j=j, w=w: e.tensor_tensor(out=cars[j][:, :], in0=csb[j][:, w:w + 1], in1=cars[j][:, :], op=ALU.add), [csb[j], cars[j]], [cars[j]])
                        ph.dve(lambda e, j=j: e.tensor_scalar(out=nbb[j][:, :], in0=cars[j][:, :], scalar1=-1.0, scalar2=None, op0=ALU.mult), [cars[j]], [nbb[j]])
                    for j in J:
                        ph.act(lambda e, j=j, w=w: e.activation(out=csb[j][:, 0:w], in_=csb[j][:, 0:w], func=AF.Exp, bias=nbb[j][:, :]), [csb[j], nbb[j]], [csb[j]])
                    for j in J:
                        ph.pool(lambda e, j=j, w=w: e.tensor_tensor(out=attb[j][:, 0:w], in0=ebuf[j][:, 0:w], in1=csb[j][:, 0:w], op=ALU.mult), [ebuf[j], csb[j]], [attb[j]])
                    for j in J:
                        for jb in range(nblk):
                            ph.tr(trp[:, j * 512 + jb * 128:j * 512 + (jb + 1) * 128], attb[j][:, jb * 128:(jb + 1) * 128], idb[:, :], [attb[j], idb], [trp])
                    ph.dve(lambda e, nblk=nblk: e.tensor_copy(out=attT[:, :, 0:nblk, :], in_=trp[:, :].rearrange("p (j b t) -> p j b t", j=2, b=4)[:, :, 0:nblk, :]),
                           [trp], [attT])
                    for j in J:
                        h = 2 * pr + j
                        for jb in range(nblk):
                            kb = kt * 4 + jb
                            last = (kt == 0 and jb == nblk - 1)
                            ph.mm(pos[j][:, h * 64:(h + 1) * 64], attT[:, j, jb, :], Vres[:, kb, h * 64:(h + 1) * 64], first[j], last, [attT, Vres], [pos[j]])
                            first[j] = False
            for j in (0, 1):
                ph.act(lambda e, j=j: e.activation(out=dout[:, :].rearrange("p (r j d) -> p r j d", r=4, j=2)[:, :, j, :],
                                                   in_=pos[j][:, :].rearrange("p (r j d) -> p r j d", r=4, j=2)[:, :, j, :], func=AF.Copy), [pos[j]], [dout])
            for cc in range(4):
                ph.tr(trp[:, cc * 128:(cc + 1) * 128], dout[:, cc * 128:(cc + 1) * 128], idb[:, :], [dout, idb], [trp])
            ph.dve(lambda e, cs=cs: e.tensor_copy(out=mixT[:, 4:8, cs], in_=trp[:, 0:512].rearrange("p (c t) -> p c t", c=4)), [trp], [mixT])
        for sub in range(4):
            n = tt * 4 + sub
            for half in range(2):
                for kc in range(8):
                    ph.mm(pj[half][:, :], mixT[:, kc, sub * 128:(sub + 1) * 128], wout[:, kc, half * 512:(half + 1) * 512], kc == 0, kc == 7, [mixT, wout], [pj[half]])
            epi.run(n, lambda lo, hi: pj[lo // 512][:, :], [pj[0], pj[1]], idb, idf, trp, pz, pg)
    ph.finish()


def mk_deps(ph, cx):
    cx.acc_dep = [ph.region("acc%d" % n) for n in range(cx.NT)]
    cx.out_dep = [ph.region("out%d" % n) for n in range(cx.NT)]
    cx.xT_dep = [ph.region("xT%d" % t) for t in range(cx.NTT)]
    cx.comb_dep = ph.region("comb")


ROW_KEYS_EVEN = ["eln1g", "eln1b", "eln2g", "eln2b", "hg"]
ROW_KEYS_ODD = ["oln1g", "oln1b", "oln2g", "oln2b", "cng", "cnb", "bs", "rb"]


def row_index():
    idx = {}
    k = 0
    for j in range(2):
        for key in ROW_KEYS_EVEN + ROW_KEYS_ODD:
            idx[(key, j)] = k
            k += 1
    return idx, k


def build(S, layers):
    nc = bass.Bass("TRN2", target_bir_lowering=False)
    gst = ExitStack()
    make_sempool(nc, gst)
    nc._gst = gst
    cx = Ctx()
    cx.S = S; cx.NT = S // 128; cx.NTT = S // 512; cx.NCST = NCST
    cx.row_idx, nrows = row_index()

    def inp(name, shape):
        return nc.dram_tensor(name, list(shape), F32, kind="ExternalInput").ap()

    cx.x = inp("x", [S, D])
    cx.out = nc.dram_tensor("out", [S, D], F32, kind="ExternalOutput").ap()
    cx.cst = inp("cst", [128, NCST])
    cx.rows = inp("rows", [nrows, D])
    cx.pe = [inp("pe0", [128, NPE]), inp("pe1", [128, NPE])]
    cx.wst = inp("wst", [2, 4, 128, 128])
    cx.wkeys = []
    if any(l % 2 == 0 for l in layers):
        cx.ab_w_in = inp("ab_w_in", [2, D, 3080]); cx.ab_w_out = inp("ab_w_out", [2, D, D])
        cx.ffn_w1 = inp("ffn_w1", [2, D, DFF]); cx.ffn_w3 = inp("ffn_w3", [2, D, DFF]); cx.ffn_w2 = inp("ffn_w2", [2, DFF, D])
        cx.wkeys += ["ab_w_in", "ab_w_out", "ffn_w1", "ffn_w3", "ffn_w2"]
    if any(l % 2 == 1 for l in layers):
        cx.cd_w_in = inp("cd_w_in", [2, D, 2560]); cx.cd_w_out = inp("cd_w_out", [2, D, D])
        cx.router_w = inp("router_w", [2, D, NE])
        cx.moe_w1 = inp("moe_w1", [2, NE, D, DFF]); cx.moe_w3 = inp("moe_w3", [2, NE, D, DFF]); cx.moe_w2 = inp("moe_w2", [2, NE, DFF, D])
        cx.wkeys += ["cd_w_in", "cd_w_out", "router_w", "moe_w1", "moe_w3", "moe_w2"]
    nc._wkeys = cx.wkeys
    cx.acc = nc.dram_tensor("acc", [S, D], F32).ap()
    cx.xT = nc.dram_tensor("xT", [8, 128, S], BF16).ap()
    cx.comb = nc.dram_tensor("comb", [S, NE], F32).ap()
    ri = cx.row_idx
    import os
    kstop = int(os.environ.get("KSTOP", "99"))
    phase_prologue(nc, cx)
    if kstop <= 0:
        return nc
    for li, l in enumerate(layers):
        j = l // 2
        last = (li == len(layers) - 1)
        if l % 2 == 0:
            phase_even_mixer(nc, cx, "em%d" % l, j)
            if kstop <= 1:
                return nc
            phase_ffn(nc, cx, "ef%d" % l, [cx.ffn_w1[j]], [cx.ffn_w3[j]], [cx.ffn_w2[j]],
                      cx.rows[ri[("eln2g", j)]:ri[("eln2g", j)] + 1, :], cx.rows[ri[("eln2b", j)]:ri[("eln2b", j)] + 1, :], last, False)
        else:
            phase_odd_mixer(nc, cx, "om%d" % l, j)
            if kstop <= 1:
                phase_dump(nc, cx)
                return nc
            phase_ffn(nc, cx, "of%d" % l, [cx.moe_w1[j][e] for e in range(NE)], [cx.moe_w3[j][e] for e in range(NE)],
                      [cx.moe_w2[j][e] for e in range(NE)],
                      cx.rows[ri[("oln2g", j)]:ri[("oln2g", j)] + 1, :], cx.rows[ri[("oln2b", j)]:ri[("oln2b", j)] + 1, :], last, True)
    return nc


def host_consts():
    c = np.zeros((128, NCST), np.float32)
    i = np.arange(128)
    c[:, CST_ID:CST_ID + 128] = np.eye(128, dtype=np.float32)
    c[:, CST_ONES:CST_ONES + 128] = 1.0
    c[:, CST_MTNEG:CST_MTNEG + 128] = np.where(i[:, None] <= i[None, :], 0.0, NEG)
    c[:, CST_M01S:CST_M01S + 128] = (i[None, :] < i[:, None]).astype(np.float32)
    c[:, CST_MT01:CST_MT01 + 128] = (i[:, None] <= i[None, :]).astype(np.float32)
    for h in range(4):
        c[h, CST_SEL + h * 128:CST_SEL + (h + 1) * 128] = 1.0
    t = np.arange(512)
    c[:, CST_SC01:CST_SC01 + 512] = (t % 128 != 0).astype(np.float32)[None, :]
    c[:, CST_SCNEG:CST_SCNEG + 512] = np.where(t % 128 == 0, NEG, 0.0)[None, :]
    return c


def host_layout(inp):
    f = lambda a: np.asarray(a, dtype=np.float32)
    idx, nrows = row_index()
    rows = np.zeros((nrows, D), np.float32)

    def put(key, j, v):
        v = f(v).reshape(-1)
        rows[idx[(key, j)], :v.size] = v

    pes = []
    for j in range(2):
        put("eln1g", j, inp["ab_ln1_g"][j]); put("eln1b", j, inp["ab_ln1_b"][j])
        put("eln2g", j, inp["ab_ln2_g"][j]); put("eln2b", j, inp["ab_ln2_b"][j])
        put("hg", j, inp["b_norm_g"][j])
        put("oln1g", j, inp["cd_ln1_g"][j]); put("oln1b", j, inp["cd_ln1_b"][j])
        put("oln2g", j, inp["cd_ln2_g"][j]); put("oln2b", j, inp["cd_ln2_b"][j])
        put("cng", j, inp["c_norm_g"][j]); put("cnb", j, inp["c_norm_b"][j])
        put("bs", j, inp["c_b_s"][j]); put("rb", j, inp["router_b"][j])
        pe = np.zeros((128, NPE), np.float32)
        cw = f(inp["a_conv_w"][j])
        pe[:, PE_CW:PE_CW + 124] = cw.reshape(31, 4, 128).transpose(2, 1, 0).reshape(128, 124)
        pe[:, PE_CB:PE_CB + 4] = f(inp["a_conv_b"][j]).reshape(4, 128).T
        pe[:, PE_AG:PE_AG + 4] = f(inp["a_norm_g"][j]).reshape(4, 128).T
        pe[:, PE_AB:PE_AB + 4] = f(inp["a_norm_b"][j]).reshape(4, 128).T
        gb = f(inp["ab_gate_bias"][j])
        for g in range(4):
            pe[32 * g:32 * g + 4, PE_IB] = gb[0:4]
            pe[32 * g:32 * g + 4, PE_FB] = gb[4:8]
        pes.append(pe)
    wst = np.ascontiguousarray(f(inp["c_w_s"]).transpose(0, 1, 3, 2))
    return rows, pes, wst


WKEYS = ["ab_w_in", "ab_w_out", "ffn_w1", "ffn_w3", "ffn_w2", "cd_w_in", "cd_w_out", "router_w", "moe_w1", "moe_w3", "moe_w2"]
_CACHE = {}


def run(inputs, S, layers, ncores):
    key = (S, tuple(layers))
    if key not in _CACHE:
        _CACHE[key] = build(S, layers)
    nc = _CACHE[key]
    rows, pes, wst = host_layout(inputs)
    cst = host_consts()
    shared = {"cst": cst, "rows": rows, "pe0": pes[0], "pe1": pes[1], "wst": wst}
    for k in nc._wkeys:
        shared[k] = np.ascontiguousarray(np.asarray(inputs[k], dtype=np.float32))
    x = np.asarray(inputs["x"], dtype=np.float32)
    in_maps = []
    for c in range(ncores):
        m = dict(shared)
        m["x"] = np.ascontiguousarray(x[c, :S, :])
        in_maps.append(m)
    import os
    if os.environ.get("KTRACE"):
        res = run_bass_kernel_spmd(nc, in_maps, core_ids=list(range(ncores)), trace=True)
        print("EXEC_NS", res.exec_time_ns)
    else:
        res = run_bass_kernel_spmd(nc, in_maps, core_ids=list(range(ncores)))
    return np.stack([res.results[c]["out"] for c in range(ncores)], axis=0)


def kernel(**inputs):
    return run(inputs, 4096, [0, 1, 2, 3], 8)


def phase_dump(nc, cx):
    ph = Phase(nc, "dump")
    mk_deps(ph, cx)
    t = ph.sb("t", [128, D])
    for n in range(cx.NT):
        ph.dma("sp", t[:, :], cx.acc[n * 128:(n + 1) * 128, :], [], [t], t)
        ph.dma("sp", cx.out[n * 128:(n + 1) * 128, :], t[:, :], [t], [cx.out_dep[n]], t)
    ph.finish()
```

```python
import math
from contextlib import ExitStack
import numpy as np
import concourse.bass as bass
import concourse.mybir as mybir
from concourse.bass_utils import run_bass_kernel_spmd

F32 = mybir.dt.float32
BF16 = mybir.dt.bfloat16
AF = mybir.ActivationFunctionType
ALU = mybir.AluOpType
AX = mybir.AxisListType

D = 1024
DFF = 2816
NFF = 22
NE = 8
DEPTH = 4
ALPHA = (2 * DEPTH) ** 0.25
EPS = 1e-5
NEG = -1.0e30
FGROUPS = [(0, 4), (4, 4), (8, 4), (12, 4), (16, 4), (20, 2)]

ENGS = ["pe", "act", "dve", "pool", "sp"]


class Dep:
    __slots__ = ("name", "lw", "rd", "sem", "dcount", "lastdma", "psum")

    def __init__(self, name):
        self.name = name
        self.psum = False
        self.lw = None
        self.rd = []
        self.sem = None
        self.dcount = 0
        self.lastdma = None


class Op:
    __slots__ = ("id", "eng", "fn", "deps", "dma", "signal", "sigval", "sem", "inc")

    def __init__(self, id, eng, fn, dma, inc):
        self.id = id
        self.eng = eng
        self.fn = fn
        self.deps = set()
        self.dma = dma
        self.signal = False
        self.sigval = 0
        self.sem = None
        self.inc = inc


class Buf:
    __slots__ = ("t", "d")

    def __init__(self, t, d):
        self.t = t
        self.d = d

    def __getitem__(self, k):
        return self.t[k]


SEMPOOL = {}
NDMASEM = 56


def make_sempool(nc, stack):
    pool = {"eng": {}, "dma": []}
    for e in ["pe", "act", "dve", "pool"]:
        pool["eng"][e] = [stack.enter_context(nc.semaphore("s_" + e)), 0]
    for i in range(NDMASEM):
        pool["dma"].append([stack.enter_context(nc.semaphore("d%d" % i)), 0])
    SEMPOOL[id(nc)] = pool


class Phase:
    def __init__(self, nc, name):
        self.nc = nc
        self.name = name
        self.ops = []
        self.by_eng = {e: [] for e in ENGS}
        self.dma_deps = []
        self.all_deps = []
        self.st = ExitStack()
        self.nsb = 0

    def dep(self, name):
        d = Dep(name)
        self.all_deps.append(d)
        return d

    def sb(self, name, shape, dt=F32):
        t = self.st.enter_context(self.nc.sbuf_tensor(self.name + "_" + name, list(shape), dt))
        return Buf(t, self.dep(name))

    def ps(self, name, shape, dt=F32):
        t = self.st.enter_context(self.nc.psum_tensor(self.name + "_" + name, list(shape), dt))
        b = Buf(t, self.dep(name))
        b.d.psum = True
        return b

    def region(self, name):
        return Buf(None, self.dep(name))

    def op(self, eng, fn, R=(), W=(), dma=None, inc=16):
        o = Op(len(self.ops), eng, fn, dma, inc)
        self.ops.append(o)
        self.by_eng[eng].append(o)
        deps = o.deps
        if any(b.d.psum for b in R):
            W = list(W) + [b for b in R if b.d.psum]
            R = [b for b in R if not b.d.psum]
        for b in R:
            t = b.d
            if t.lw is not None:
                deps.add(t.lw)
        for b in W:
            t = b.d
            if t.lw is not None:
                deps.add(t.lw)
            deps.update(t.rd)
        if dma is not None:
            dd = dma.d
            if dd.lastdma is not None:
                deps.add(dd.lastdma)
            if fn is not None:
                dd.lastdma = o.id
            if dd.sem is None:
                dd.sem = True
                self.dma_deps.append(dd)
        deps.discard(o.id)
        best = {}
        for d in deps:
            od = self.ops[d]
            if od.dma is None and od.fn is not None:
                if d > best.get(od.eng, -1):
                    best[od.eng] = d
        for d in list(deps):
            od = self.ops[d]
            if od.dma is None and od.fn is not None and best[od.eng] != d:
                deps.discard(d)
        if eng == "pe":
            for d in list(deps):
                od = self.ops[d]
                if od.eng == "pe" and od.dma is None:
                    deps.discard(d)
        for d in deps:
            self.ops[d].signal = True
        if fn is not None:
            for b in R:
                b.d.rd.append(o.id)
            for b in W:
                b.d.lw = o.id
                b.d.rd = []
        return o

    def pe(self, fn, R=(), W=()):
        return self.op("pe", fn, R, W)

    def act(self, fn, R=(), W=()):
        return self.op("act", fn, R, W)

    def dve(self, fn, R=(), W=()):
        return self.op("dve", fn, R, W)

    def pool(self, fn, R=(), W=()):
        return self.op("pool", fn, R, W)

    def dma(self, q, out_ap, in_ap, R, W, semb):
        return self.op(q, lambda e: e.dma_start(out=out_ap, in_=in_ap), R, W, dma=semb)

    def mm(self, out_ap, lhsT, rhs, start, stop, R, W):
        return self.op("pe", lambda e: e.matmul(out_ap, lhsT=lhsT, rhs=rhs, start=start, stop=stop), R, W)

    def tr(self, out_ap, in_ap, ident_ap, R, W):
        return self.op("pe", lambda e: e.transpose(out=out_ap, in_=in_ap, identity=ident_ap), R, W)

    def barrier(self):
        allb = [Buf(None, d) for d in self.all_deps]
        for e in ENGS:
            self.op(e, None, R=(), W=allb)

    def finish(self):
        nc = self.nc
        st = self.st
        allb = [Buf(None, d) for d in self.all_deps]
        for e in ENGS:
            self.op(e, None, R=(), W=allb)
        pool = SEMPOOL[id(nc)]
        engsem = {}
        cnt = {e: 0 for e in ENGS}
        for e in ["pe", "act", "dve", "pool"]:
            engsem[e] = pool["eng"][e][0]
            cnt[e] = pool["eng"][e][1]
        assert len(self.dma_deps) <= len(pool["dma"]), len(self.dma_deps)
        for i, d in enumerate(self.dma_deps):
            d.sem = pool["dma"][i][0]
            d.dcount = pool["dma"][i][1]
        for o in self.ops:
            if o.fn is None:
                continue
            if o.dma is not None:
                o.dma.d.dcount += o.inc
                o.sigval = o.dma.d.dcount
                o.sem = o.dma.d.sem
            elif o.signal:
                cnt[o.eng] += 1
                o.sigval = cnt[o.eng]
                o.sem = engsem[o.eng]
        ops = self.ops

        def run(eng_name):
            def body(e):
                seen = {}
                for o in self.by_eng[eng_name]:
                    need = {}
                    for d in o.deps:
                        od = ops[d]
                        if od.fn is None:
                            continue
                        key = id(od.sem)
                        if od.sigval > need.get(key, (0, None))[0]:
                            need[key] = (od.sigval, od.sem)
                    for key, (val, sem) in need.items():
                        if seen.get(key, 0) >= val:
                            continue
                        e.wait_ge(sem, val)
                        seen[key] = val
                    if o.fn is None:
                        continue
                    ins = o.fn(e)
                    if o.dma is not None:
                        ins.then_inc(o.sem, o.inc)
                    elif o.signal:
                        ins.then_inc(o.sem, 1)
            return body

        for e in ["pe", "act", "dve", "pool"]:
            pool["eng"][e][1] = cnt[e]
        for i, d in enumerate(self.dma_deps):
            pool["dma"][i][1] = d.dcount
        block = st.enter_context(nc.Block())
        block.tensor(run("pe"))
        block.scalar(run("act"))
        block.vector(run("dve"))
        block.gpsimd(run("pool"))
        block.sync(run("sp"))
        st.close()


class Ctx:
    pass


def load_rows(ph, q, buf, dram_row_ap, n):
    ph.dma(q, buf[:, 0:n], dram_row_ap.partition_broadcast(128), [], [buf], buf)


def ln_rows(ph, s, mv, stats, rstd, nb, width, eps=EPS):
    nchunk = (width + 511) // 512
    for h in range(nchunk):
        lo, hi = h * 512, min(width, (h + 1) * 512)
        ph.dve(lambda e, h=h, lo=lo, hi=hi: e.bn_stats(out=stats[:, h, :], in_=s[:, lo:hi]), [s], [stats])
    ph.dve(lambda e: e.bn_aggr(out=mv[:, :], in_=stats[:, 0:nchunk, :].rearrange("p a b -> p (a b)")), [stats], [mv])
    ph.dve(lambda e: e.tensor_scalar(out=rstd[:, :], in0=mv[:, 1:2], scalar1=eps, scalar2=None, op0=ALU.add), [mv], [rstd])
    ph.act(lambda e: e.activation(out=rstd[:, :], in_=rstd[:, :], func=AF.Sqrt), [rstd], [rstd])
    ph.dve(lambda e: e.reciprocal(out=rstd[:, :], in_=rstd[:, :]), [rstd], [rstd])
    ph.dve(lambda e: e.scalar_tensor_tensor(out=nb[:, :], in0=mv[:, 0:1], scalar=-1.0, in1=rstd[:, :], op0=ALU.mult, op1=ALU.mult),
           [mv, rstd], [nb])


class Epi:
    def __init__(self, ph, cx, g_row, b_row, last, router=None, nbuf=2):
        self.ph = ph
        self.cx = cx
        self.last = last
        self.router = router
        self.grow = ph.sb("e_g", [128, D]); load_rows(ph, "sp", self.grow, g_row, D)
        self.brow = ph.sb("e_b", [128, D]); load_rows(ph, "sp", self.brow, b_row, D)
        self.nbuf = nbuf
        self.s = [ph.sb("e_s%d" % i, [128, D]) for i in range(nbuf)]
        self.y = [ph.sb("e_y%d" % i, [128, D]) for i in range(nbuf)]
        self.yb = [ph.sb("e_yb%d" % i, [128, D], BF16) for i in range(nbuf)]
        self.ya = self.s
        self.stats = ph.sb("e_st", [128, 2, 6]); self.mv = ph.sb("e_mv", [128, 2])
        self.rstd = ph.sb("e_rstd", [128, 1]); self.nb = ph.sb("e_nb", [128, 1])
        self.xTs = [ph.sb("e_xT%d" % i, [128, 8, 512], BF16) for i in range(nbuf)]
        self.k = 0
        if router is not None:
            self.rw = ph.sb("r_w", [128, 8, NE]); ph.dma("sp", self.rw[:, :, :], router["w"].rearrange("(c p) e -> p c e", p=128), [], [self.rw], self.rw)
            self.rb = ph.sb("r_b", [128, NE]); load_rows(ph, "sp", self.rb, router["b"], NE)
            self.yT = ph.sb("r_yT", [128, 8, 128])
            self.lg = ph.sb("r_lg", [128, NE]); self.top = ph.sb("r_top", [128, 8])
            self.g1 = ph.sb("r_g1", [128, 1]); self.g2 = ph.sb("r_g2", [128, 1]); self.dd = ph.sb("r_dd", [128, 1])
            self.c1 = ph.sb("r_c1", [128, NE]); self.c2 = ph.sb("r_c2", [128, NE])

    def run(self, n, res_ap_fn, res_R, idb, idf, trp, trf, rlp):
        ph, cx = self.ph, self.cx
        k = self.k; self.k += 1
        nbuf = self.nbuf
        s = self.s[k % nbuf]; y = self.y[k % nbuf]; yb = self.yb[k % nbuf]; ya = self.ya[k % nbuf]
        xTs = self.xTs[(n // 4) % nbuf]
        accr = cx.acc_dep[n]
        ph.dma("sp", s[:, :], cx.acc[n * 128:(n + 1) * 128, :], [accr], [s], s)
        if res_ap_fn is not None:
            for h in range(2):
                ph.dve(lambda e, h=h: e.tensor_tensor(out=s[:, h * 512:(h + 1) * 512], in0=res_ap_fn(h * 512, (h + 1) * 512),
                                                      in1=s[:, h * 512:(h + 1) * 512], op=ALU.add), [s] + list(res_R), [s])
        ln_rows(ph, s, self.mv, self.stats, self.rstd, self.nb, D)
        ph.act(lambda e: e.activation(out=y[:, :], in_=s[:, :], func=AF.Identity, bias=self.nb[:, :], scale=self.rstd[:, :]),
               [s, self.nb, self.rstd], [y])
        ph.pool(lambda e: e.tensor_tensor(out=y[:, :], in0=y[:, :], in1=self.grow[:, :], op=ALU.mult), [y, self.grow], [y])
        ph.pool(lambda e: e.tensor_tensor(out=y[:, :], in0=y[:, :], in1=self.brow[:, :], op=ALU.add), [y, self.brow], [y])
        if self.last:
            ph.dma("sp", cx.out[n * 128:(n + 1) * 128, :], y[:, :], [y], [cx.out_dep[n]], y)
            return
        ph.act(lambda e: e.activation(out=ya[:, :], in_=y[:, :], func=AF.Copy, scale=ALPHA), [y], [ya])
        ph.dma("sp", cx.acc[n * 128:(n + 1) * 128, :], ya[:, :], [ya], [accr], ya)
        ph.act(lambda e: e.activation(out=yb[:, :], in_=y[:, :], func=AF.Copy), [y], [yb])
        for c in range(8):
            ph.tr(trp[:, c * 128:(c + 1) * 128], yb[:, c * 128:(c + 1) * 128], idb[:, :], [yb, idb], [trp])
        sub = n % 4
        ph.dve(lambda e: e.tensor_copy(out=xTs[:, :, sub * 128:(sub + 1) * 128], in_=trp[:, :].rearrange("p (c t) -> p c t", c=8)),
               [trp], [xTs])
        if sub == 3:
            t0 = (n // 4) * 512
            ph.dma("sp", cx.xT.rearrange("c p s -> p c s")[:, :, t0:t0 + 512], xTs[:, :, :], [xTs], [cx.xT_dep[n // 4]], xTs)
        if self.router is not None:
            for half in range(2):
                tf = trf[half]
                for c in range(4):
                    cc = half * 4 + c
                    ph.tr(tf[:, c * 128:(c + 1) * 128], y[:, cc * 128:(cc + 1) * 128], idf[:, :], [y, idf], [tf])
                ph.act(lambda e, half=half, tf=tf: e.activation(out=self.yT[:, half * 4:(half + 1) * 4, :],
                                                                in_=tf[:, :].rearrange("p (c t) -> p c t", c=4), func=AF.Copy),
                       [tf], [self.yT])
            for c in range(8):
                ph.mm(rlp[:, 0:NE], self.yT[:, c, :], self.rw[:, c, :], c == 0, c == 7, [self.yT, self.rw], [rlp])
            ph.dve(lambda e: e.tensor_tensor(out=self.lg[:, :], in0=rlp[:, 0:NE], in1=self.rb[:, :], op=ALU.add), [rlp, self.rb], [self.lg])
            ph.dve(lambda e: e.max(out=self.top[:, :], in_=self.lg[:, :]), [self.lg], [self.top])
            ph.dve(lambda e: e.tensor_tensor(out=self.dd[:, :], in0=self.top[:, 0:1], in1=self.top[:, 1:2], op=ALU.subtract), [self.top], [self.dd])
            ph.act(lambda e: e.activation(out=self.g1[:, :], in_=self.dd[:, :], func=AF.Sigmoid), [self.dd], [self.g1])
            ph.dve(lambda e: e.tensor_scalar(out=self.g2[:, :], in0=self.g1[:, :], scalar1=-1.0, scalar2=1.0, op0=ALU.mult, op1=ALU.add),
                   [self.g1], [self.g2])
            ph.dve(lambda e: e.tensor_scalar(out=self.c1[:, :], in0=self.lg[:, :], scalar1=self.top[:, 0:1], scalar2=self.g1[:, :],
                                             op0=ALU.is_equal, op1=ALU.mult), [self.lg, self.top, self.g1], [self.c1])
            ph.dve(lambda e: e.tensor_scalar(out=self.c2[:, :], in0=self.lg[:, :], scalar1=self.top[:, 1:2], scalar2=self.g2[:, :],
                                             op0=ALU.is_equal, op1=ALU.mult), [self.lg, self.top, self.g2], [self.c2])
            ph.dve(lambda e: e.tensor_tensor(out=self.c1[:, :], in0=self.c1[:, :], in1=self.c2[:, :], op=ALU.add), [self.c1, self.c2], [self.c1])
            ph.dma("sp", cx.comb[n * 128:(n + 1) * 128, :], self.c1[:, :], [self.c1], [cx.comb_dep], self.c1)


def load_consts(ph, cx, ncols=None):
    ncols = ncols or cx.NCST
    c = ph.sb("cst", [128, ncols])
    ph.dma("sp", c[:, :], cx.cst[:, 0:ncols], [], [c], c)
    idb = ph.sb("idb", [128, 128], BF16)
    ph.dve(lambda e: e.tensor_copy(out=idb[:, :], in_=c[:, 0:128]), [c], [idb])
    return c, idb


def phase_prologue(nc, cx):
    ph = Phase(nc, "p0")
    mk_deps(ph, cx)
    c, idb = load_consts(ph, cx, 128)
    trp = ph.ps("trp", [128, 1024], BF16)
    xs = [ph.sb("x%d" % i, [128, D]) for i in range(2)]
    xa = [ph.sb("xa%d" % i, [128, D]) for i in range(2)]
    xb = [ph.sb("xb%d" % i, [128, D], BF16) for i in range(2)]
    xTs = [ph.sb("xT%d" % i, [128, 8, 512], BF16) for i in range(2)]
    for n in range(cx.NT):
        x = xs[n % 2]; a = xa[n % 2]; b = xb[n % 2]; xt = xTs[(n // 4) % 2]
        ph.dma("sp", x[:, :], cx.x[n * 128:(n + 1) * 128, :], [], [x], x)
        ph.act(lambda e, x=x, a=a: e.activation(out=a[:, :], in_=x[:, :], func=AF.Copy, scale=ALPHA), [x], [a])
        ph.dma("sp", cx.acc[n * 128:(n + 1) * 128, :], a[:, :], [a], [cx.acc_dep[n]], a)
        ph.dve(lambda e, x=x, b=b: e.tensor_copy(out=b[:, :], in_=x[:, :]), [x], [b])
        for cc in range(8):
            ph.tr(trp[:, cc * 128:(cc + 1) * 128], b[:, cc * 128:(cc + 1) * 128], idb[:, :], [b, idb], [trp])
        sub = n % 4
        ph.dve(lambda e, xt=xt, sub=sub: e.tensor_copy(out=xt[:, :, sub * 128:(sub + 1) * 128],
                                                        in_=trp[:, :].rearrange("p (c t) -> p c t", c=8)), [trp], [xt])
        if sub == 3:
            t0 = (n // 4) * 512
            ph.dma("sp", cx.xT.rearrange("c p s -> p c s")[:, :, t0:t0 + 512], xt[:, :, :], [xt], [cx.xT_dep[n // 4]], xt)
    ph.finish()


def phase_ffn(nc, cx, name, w1s, w3s, w2s, ln_g, ln_b, last, moe):
    ph = Phase(nc, name)
    mk_deps(ph, cx)
    c, idb = load_consts(ph, cx, 128)
    idf = Buf(c.t[:, 0:128], c.d)
    NEXP = len(w1s)
    w1g = [ph.sb("w1g%d" % g, [128, 8, n * 128], BF16) for g, (s0, n) in enumerate(FGROUPS)]
    w3g = [ph.sb("w3g%d" % g, [128, 8, n * 128], BF16) for g, (s0, n) in enumerate(FGROUPS)]
    w2g = [ph.sb("w2g%d" % g, [128, n, D], BF16) for g, (s0, n) in enumerate(FGROUPS)]

    import os
    noroll = bool(os.environ.get("NOROLL"))

    def load_w(e):
        xr = [acct[0]] if (noroll and e > 0) else []
        for g, (s0, n) in enumerate(FGROUPS):
            ph.dma("pool", w1g[g][:, :, :], w1s[e][:, s0 * 128:(s0 + n) * 128].rearrange("(c p) n -> p c n", p=128), xr, [w1g[g]], w1g[g])
            ph.dma("pool", w3g[g][:, :, :], w3s[e][:, s0 * 128:(s0 + n) * 128].rearrange("(c p) n -> p c n", p=128), xr, [w3g[g]], w3g[g])
        for g, (s0, n) in enumerate(FGROUPS):
            ph.dma("pool", w2g[g][:, :, :], w2s[e][s0 * 128:(s0 + n) * 128, :].rearrange("(c p) n -> p c n", p=128), xr, [w2g[g]], w2g[g])

    epi = Epi(ph, cx, ln_g, ln_b, last, nbuf=1)
    xTt = [ph.sb("xTt%d" % i, [128, 8, 512], BF16) for i in range(2)]
    gT = ph.sb("gT", [128, NFF, 512], BF16)
    gdeps = [Buf(gT.t, ph.dep("gT%d" % f)) for f in range(NFF)]
    sg = [ph.sb("sg%d" % i, [128, 512]) for i in range(2)]
    acct = [ph.sb("acct%d" % i, [128, D]) for i in range(1)]
    comb = None
    if moe:
        comb = ph.sb("comb", [128, cx.NT, NE])
        ph.dma("sp", comb[:, :, :], cx.comb.rearrange("(n p) e -> p n e", p=128), [cx.comb_dep], [comb], comb)
    hp = [ph.ps("hp%d" % i, [128, 512]) for i in range(4)]
    op = [ph.ps("op%d" % i, [128, 512]) for i in range(2)]
    trp = ph.ps("trp", [128, 1024], BF16)
    k = 0
    for e in range(NEXP):
        load_w(e)
        for tt in range(cx.NTT):
            xt = xTt[k % 2]
            ph.dma("sp", xt[:, :, :], cx.xT.rearrange("c p s -> p c s")[:, :, tt * 512:(tt + 1) * 512], [cx.xT_dep[tt]], [xt], xt)
            for f in range(NFF):
                g = min(f // 4, 5); fo = (f - FGROUPS[g][0]) * 128
                h1 = hp[(2 * f) % 4]; h3 = hp[(2 * f + 1) % 4]
                for kc in range(8):
                    ph.mm(h1[:, :], w1g[g][:, kc, fo:fo + 128], xt[:, kc, :], kc == 0, kc == 7, [w1g[g], xt], [h1])
                for kc in range(8):
                    ph.mm(h3[:, :], w3g[g][:, kc, fo:fo + 128], xt[:, kc, :], kc == 0, kc == 7, [w3g[g], xt], [h3])
                s = sg[f % 2]
                ph.act(lambda e_, h1=h1, s=s: e_.activation(out=s[:, :], in_=h1[:, :], func=AF.Silu), [h1], [s])
                ph.dve(lambda e_, h3=h3, s=s, f=f: e_.tensor_tensor(out=gT[:, f, :], in0=h3[:, :], in1=s[:, :], op=ALU.mult), [h3, s], [gdeps[f]])
            for sub in range(4):
                n = tt * 4 + sub
                for half in range(2):
                    o = op[half]
                    for f in range(NFF):
                        g = min(f // 4, 5); fi = f - FGROUPS[g][0]
                        ph.mm(o[:, :], gT[:, f, sub * 128:(sub + 1) * 128], w2g[g][:, fi, half * 512:(half + 1) * 512], f == 0, f == NFF - 1,
                              [gdeps[f], w2g[g]], [o])
                if e == NEXP - 1 and not moe:
                    epi.run(n, lambda lo, hi: op[lo // 512][:, :], [op[0], op[1]], idb, idf, trp, None, None)
                else:
                    a = acct[0]
                    ph.dma("sp", a[:, :], cx.acc[n * 128:(n + 1) * 128, :], [cx.acc_dep[n]], [a], a)
                    for half in range(2):
                        if moe:
                            ph.dve(lambda e_, a=a, half=half, n=n, e=e: e_.scalar_tensor_tensor(
                                out=a[:, half * 512:(half + 1) * 512], in0=op[half][:, :], scalar=comb[:, n, e:e + 1],
                                in1=a[:, half * 512:(half + 1) * 512], op0=ALU.mult, op1=ALU.add), [op[half], a, comb], [a])
                        else:
                            ph.dve(lambda e_, a=a, half=half: e_.tensor_tensor(out=a[:, half * 512:(half + 1) * 512], in0=op[half][:, :],
                                                                            in1=a[:, half * 512:(half + 1) * 512], op=ALU.add), [op[half], a], [a])
                    if e == NEXP - 1:
                        ph.dma("sp", cx.acc[n * 128:(n + 1) * 128, :], a[:, :], [a], [cx.acc_dep[n]], a)
                        epi.run(n, None, [], idb, idf, trp, None, None)
                    else:
                        ph.dma("sp", cx.acc[n * 128:(n + 1) * 128, :], a[:, :], [a], [cx.acc_dep[n]], a)
            k += 1
    ph.finish()


CST_ID, CST_ONES, CST_MTNEG, CST_M01S, CST_MT01, CST_SEL, CST_SC01, CST_SCNEG, NCST = 0, 128, 256, 384, 512, 640, 1152, 1664, 2176
PE_CW, PE_CB, PE_AG, PE_AB, PE_IB, PE_FB, NPE = 0, 124, 128, 132, 136, 137, 138


def phase_even_mixer(nc, cx, name, j):
    ph = Phase(nc, name)
    mk_deps(ph, cx)
    c, idb = load_consts(ph, cx)
    cd = [c]
    idf = Buf(c.t[:, 0:128], c.d)
    ones = c.t[:, CST_ONES:CST_ONES + 128]
    win = ph.sb("win", [128, 8, 3080], BF16)
    ph.dma("pool", win[:, :, :], cx.ab_w_in[j].rearrange("(c p) n -> p c n", p=128), [], [win], win)
    wout = ph.sb("wout", [128, 8, D], BF16)
    ph.dma("pool", wout[:, :, :], cx.ab_w_out[j].rearrange("(c p) n -> p c n", p=128), [], [wout], wout)
    pp = ph.sb("pp", [128, NPE])
    ph.dma("sp", pp[:, :], cx.pe[j], [], [pp], pp)
    nfb = ph.sb("nfb", [128, 1])
    ph.dve(lambda e: e.tensor_scalar(out=nfb[:, :], in0=pp[:, PE_FB:PE_FB + 1], scalar1=-1.0, scalar2=None, op0=ALU.mult), [pp], [nfb])
    hg = ph.sb("hg", [128, 512]); load_rows(ph, "sp", hg, cx.rows[cx.row_idx[("hg", j)]:cx.row_idx[("hg", j)] + 1, 0:512], 512)
    epi = Epi(ph, cx, cx.rows[cx.row_idx[("eln1g", j)]:cx.row_idx[("eln1g", j)] + 1, :], cx.rows[cx.row_idx[("eln1b", j)]:cx.row_idx[("eln1b", j)] + 1, :], False, nbuf=1)
    pj = [ph.ps("pj%d" % i, [128, 512]) for i in range(2)]
    pm = ph.ps("pm", [128, 512]); pm_st = pm; pm_ub = pm; pm_cols = pm; pm_sc = pm
    pn = ph.ps("pn", [128, 512]); pn_num = pn; pn_int = pn; pn_kv = pn
    pgi = ph.ps("pgi", [128, 512]); pgf = ph.ps("pgf", [128, 512])
    trp = ph.ps("trp", [128, 1024], BF16)
    pn2 = ph.ps("pn2", [128, 512])
    pnb = [pn, pn2, pgi, pgf]
    xTt = [ph.sb("xTt%d" % i, [128, 8, 512], BF16) for i in range(1)]
    glu = ph.sb("glu", [128, 4, 542])
    ph.dve(lambda e: e.memset(glu[:, :, 0:30], 0.0), [], [glu])
    sig = ph.sb("sig", [128, 512])
    cy = ph.sb("cy", [128, 4, 512]); sq = sig
    cyd = [Buf(cy.t, ph.dep("cy%d" % ch)) for ch in range(4)]
    mean = ph.sb("mean", [128, 512]); var = ph.sb("var", [128, 512]); tmpa = sig
    mixT = ph.sb("mixT", [128, 8, 512], BF16)
    qT = ph.sb("qT", [128, 4, 512], BF16); kT = ph.sb("kT", [128, 4, 512], BF16)
    ktok = ph.sb("ktok", [128, 4, 512], BF16)
    vext = ph.sb("vext", [128, 4, 4, 132], BF16)
    ph.dve(lambda e: e.memset(vext[:, :, :, 128:129], 1.0), [], [vext])
    osig = ph.sb("osig", [128, 4, 512])
    ig = ph.sb("ig", [128, 512]); sp_ = ph.sb("sp", [128, 512]); Bc = ph.sb("Bc", [128, 512]); lw = sp_
    Mx = ph.sb("Mx", [128, 512]); U = ph.sb("U", [128, 512]); rowsT = ph.sb("rowsT", [128, 512]); tmpr = ig
    ph.dve(lambda e: e.memset(rowsT[:, :], 0.0), [], [rowsT])
    ph.dve(lambda e: e.memset(U[:, :], 0.0), [], [U])
    am = ph.sb("am", [128, 4]); nam = ph.sb("nam", [128, 4]); mp = ph.sb("mp", [128, 5]); sv = ph.sb("sv", [128, 4, 2]); t1 = ph.sb("t1", [128, 1]); t2 = ph.sb("t2", [128, 1])
    ph.dve(lambda e: e.memset(sv[:, :, :], 0.0), [], [sv])
    mcar = ph.sb("mcar", [128, 1])
    ph.dve(lambda e: e.memset(mcar[:, :], 0.0), [], [mcar])
    cols = ph.sb("cols", [128, 4, 128]); scol = ph.sb("scol", [128, 4, 128])
    vt = ph.sb("vt", [128, 512]); rows2 = ph.sb("rows2", [128, 512]); ax = ph.sb("ax", [128, 4]); nax = ph.sb("nax", [128, 4])
    ph.dve(lambda e: e.memset(rows2[:, :], 0.0), [], [rows2])
    wgi = ph.sb("wgi", [128, 8, 128], BF16); wgf = ph.sb("wgf", [128, 8, 128], BF16)
    ph.dve(lambda e: e.memset(wgi[:, :, :], 0.0), [], [wgi])
    ph.dve(lambda e: e.memset(wgf[:, :, :], 0.0), [], [wgf])
    for g in range(4):
        ph.dve(lambda e, g=g: e.tensor_copy(out=wgi[:, :, 32 * g:32 * g + 4], in_=win[:, :, 3072:3076]), [win], [wgi])
        ph.dve(lambda e, g=g: e.tensor_copy(out=wgf[:, :, 32 * g:32 * g + 4], in_=win[:, :, 3076:3080]), [win], [wgf])
    Cf = ph.sb("Cf", [128, 4, 132]); Cb = ph.sb("Cb", [128, 4, 132], BF16)
    ph.dve(lambda e: e.memset(Cf[:, :, :], 0.0), [], [Cf])
    ph.dve(lambda e: e.memset(Cb[:, :, :], 0.0), [], [Cb])
    PTs = [ph.sb("PT%d" % h, [128, 128], BF16) for h in range(4)]
    tis = [ph.sb("ti%d" % h, [128, 129]) for h in range(4)]; tots = [ph.sb("tot%d" % h, [128, 129]) for h in range(4)]
    dd4 = ph.sb("dd4", [128, 4]); rec4 = ph.sb("rec4", [128, 4])
    kws = [ph.sb("kw%d" % h, [128, 128], BF16) for h in range(4)]
    vscs = [ph.sb("vsc%d" % h, [128, 132], BF16) for h in range(4)]
    Cfd = [Buf(None, ph.dep("Cf%d" % h)) for h in range(4)]
    ph.dve(lambda e: e.memset(Cf[:, :, :], 0.0), [Cf], Cfd)
    hraw = ph.sb("hraw", [128, 512]); hst = ph.sb("hst", [128, 4, 6]); hmv = ph.sb("hmv", [128, 4, 2]); hrs = ph.sb("hrs", [128, 4]); hb = ph.sb("hb", [128, 512], BF16)
    sc01 = c.t[:, CST_SC01:CST_SC01 + 512]; scneg = c.t[:, CST_SCNEG:CST_SCNEG + 512]
    k128 = 128 ** -0.5

    def proj_fm(col0, dst_fn, pjk):
        p = pj[pjk % 2]
        for kc in range(8):
            ph.mm(p[:, :], win[:, kc, col0:col0 + 128], xt[:, kc, :], kc == 0, kc == 7, [win, xt], [p])
        dst_fn(p)

    kpj = [0]
    import os
    emstop = int(os.environ.get("EMSTOP", "99"))
    for tt in range(cx.NTT if emstop > 0 else 0):
        xt = xTt[0]
        ph.dma("sp", xt[:, :, :], cx.xT.rearrange("c p s -> p c s")[:, :, tt * 512:(tt + 1) * 512], [cx.xT_dep[tt]], [xt], xt)
        for ch in range(4):
            p = pj[kpj[0] % 2]; kpj[0] += 1
            for kc in range(8):
                ph.mm(p[:, :], win[:, kc, 512 + ch * 128:512 + (ch + 1) * 128], xt[:, kc, :], kc == 0, kc == 7, [win, xt], [p])
            ph.act(lambda e, p=p: e.activation(out=sig[:, :], in_=p[:, :], func=AF.Sigmoid), [p], [sig])
            p2 = pj[kpj[0] % 2]; kpj[0] += 1
            for kc in range(8):
                ph.mm(p2[:, :], win[:, kc, ch * 128:(ch + 1) * 128], xt[:, kc, :], kc == 0, kc == 7, [win, xt], [p2])
            ph.dve(lambda e, p2=p2, ch=ch: e.tensor_tensor(out=glu[:, ch, 30:542], in0=p2[:, :], in1=sig[:, :], op=ALU.mult), [p2, sig], [glu])
        for ch in range(4):
            ph.dve(lambda e, ch=ch: e.tensor_scalar(out=cy[:, ch, :], in0=glu[:, ch, 0:512], scalar1=pp[:, PE_CW + ch * 31:PE_CW + ch * 31 + 1],
                                                    scalar2=pp[:, PE_CB + ch:PE_CB + ch + 1], op0=ALU.mult, op1=ALU.add), [glu, pp], [cyd[ch]])
        for jj in range(1, 31):
            for ch in range(4):
                ph.dve(lambda e, ch=ch, jj=jj: e.scalar_tensor_tensor(out=cy[:, ch, :], in0=glu[:, ch, jj:jj + 512],
                                                                     scalar=pp[:, PE_CW + ch * 31 + jj:PE_CW + ch * 31 + jj + 1],
                                                                     in1=cy[:, ch, :], op0=ALU.mult, op1=ALU.add), [glu, pp, cyd[ch]], [cyd[ch]])
        for ch in range(4):
            ph.pool(lambda e, ch=ch: e.tensor_copy(out=glu[:, ch, 0:30], in_=glu[:, ch, 512:542]), [glu] + cyd, [glu])
        if emstop <= 1:
            continue
        for ch in range(4):
            ph.mm(pgi[:, :], ones, cy[:, ch, :], ch == 0, ch == 3, [cyd[ch]] + cd, [pgi])
        for ch in range(4):
            ph.act(lambda e, ch=ch: e.activation(out=sq[:, :], in_=cy[:, ch, :], func=AF.Square), [cyd[ch]], [sq])
            ph.mm(pgf[:, :], ones, sq[:, :], ch == 0, ch == 3, [sq] + cd, [pgf])
        ph.act(lambda e: e.activation(out=mean[:, :], in_=pgi[:, :], func=AF.Copy, scale=1.0 / 512), [pgi], [mean])
        ph.dve(lambda e: e.tensor_tensor(out=tmpa[:, :], in0=mean[:, :], in1=mean[:, :], op=ALU.mult), [mean], [tmpa])
        ph.dve(lambda e: e.scalar_tensor_tensor(out=var[:, :], in0=pgf[:, :], scalar=1.0 / 512, in1=tmpa[:, :], op0=ALU.mult, op1=ALU.subtract),
               [pgf, tmpa], [var])
        ph.dve(lambda e: e.tensor_scalar(out=var[:, :], in0=var[:, :], scalar1=EPS, scalar2=None, op0=ALU.add), [var], [var])
        ph.act(lambda e: e.activation(out=var[:, :], in_=var[:, :], func=AF.Sqrt), [var], [var])
        ph.dve(lambda e: e.reciprocal(out=var[:, :], in_=var[:, :]), [var], [var])
        for ch in range(4):
            ph.dve(lambda e, ch=ch: e.tensor_tensor(out=cy[:, ch, :], in0=cy[:, ch, :], in1=mean[:, :], op=ALU.subtract), [cyd[ch], mean], [cyd[ch]])
            ph.dve(lambda e, ch=ch: e.tensor_tensor(out=cy[:, ch, :], in0=cy[:, ch, :], in1=var[:, :], op=ALU.mult), [cyd[ch], var], [cyd[ch]])
            ph.act(lambda e, ch=ch: e.activation(out=mixT[:, ch, :], in_=cy[:, ch, :], func=AF.Silu, bias=pp[:, PE_AB + ch:PE_AB + ch + 1],
                                                 scale=pp[:, PE_AG + ch:PE_AG + ch + 1]), [cyd[ch], pp], [mixT])
        if emstop <= 2:
            continue
        for h in range(4):
            p = pj[kpj[0] % 2]; kpj[0] += 1
            for kc in range(8):
                ph.mm(p[:, :], win[:, kc, 1024 + h * 128:1024 + (h + 1) * 128], xt[:, kc, :], kc == 0, kc == 7, [win, xt], [p])
            ph.act(lambda e, p=p, h=h: e.activation(out=qT[:, h, :], in_=p[:, :], func=AF.Copy), [p], [qT])
            p = pj[kpj[0] % 2]; kpj[0] += 1
            for kc in range(8):
                ph.mm(p[:, :], win[:, kc, 1536 + h * 128:1536 + (h + 1) * 128], xt[:, kc, :], kc == 0, kc == 7, [win, xt], [p])
            ph.act(lambda e, p=p, h=h: e.activation(out=kT[:, h, :], in_=p[:, :], func=AF.Copy, scale=k128), [p], [kT])
        for sub in range(4):
            p = pj[kpj[0] % 2]; kpj[0] += 1
            for kc in range(8):
                ph.mm(p[:, :], xt[:, kc, sub * 128:(sub + 1) * 128], win[:, kc, 1536:2048], kc == 0, kc == 7, [win, xt], [p])
            ph.act(lambda e, p=p, sub=sub: e.activation(out=ktok[:, sub, :], in_=p[:, :], func=AF.Copy, scale=k128), [p], [ktok])
            p = pj[kpj[0] % 2]; kpj[0] += 1
            for kc in range(8):
                ph.mm(p[:, :], xt[:, kc, sub * 128:(sub + 1) * 128], win[:, kc, 2048:2560], kc == 0, kc == 7, [win, xt], [p])
            ph.dve(lambda e, p=p, sub=sub: e.tensor_copy(out=vext[:, sub, :, 0:128], in_=p[:, :].rearrange("p (h d) -> p h d", h=4)), [p], [vext])
            p = pj[kpj[0] % 2]; kpj[0] += 1
            for kc in range(8):
                ph.mm(p[:, :], xt[:, kc, sub * 128:(sub + 1) * 128], win[:, kc, 2560:3072], kc == 0, kc == 7, [win, xt], [p])
            ph.act(lambda e, p=p, sub=sub: e.activation(out=osig[:, sub, :], in_=p[:, :], func=AF.Sigmoid), [p], [osig])
        for kc in range(8):
            ph.mm(pgi[:, :], wgi[:, kc, :], xt[:, kc, :], kc == 0, kc == 7, [wgi, xt], [pgi])
        for kc in range(8):
            ph.mm(pgf[:, :], wgf[:, kc, :], xt[:, kc, :], kc == 0, kc == 7, [wgf, xt], [pgf])
        if emstop <= 3:
            continue
        G = [slice(32 * g, 32 * g + 4) for g in range(4)]
        ph.act(lambda e: e.activation(out=ig[:, :], in_=pgi[:, :], func=AF.Identity, bias=pp[:, PE_IB:PE_IB + 1]), [pgi, pp], [ig])
        ph.act(lambda e: e.activation(out=sp_[:, :], in_=pgf[:, :], func=AF.Exp, bias=nfb[:, :], scale=-1.0), [pgf, nfb], [sp_])
        ph.act(lambda e: e.activation(out=sp_[:, :], in_=sp_[:, :], func=AF.Ln, bias=1.0), [sp_], [sp_])
        ph.dve(lambda e: e.tensor_tensor_scan(out=Bc[:, :], data0=sc01, data1=sp_[:, :], initial=0.0, op0=ALU.mult, op1=ALU.add), [sp_] + cd, [Bc])
        ph.dve(lambda e: e.tensor_tensor(out=vt[:, :], in0=ig[:, :], in1=Bc[:, :], op=ALU.add), [ig, Bc], [vt])
        ph.dve(lambda e: e.tensor_tensor_scan(out=Mx[:, :], data0=scneg, data1=vt[:, :], initial=NEG, op0=ALU.add, op1=ALU.max), [vt] + cd, [Mx])
        ph.dve(lambda e: e.tensor_copy(out=ax[:, :], in_=Mx[:, :].rearrange("p (c t) -> p c t", c=4)[:, :, 127]), [Mx], [ax])
        ph.dve(lambda e: e.tensor_scalar(out=nax[:, :], in0=ax[:, :], scalar1=-1.0, scalar2=None, op0=ALU.mult), [ax], [nax])
        for cch in range(4):
            cs = slice(cch * 128, (cch + 1) * 128)
            ph.act(lambda e, cs=cs, cch=cch: e.activation(out=rowsT[G[0], cs], in_=vt[G[0], cs], func=AF.Exp, bias=nax[G[0], cch:cch + 1]), [vt, nax], [rowsT])
        for cch in range(4):
            cs = slice(cch * 128, (cch + 1) * 128)
            ph.dve(lambda e, cs=cs, cch=cch: e.tensor_scalar(out=lw[:, cs], in0=vt[:, cs], scalar1=Bc[:, cch * 128 + 127:cch * 128 + 128], scalar2=None,
                                                            op0=ALU.subtract), [vt, Bc], [lw])
        ph.dve(lambda e: e.tensor_reduce(out=am[:, :], in_=lw[:, :].rearrange("p (c t) -> p c t", c=4), axis=AX.X, op=ALU.max), [lw], [am])
        ph.dve(lambda e: e.tensor_scalar(out=nam[:, :], in0=am[:, :], scalar1=-1.0, scalar2=None, op0=ALU.mult), [am], [nam])
        for cch in range(4):
            cs = slice(cch * 128, (cch + 1) * 128)
            ph.act(lambda e, cs=cs, cch=cch: e.activation(out=rowsT[G[1], cs], in_=lw[G[1], cs], func=AF.Exp, bias=nam[G[1], cch:cch + 1]), [lw, nam], [rowsT])
        ph.dve(lambda e: e.tensor_copy(out=mp[:, 0:1], in_=mcar[:, :]), [mcar], [mp])
        for cch in range(4):
            be = Bc[:, cch * 128 + 127:cch * 128 + 128]
            ph.dve(lambda e, cch=cch, be=be: e.tensor_tensor(out=t1[:, :], in0=mp[:, cch:cch + 1], in1=be, op=ALU.subtract), [mp, Bc], [t1])
            ph.dve(lambda e, cch=cch: e.tensor_tensor(out=mp[:, cch + 1:cch + 2], in0=t1[:, :], in1=am[:, cch:cch + 1], op=ALU.max), [t1, am], [mp])
            ph.dve(lambda e, cch=cch: e.tensor_tensor(out=t1[:, :], in0=t1[:, :], in1=mp[:, cch + 1:cch + 2], op=ALU.subtract), [t1, mp], [t1])
            ph.dve(lambda e, cch=cch: e.tensor_tensor(out=t2[:, :], in0=am[:, cch:cch + 1], in1=mp[:, cch + 1:cch + 2], op=ALU.subtract), [am, mp], [t2])
            ph.act(lambda e, cch=cch: e.activation(out=sv[:, cch, 0:1], in_=t1[:, :], func=AF.Exp), [t1], [sv])
            ph.act(lambda e, cch=cch: e.activation(out=sv[:, cch, 1:2], in_=t2[:, :], func=AF.Exp), [t2], [sv])
        ph.dve(lambda e: e.tensor_copy(out=mcar[:, :], in_=mp[:, 4:5]), [mp], [mcar])
        for cch in range(4):
            cs = slice(cch * 128, (cch + 1) * 128)
            ph.dve(lambda e, cs=cs, cch=cch: e.tensor_scalar(out=U[:, cs], in0=Mx[:, cs], scalar1=mp[:, cch:cch + 1], scalar2=-1.0, op0=ALU.max, op1=ALU.mult),
                   [Mx, mp], [U])
            ph.act(lambda e, cs=cs, cch=cch: e.activation(out=rowsT[G[2], cs], in_=U[G[2], cs], func=AF.Exp, bias=mp[G[2], cch:cch + 1]), [U, mp], [rowsT])
            ph.act(lambda e, cs=cs, cch=cch: e.activation(out=rows2[G[2], cs], in_=U[G[2], cs], func=AF.Exp, bias=ax[G[2], cch:cch + 1]), [U, ax], [rows2])
            ph.dve(lambda e, cs=cs, cch=cch: e.tensor_scalar(out=rows2[G[0], cs], in0=c.t[G[0], CST_ONES:CST_ONES + 128], scalar1=sv[G[0], cch, 0:1], scalar2=None,
                                                            op0=ALU.mult), [sv] + cd, [rows2])
            ph.dve(lambda e, cs=cs, cch=cch: e.tensor_scalar(out=rows2[G[1], cs], in0=c.t[G[1], CST_ONES:CST_ONES + 128], scalar1=sv[G[1], cch, 1:2], scalar2=None,
                                                            op0=ALU.mult), [sv] + cd, [rows2])
        ph.dve(lambda e: e.tensor_tensor(out=tmpr[:, :], in0=Bc[:, :], in1=U[:, :], op=ALU.add), [Bc, U], [tmpr])
        ph.act(lambda e: e.activation(out=rowsT[G[3], :], in_=tmpr[G[3], :], func=AF.Exp), [tmpr], [rowsT])
        if emstop <= 4:
            continue
        for sub in range(4):
            ph.tr(pm[:, 256:384], rowsT[:, sub * 128:(sub + 1) * 128], idf[:, :], [rowsT, idf], [pm_cols])
            ph.act(lambda e, sub=sub: e.activation(out=cols[:, sub, :], in_=pm[:, 256:384], func=AF.Copy), [pm_cols], [cols])
            ph.tr(pm[:, 384:512], rows2[:, sub * 128:(sub + 1) * 128], idf[:, :], [rows2, idf], [pm_sc])
            ph.act(lambda e, sub=sub: e.activation(out=scol[:, sub, :], in_=pm[:, 384:512], func=AF.Copy), [pm_sc], [scol])
        if emstop <= 5:
            continue
        hstop = int(os.environ.get("HSTOP", "99"))
        for sub in range(4):
            cs = slice(sub * 128, (sub + 1) * 128)
            H4 = range(4)
            for h in H4:
                ph.mm(pm[:, h * 128:(h + 1) * 128], kT[:, h, cs], qT[:, h, cs], True, True, [kT, qT], [pm])
            for h in H4:
                ph.dve(lambda e, h=h: e.tensor_tensor(out=PTs[h][:, :], in0=pm[:, h * 128:(h + 1) * 128], in1=c.t[:, CST_MT01:CST_MT01 + 128], op=ALU.mult), [pm] + cd, [PTs[h]])
                ph.pool(lambda e, sub=sub, h=h: e.tensor_scalar(out=vscs[h][:, 0:130], in0=vext[:, sub, h, 0:130], scalar1=cols[:, sub, h:h + 1], scalar2=None, op0=ALU.mult),
                        [vext, cols], [vscs[h]])
            for h in H4:
                ph.mm(pnb[h][:, 0:129], PTs[h][:, :], vscs[h][:, 0:129], True, True, [PTs[h], vscs[h]], [pnb[h]])
                ph.mm(pnb[h][:, 129:258], qT[:, h, cs], Cb[:, h, 0:129], True, True, [qT, Cb], [pnb[h]])
            for h in H4:
                ph.act(lambda e, sub=sub, h=h: e.activation(out=tis[h][:, :], in_=pnb[h][:, 129:258], func=AF.Copy, scale=cols[:, sub, 64 + h:64 + h + 1]), [pnb[h], cols], [tis[h]])
            for h in H4:
                ph.dve(lambda e, sub=sub, h=h: e.scalar_tensor_tensor(out=tots[h][:, :], in0=pnb[h][:, 0:129], scalar=scol[:, sub, 64 + h:64 + h + 1], in1=tis[h][:, :],
                                                                      op0=ALU.mult, op1=ALU.add), [pnb[h], tis[h], scol], [tots[h]])
            for h in H4:
                ph.act(lambda e, h=h: e.activation(out=dd4[:, h:h + 1], in_=tots[h][:, 128:129], func=AF.Abs), [tots[h]], [dd4])
            ph.dve(lambda e, sub=sub: e.tensor_tensor(out=dd4[:, :], in0=dd4[:, :], in1=cols[:, sub, 96:100], op=ALU.max), [dd4, cols], [dd4])
            ph.dve(lambda e: e.reciprocal(out=rec4[:, :], in_=dd4[:, :]), [dd4], [rec4])
            for h in H4:
                ph.dve(lambda e, h=h: e.tensor_scalar(out=hraw[:, h * 128:(h + 1) * 128], in0=tots[h][:, 0:128], scalar1=rec4[:, h:h + 1], scalar2=None, op0=ALU.mult),
                       [tots[h], rec4], [hraw])
            for h in H4:
                ph.dve(lambda e, sub=sub, h=h: e.tensor_scalar(out=kws[h][:, :], in0=ktok[:, sub, h * 128:(h + 1) * 128], scalar1=cols[:, sub, 32 + h:32 + h + 1],
                                                               scalar2=None, op0=ALU.mult), [ktok, cols], [kws[h]])
            for h in H4:
                ph.mm(pnb[h][:, 258:387], kws[h][:, :], vext[:, sub, h, 0:129], True, True, [kws[h], vext], [pnb[h]])
            for h in H4:
                ph.dve(lambda e, sub=sub, h=h: e.tensor_scalar(out=Cf[:, h, 0:129], in0=Cf[:, h, 0:129], scalar1=scol[:, sub, h:h + 1], scalar2=None, op0=ALU.mult),
                       [Cfd[h], scol], [Cfd[h]])
                ph.dve(lambda e, sub=sub, h=h: e.scalar_tensor_tensor(out=Cf[:, h, 0:129], in0=pnb[h][:, 258:387], scalar=scol[:, sub, 32 + h:32 + h + 1], in1=Cf[:, h, 0:129],
                                                                      op0=ALU.mult, op1=ALU.add), [pnb[h], scol, Cfd[h]], [Cfd[h]])
            for h in H4:
                ph.act(lambda e, h=h: e.activation(out=Cb[:, h, 0:129], in_=Cf[:, h, 0:129], func=AF.Copy), [Cfd[h]], [Cb])
            if hstop <= 4:
                continue
            for h in range(4):
                ph.dve(lambda e, h=h: e.bn_stats(out=hst[:, h, :], in_=hraw[:, h * 128:(h + 1) * 128]), [hraw], [hst])
                ph.dve(lambda e, h=h: e.bn_aggr(out=hmv[:, h, :], in_=hst[:, h, :]), [hst], [hmv])
            ph.dve(lambda e: e.tensor_scalar(out=hrs[:, :], in0=hmv[:, :, 1], scalar1=EPS, scalar2=None, op0=ALU.add), [hmv], [hrs])
            ph.act(lambda e: e.activation(out=hrs[:, :], in_=hrs[:, :], func=AF.Sqrt), [hrs], [hrs])
            ph.dve(lambda e: e.reciprocal(out=hrs[:, :], in_=hrs[:, :]), [hrs], [hrs])
            for h in range(4):
                ph.dve(lambda e, h=h: e.tensor_scalar(out=hraw[:, h * 128:(h + 1) * 128], in0=hraw[:, h * 128:(h + 1) * 128], scalar1=hmv[:, h, 0:1],
                                                      scalar2=hrs[:, h:h + 1], op0=ALU.subtract, op1=ALU.mult), [hraw, hmv, hrs], [hraw])
            ph.pool(lambda e: e.tensor_tensor(out=hraw[:, :], in0=hraw[:, :], in1=hg[:, :], op=ALU.mult), [hraw, hg], [hraw])
            ph.dve(lambda e, sub=sub: e.tensor_tensor(out=hb[:, :], in0=hraw[:, :], in1=osig[:, sub, :], op=ALU.mult), [hraw, osig], [hb])
            for h in range(4):
                ph.tr(trp[:, h * 128:(h + 1) * 128], hb[:, h * 128:(h + 1) * 128], idb[:, :], [hb, idb], [trp])
            ph.act(lambda e, cs=cs: e.activation(out=mixT[:, 4:8, cs], in_=trp[:, 0:512].rearrange("p (c t) -> p c t", c=4), func=AF.Copy), [trp], [mixT])
        if emstop <= 6:
            continue
        for sub in range(4):
            n = tt * 4 + sub
            for half in range(2):
                for kc in range(8):
                    ph.mm(pj[half][:, :], mixT[:, kc, sub * 128:(sub + 1) * 128], wout[:, kc, half * 512:(half + 1) * 512], kc == 0, kc == 7, [mixT, wout], [pj[half]])
            epi.run(n, lambda lo, hi: pj[lo // 512][:, :], [pj[0], pj[1]], idb, idf, trp, None, None)
    ph.finish()


def phase_odd_mixer(nc, cx, name, j):
    ph = Phase(nc, name)
    mk_deps(ph, cx)
    c, idb = load_consts(ph, cx, 640)
    cd = [c]
    idf = Buf(c.t[:, 0:128], c.d)
    S = cx.S
    win = ph.sb("win", [128, 8, 2560], BF16)
    ph.dma("pool", win[:, :, :], cx.cd_w_in[j].rearrange("(c p) n -> p c n", p=128), [], [win], win)
    wout = ph.sb("wout", [128, 8, D], BF16)
    ph.dma("pool", wout[:, :, :], cx.cd_w_out[j].rearrange("(c p) n -> p c n", p=128), [], [wout], wout)
    ri = cx.row_idx
    cng = ph.sb("cng", [128, 512]); load_rows(ph, "sp", cng, cx.rows[ri[("cng", j)]:ri[("cng", j)] + 1, 0:512], 512)
    cnb = ph.sb("cnb", [128, 512]); load_rows(ph, "sp", cnb, cx.rows[ri[("cnb", j)]:ri[("cnb", j)] + 1, 0:512], 512)
    bsb = ph.sb("bsb", [128, 512]); load_rows(ph, "sp", bsb, cx.rows[ri[("bs", j)]:ri[("bs", j)] + 1, 0:512], 512)
    wsf = ph.sb("wsf", [128, 4, 128])
    ph.dma("sp", wsf[:, :, :], cx.wst[j].rearrange("g s t -> s g t"), [], [wsf], wsf)
    WcT = ph.sb("WcT", [128, 4, 128], BF16)
    for g in range(4):
        ph.dve(lambda e, g=g: e.tensor_tensor(out=WcT[:, g, :], in0=wsf[:, g, :], in1=c.t[:, CST_MT01:CST_MT01 + 128], op=ALU.mult), [wsf] + cd, [WcT])
    epi = Epi(ph, cx, cx.rows[ri[("oln1g", j)]:ri[("oln1g", j)] + 1, :], cx.rows[ri[("oln1b", j)]:ri[("oln1b", j)] + 1, :], False,
              router={"w": cx.router_w[j], "b": cx.rows[ri[("rb", j)]:ri[("rb", j)] + 1, 0:NE]}, nbuf=1)
    kTres = ph.sb("kTres", [128, 4, S], BF16)
    Vres = ph.sb("Vres", [128, cx.NT, 512], BF16)
    pj = [ph.ps("pj%d" % i, [128, 512]) for i in range(2)]
    pz = [ph.ps("pz%d" % i, [128, 512]) for i in range(2)]
    pg = ph.ps("pg", [128, 512]); pos = [ph.ps("po%d" % i, [128, 512]) for i in range(2)]
    trp = ph.ps("trp", [128, 1024], BF16)
    xt = ph.sb("xTt", [128, 8, 512], BF16)
    uT = ph.sb("uT", [128, 4, 512], BF16)
    zf = ph.sb("zf", [128, 512]); zb = ph.sb("zb", [128, 512], BF16)
    zst = ph.sb("zst", [128, 1, 6]); zmv = ph.sb("zmv", [128, 2]); zrs = ph.sb("zrs", [128, 1]); znb = ph.sb("znb", [128, 1])
    gtmp = zf
    mixT = ph.sb("mixT", [128, 8, 512], BF16)
    qT = ph.sb("qT", [128, 4, 512], BF16)
    NB = 2
    ebuf = [ph.sb("ebuf%d" % i, [128, 512]) for i in range(NB)]
    spb = [ph.sb("spb%d" % i, [128, 513]) for i in range(NB)]
    csb = [ph.sb("csb%d" % i, [128, 513]) for i in range(NB)]
    attb = [ph.sb("attb%d" % i, [128, 512], BF16) for i in range(NB)]
    attT = ph.sb("attT", [128, 2, 4, 128], BF16)
    for i in range(NB):
        ph.dve(lambda e, i=i: e.memset(spb[i][:, 0:1], 0.0), [], [spb[i]])
    cars = [ph.sb("car%d" % i, [128, 1]) for i in range(NB)]
    nbb = [ph.sb("nbb%d" % i, [128, 1]) for i in range(NB)]
    dout = ph.sb("dout", [128, 512], BF16)
    onesr = c.t[:, CST_ONES:CST_ONES + 128]
    ones513 = ph.sb("ones513", [128, 513], BF16)
    ph.dve(lambda e: e.memset(ones513[:, :], 1.0), [], [ones513])
    m01s = c.t[:, CST_M01S:CST_M01S + 128]
    kpj = [0]
    kz = [0]
    import os
    for tt in range(int(os.environ.get("OMT", cx.NTT))):
        t0 = tt * 512
        ph.dma("sp", xt[:, :, :], cx.xT.rearrange("c p s -> p c s")[:, :, t0:t0 + 512], [cx.xT_dep[tt]], [xt], xt)
        for ch in range(4):
            p = pj[kpj[0] % 2]; kpj[0] += 1
            for kc in range(8):
                ph.mm(p[:, :], win[:, kc, ch * 128:(ch + 1) * 128], xt[:, kc, :], kc == 0, kc == 7, [win, xt], [p])
            ph.act(lambda e, p=p, ch=ch: e.activation(out=uT[:, ch, :], in_=p[:, :], func=AF.Gelu), [p], [uT])
        for pr in range(4):
            p = pj[kpj[0] % 2]; kpj[0] += 1
            for kc in range(8):
                ph.mm(p[:, :], win[:, kc, 1024 + pr * 128:1024 + (pr + 1) * 128], xt[:, kc, :], kc == 0, kc == 7, [win, xt], [p])
            ph.act(lambda e, p=p, pr=pr: e.activation(out=qT[:, pr, :], in_=p[:, :], func=AF.Copy, scale=0.125), [p], [qT])
            p = pj[kpj[0] % 2]; kpj[0] += 1
            for kc in range(8):
                ph.mm(p[:, :], win[:, kc, 1536 + pr * 128:1536 + (pr + 1) * 128], xt[:, kc, :], kc == 0, kc == 7, [win, xt], [p])
            ph.dve(lambda e, p=p, pr=pr, t0=t0: e.tensor_copy(out=kTres[:, pr, t0:t0 + 512], in_=p[:, :]), [p], [kTres])
        for sub in range(4):
            n = tt * 4 + sub
            cs = slice(sub * 128, (sub + 1) * 128)
            p = pj[kpj[0] % 2]; kpj[0] += 1
            for kc in range(8):
                ph.mm(p[:, :], xt[:, kc, cs], win[:, kc, 2048:2560], kc == 0, kc == 7, [win, xt], [p])
            ph.act(lambda e, p=p, n=n: e.activation(out=Vres[:, n, :], in_=p[:, :], func=AF.Copy), [p], [Vres])
            p = pj[kpj[0] % 2]; kpj[0] += 1
            for kc in range(8):
                ph.mm(p[:, :], xt[:, kc, cs], win[:, kc, 512:1024], kc == 0, kc == 7, [win, xt], [p])
            ph.act(lambda e, p=p: e.activation(out=zf[:, :], in_=p[:, :], func=AF.Gelu), [p], [zf])
            ln_rows(ph, zf, zmv, zst, zrs, znb, 512)
            ph.act(lambda e: e.activation(out=zf[:, :], in_=zf[:, :], func=AF.Identity, bias=znb[:, :], scale=zrs[:, :]), [zf, znb, zrs], [zf])
            ph.pool(lambda e: e.tensor_tensor(out=zf[:, :], in0=zf[:, :], in1=cng[:, :], op=ALU.mult), [zf, cng], [zf])
            ph.pool(lambda e: e.tensor_tensor(out=zb[:, :], in0=zf[:, :], in1=cnb[:, :], op=ALU.add), [zf, cnb], [zb])
            for g in range(4):
                ph.mm(pg[:, g * 128:(g + 1) * 128], zb[:, g * 128:(g + 1) * 128], WcT[:, g, :], True, True, [zb, WcT], [pg])
            ph.dve(lambda e: e.tensor_tensor(out=gtmp[:, :], in0=pg[:, :], in1=bsb[:, :], op=ALU.add), [pg, bsb], [gtmp])
            ph.dve(lambda e, cs=cs: e.tensor_tensor(out=mixT[:, 0:4, cs], in0=gtmp[:, :].rearrange("p (g t) -> p g t", g=4), in1=uT[:, :, cs], op=ALU.mult),
                   [gtmp, uT], [mixT])
            kend = (n + 1) * 128
            KT = (kend + 511) // 512
            for pr in range(4):
                J = (0, 1)
                for j in J:
                    ph.dve(lambda e, j=j: e.memset(cars[j][:, :], 0.0), [], [cars[j]])
                first = [True, True]
                steps = []
                for kt in reversed(range(KT)):
                    k0 = kt * 512
                    w = min(kend, k0 + 512) - k0
                    steps.append((kt, k0, w, w // 128))

                def emit_z(st):
                    kt_, k0_, w_, _ = st
                    for j in J:
                        b0 = j * 64
                        ph.mm(pz[j][:, 0:w_], qT[b0:b0 + 64, pr, cs], kTres[b0:b0 + 64, pr, k0_:k0_ + w_], True, True, [qT, kTres], [pz[j]])

                emit_z(steps[0])
                for si, (kt, k0, w, nblk) in enumerate(steps):
                    for j in J:
                        ph.act(lambda e, j=j, w=w: e.activation(out=ebuf[j][:, 0:w], in_=pz[j][:, 0:w], func=AF.Exp), [pz[j]], [ebuf[j]])
                    if si + 1 < len(steps):
                        emit_z(steps[si + 1])
                    for j in J:
                        ph.act(lambda e, j=j, w=w: e.activation(out=spb[j][:, 1:w + 1], in_=ebuf[j][:, 0:w], func=AF.Ln, bias=1.0), [ebuf[j]], [spb[j]])
                    if kt == KT - 1:
                        for j in J:
                            ph.pool(lambda e, j=j, w=w: e.tensor_tensor(out=spb[j][:, 1 + w - 128:1 + w], in0=spb[j][:, 1 + w - 128:1 + w], in1=m01s, op=ALU.mult),
                                    [spb[j]] + cd, [spb[j]])
                            ph.pool(lambda e, j=j, w=w: e.tensor_tensor(out=ebuf[j][:, w - 128:w], in0=ebuf[j][:, w - 128:w], in1=m01s, op=ALU.mult),
                                    [ebuf[j]] + cd, [ebuf[j]])
                    for j in J:
                        ph.dve(lambda e, j=j, w=w: e.tensor_tensor_scan(out=csb[j][:, 0:w + 1], data0=ones513[:, 0:w + 1], data1=spb[j][:, 0:w + 1], initial=0.0,
                                                                       op0=ALU.mult, op1=ALU.add), [spb[j], ones513], [csb[j]])
                    for j in J:
                        ph.dve(lambda e, j=j, w=w: e.tensor_tensor(out=cars[j][:, :], in0=csb[j][:, w:w + 1], in1=cars[j][:, :], op=ALU.add), [csb[j], cars[j]], [cars[j]])
                        ph.dve(lambda e, j=j: e.tensor_scalar(out=nbb[j][:, :], in0=cars[j][:, :], scalar1=-1.0, scalar2=None, op0=ALU.mult), [cars[j]], [nbb[j]])
                    for j in J:
                        ph.act(lambda e, j=j, w=w: e.activation(out=csb[j][:, 0:w], in_=csb[j][:, 0:w], func=AF.Exp, bias=nbb[j][:, :]), [csb[j], nbb[j]], [csb[j]])
                    for j in J:
                        ph.pool(lambda e, j=j, w=w: e.tensor_tensor(out=attb[j][:, 0:w], in0=ebuf[j][:, 0:w], in1=csb[j][:, 0:w], op=ALU.mult), [ebuf[j], csb[j]], [attb[j]])
                    for j in J:
                        for jb in range(nblk):
                            ph.tr(trp[:, j * 512 + jb * 128:j * 512 + (jb + 1) * 128], attb[j][:, jb * 128:(jb + 1) * 128], idb[:, :], [attb[j], idb], [trp])
                    ph.dve(lambda e, nblk=nblk: e.tensor_copy(out=attT[:, :, 0:nblk, :], in_=trp[:, :].rearrange("p (j b t) -> p j b t", j=2, b=4)[:, :, 0:nblk, :]),
                           [trp], [attT])
                    for j in J:
                        h = 2 * pr + j
                        for jb in range(nblk):
                            kb = kt * 4 + jb
                            last = (kt == 0 and jb == nblk - 1)
                            ph.mm(pos[j][:, h * 64:(h + 1) * 64], attT[:, j, jb, :], Vres[:, kb, h * 64:(h + 1) * 64], first[j], last, [attT, Vres], [pos[j]])
                            first[j] = False
            for j in (0, 1):
                ph.act(lambda e, j=j: e.activation(out=dout[:, :].rearrange("p (r j d) -> p r j d", r=4, j=2)[:, :, j, :],
                                                   in_=pos[j][:, :].rearrange("p (r j d) -> p r j d", r=4, j=2)[:, :, j, :], func=AF.Copy), [pos[j]], [dout])
            for cc in range(4):
                ph.tr(trp[:, cc * 128:(cc + 1) * 128], dout[:, cc * 128:(cc + 1) * 128], idb[:, :], [dout, idb], [trp])
            ph.dve(lambda e, cs=cs: e.tensor_copy(out=mixT[:, 4:8, cs], in_=trp[:, 0:512].rearrange("p (c t) -> p c t", c=4)), [trp], [mixT])
        for sub in range(4):
            n = tt * 4 + sub
            for half in range(2):
                for kc in range(8):
                    ph.mm(pj[half][:, :], mixT[:, kc, sub * 128:(sub + 1) * 128], wout[:, kc, half * 512:(half + 1) * 512], kc == 0, kc == 7, [mixT, wout], [pj[half]])
            epi.run(n, lambda lo, hi: pj[lo // 512][:, :], [pj[0], pj[1]], idb, idf, trp, pz, pg)
    ph.finish()


def mk_deps(ph, cx):
    cx.acc_dep = [ph.region("acc%d" % n) for n in range(cx.NT)]
    cx.out_dep = [ph.region("out%d" % n) for n in range(cx.NT)]
    cx.xT_dep = [ph.region("xT%d" % t) for t in range(cx.NTT)]
    cx.comb_dep = ph.region("comb")


ROW_KEYS_EVEN = ["eln1g", "eln1b", "eln2g", "eln2b", "hg"]
ROW_KEYS_ODD = ["oln1g", "oln1b", "oln2g", "oln2b", "cng", "cnb", "bs", "rb"]


def row_index():
    idx = {}
    k = 0
    for j in range(2):
        for key in ROW_KEYS_EVEN + ROW_KEYS_ODD:
            idx[(key, j)] = k
            k += 1
    return idx, k


def build(S, layers):
    nc = bass.Bass("TRN2", target_bir_lowering=False)
    gst = ExitStack()
    make_sempool(nc, gst)
    nc._gst = gst
    cx = Ctx()
    cx.S = S; cx.NT = S // 128; cx.NTT = S // 512; cx.NCST = NCST
    cx.row_idx, nrows = row_index()

    def inp(name, shape):
        return nc.dram_tensor(name, list(shape), F32, kind="ExternalInput").ap()

    cx.x = inp("x", [S, D])
    cx.out = nc.dram_tensor("out", [S, D], F32, kind="ExternalOutput").ap()
    cx.cst = inp("cst", [128, NCST])
    cx.rows = inp("rows", [nrows, D])
    cx.pe = [inp("pe0", [128, NPE]), inp("pe1", [128, NPE])]
    cx.wst = inp("wst", [2, 4, 128, 128])
    cx.wkeys = []
    if any(l % 2 == 0 for l in layers):
        cx.ab_w_in = inp("ab_w_in", [2, D, 3080]); cx.ab_w_out = inp("ab_w_out", [2, D, D])
        cx.ffn_w1 = inp("ffn_w1", [2, D, DFF]); cx.ffn_w3 = inp("ffn_w3", [2, D, DFF]); cx.ffn_w2 = inp("ffn_w2", [2, DFF, D])
        cx.wkeys += ["ab_w_in", "ab_w_out", "ffn_w1", "ffn_w3", "ffn_w2"]
    if any(l % 2 == 1 for l in layers):
        cx.cd_w_in = inp("cd_w_in", [2, D, 2560]); cx.cd_w_out = inp("cd_w_out", [2, D, D])
        cx.router_w = inp("router_w", [2, D, NE])
        cx.moe_w1 = inp("moe_w1", [2, NE, D, DFF]); cx.moe_w3 = inp("moe_w3", [2, NE, D, DFF]); cx.moe_w2 = inp("moe_w2", [2, NE, DFF, D])
        cx.wkeys += ["cd_w_in", "cd_w_out", "router_w", "moe_w1", "moe_w3", "moe_w2"]
    nc._wkeys = cx.wkeys
    cx.acc = nc.dram_tensor("acc", [S, D], F32).ap()
    cx.xT = nc.dram_tensor("xT", [8, 128, S], BF16).ap()
    cx.comb = nc.dram_tensor("comb", [S, NE], F32).ap()
    ri = cx.row_idx
    import os
    kstop = int(os.environ.get("KSTOP", "99"))
    phase_prologue(nc, cx)
    if kstop <= 0:
        return nc
    for li, l in enumerate(layers):
        j = l // 2
        last = (li == len(layers) - 1)
        if l % 2 == 0:
            phase_even_mixer(nc, cx, "em%d" % l, j)
            if kstop <= 1:
                return nc
            phase_ffn(nc, cx, "ef%d" % l, [cx.ffn_w1[j]], [cx.ffn_w3[j]], [cx.ffn_w2[j]],
                      cx.rows[ri[("eln2g", j)]:ri[("eln2g", j)] + 1, :], cx.rows[ri[("eln2b", j)]:ri[("eln2b", j)] + 1, :], last, False)
        else:
            phase_odd_mixer(nc, cx, "om%d" % l, j)
            if kstop <= 1:
                phase_dump(nc, cx)
                return nc
            phase_ffn(nc, cx, "of%d" % l, [cx.moe_w1[j][e] for e in range(NE)], [cx.moe_w3[j][e] for e in range(NE)],
                      [cx.moe_w2[j][e] for e in range(NE)],
                      cx.rows[ri[("oln2g", j)]:ri[("oln2g", j)] + 1, :], cx.rows[ri[("oln2b", j)]:ri[("oln2b", j)] + 1, :], last, True)
    return nc


def host_consts():
    c = np.zeros((128, NCST), np.float32)
    i = np.arange(128)
    c[:, CST_ID:CST_ID + 128] = np.eye(128, dtype=np.float32)
    c[:, CST_ONES:CST_ONES + 128] = 1.0
    c[:, CST_MTNEG:CST_MTNEG + 128] = np.where(i[:, None] <= i[None, :], 0.0, NEG)
    c[:, CST_M01S:CST_M01S + 128] = (i[None, :] < i[:, None]).astype(np.float32)
    c[:, CST_MT01:CST_MT01 + 128] = (i[:, None] <= i[None, :]).astype(np.float32)
    for h in range(4):
        c[h, CST_SEL + h * 128:CST_SEL + (h + 1) * 128] = 1.0
    t = np.arange(512)
    c[:, CST_SC01:CST_SC01 + 512] = (t % 128 != 0).astype(np.float32)[None, :]
    c[:, CST_SCNEG:CST_SCNEG + 512] = np.where(t % 128 == 0, NEG, 0.0)[None, :]
    return c


def host_layout(inp):
    f = lambda a: np.asarray(a, dtype=np.float32)
    idx, nrows = row_index()
    rows = np.zeros((nrows, D), np.float32)

    def put(key, j, v):
        v = f(v).reshape(-1)
        rows[idx[(key, j)], :v.size] = v

    pes = []
    for j in range(2):
        put("eln1g", j, inp["ab_ln1_g"][j]); put("eln1b", j, inp["ab_ln1_b"][j])
        put("eln2g", j, inp["ab_ln2_g"][j]); put("eln2b", j, inp["ab_ln2_b"][j])
        put("hg", j, inp["b_norm_g"][j])
        put("oln1g", j, inp["cd_ln1_g"][j]); put("oln1b", j, inp["cd_ln1_b"][j])
        put("oln2g", j, inp["cd_ln2_g"][j]); put("oln2b", j, inp["cd_ln2_b"][j])
        put("cng", j, inp["c_norm_g"][j]); put("cnb", j, inp["c_norm_b"][j])
        put("bs", j, inp["c_b_s"][j]); put("rb", j, inp["router_b"][j])
        pe = np.zeros((128, NPE), np.float32)
        cw = f(inp["a_conv_w"][j])
        pe[:, PE_CW:PE_CW + 124] = cw.reshape(31, 4, 128).transpose(2, 1, 0).reshape(128, 124)
        pe[:, PE_CB:PE_CB + 4] = f(inp["a_conv_b"][j]).reshape(4, 128).T
        pe[:, PE_AG:PE_AG + 4] = f(inp["a_norm_g"][j]).reshape(4, 128).T
        pe[:, PE_AB:PE_AB + 4] = f(inp["a_norm_b"][j]).reshape(4, 128).T
        gb = f(inp["ab_gate_bias"][j])
        for g in range(4):
            pe[32 * g:32 * g + 4, PE_IB] = gb[0:4]
            pe[32 * g:32 * g + 4, PE_FB] = gb[4:8]
        pes.append(pe)
    wst = np.ascontiguousarray(f(inp["c_w_s"]).transpose(0, 1, 3, 2))
    return rows, pes, wst


WKEYS = ["ab_w_in", "ab_w_out", "ffn_w1", "ffn_w3", "ffn_w2", "cd_w_in", "cd_w_out", "router_w", "moe_w1", "moe_w3", "moe_w2"]
_CACHE = {}


def run(inputs, S, layers, ncores):
    key = (S, tuple(layers))
    if key not in _CACHE:
        _CACHE[key] = build(S, layers)
    nc = _CACHE[key]
    rows, pes, wst = host_layout(inputs)
    cst = host_consts()
    shared = {"cst": cst, "rows": rows, "pe0": pes[0], "pe1": pes[1], "wst": wst}
    for k in nc._wkeys:
        shared[k] = np.ascontiguousarray(np.asarray(inputs[k], dtype=np.float32))
    x = np.asarray(inputs["x"], dtype=np.float32)
    in_maps = []
    for c in range(ncores):
        m = dict(shared)
        m["x"] = np.ascontiguousarray(x[c, :S, :])
        in_maps.append(m)
    import os
    if os.environ.get("KTRACE"):
        res = run_bass_kernel_spmd(nc, in_maps, core_ids=list(range(ncores)), trace=True)
        print("EXEC_NS", res.exec_time_ns)
    else:
        res = run_bass_kernel_spmd(nc, in_maps, core_ids=list(range(ncores)))
    return np.stack([res.results[c]["out"] for c in range(ncores)], axis=0)


def kernel(**inputs):
    return run(inputs, 4096, [0, 1, 2, 3], 8)


def phase_dump(nc, cx):
    ph = Phase(nc, "dump")
    mk_deps(ph, cx)
    t = ph.sb("t", [128, D])
    for n in range(cx.NT):
        ph.dma("sp", t[:, :], cx.acc[n * 128:(n + 1) * 128, :], [], [t], t)
        ph.dma("sp", cx.out[n * 128:(n + 1) * 128, :], t[:, :], [t], [cx.out_dep[n]], t)
    ph.finish()
```

```python
import math
from contextlib import ExitStack
import numpy as np
import concourse.bass as bass
import concourse.mybir as mybir
from concourse.bass_utils import run_bass_kernel_spmd

F32 = mybir.dt.float32
BF16 = mybir.dt.bfloat16
AF = mybir.ActivationFunctionType
ALU = mybir.AluOpType
AX = mybir.AxisListType

D = 1024
DFF = 2816
NFF = 22
NE = 8
DEPTH = 4
ALPHA = (2 * DEPTH) ** 0.25
EPS = 1e-5
NEG = -1.0e30
FGROUPS = [(0, 4), (4, 4), (8, 4), (12, 4), (16, 4), (20, 2)]

ENGS = ["pe", "act", "dve", "pool", "sp"]


class Dep:
    __slots__ = ("name", "lw", "rd", "sem", "dcount", "lastdma", "psum")

    def __init__(self, name):
        self.name = name
        self.psum = False
        self.lw = None
        self.rd = []
        self.sem = None
        self.dcount = 0
        self.lastdma = None


class Op:
    __slots__ = ("id", "eng", "fn", "deps", "dma", "signal", "sigval", "sem", "inc")

    def __init__(self, id, eng, fn, dma, inc):
        self.id = id
        self.eng = eng
        self.fn = fn
        self.deps = set()
        self.dma = dma
        self.signal = False
        self.sigval = 0
        self.sem = None
        self.inc = inc


class Buf:
    __slots__ = ("t", "d")

    def __init__(self, t, d):
        self.t = t
        self.d = d

    def __getitem__(self, k):
        return self.t[k]


SEMPOOL = {}
NDMASEM = 40
NSWSEM = 24


def make_sempool(nc, stack):
    pool = {"eng": {}, "dma": []}
    for e in ["pe", "act", "dve", "pool"]:
        pool["eng"][e] = [stack.enter_context(nc.semaphore("s_" + e)), 0]
    for i in range(NDMASEM):
        pool["dma"].append([stack.enter_context(nc.semaphore("d%d" % i)), 0])
    pool["sw"] = []
    for i in range(NSWSEM):
        pool["sw"].append([stack.enter_context(nc.semaphore("w%d" % i)), 0])
    SEMPOOL[id(nc)] = pool


class Phase:
    def __init__(self, nc, name):
        self.nc = nc
        self.name = name
        self.ops = []
        self.by_eng = {e: [] for e in ENGS}
        self.dma_deps = []
        self.all_deps = []
        self.st = ExitStack()
        self.nsb = 0

    def dep(self, name):
        d = Dep(name)
        self.all_deps.append(d)
        return d

    def sb(self, name, shape, dt=F32):
        t = self.st.enter_context(self.nc.sbuf_tensor(self.name + "_" + name, list(shape), dt))
        return Buf(t, self.dep(name))

    def ps(self, name, shape, dt=F32):
        t = self.st.enter_context(self.nc.psum_tensor(self.name + "_" + name, list(shape), dt))
        b = Buf(t, self.dep(name))
        b.d.psum = True
        return b

    def region(self, name):
        return Buf(None, self.dep(name))

    def op(self, eng, fn, R=(), W=(), dma=None, inc=16):
        o = Op(len(self.ops), eng, fn, dma, inc)
        self.ops.append(o)
        self.by_eng[eng].append(o)
        deps = o.deps
        if any(b.d.psum for b in R):
            W = list(W) + [b for b in R if b.d.psum]
            R = [b for b in R if not b.d.psum]
        for b in R:
            t = b.d
            if t.lw is not None:
                deps.add(t.lw)
        for b in W:
            t = b.d
            if t.lw is not None:
                deps.add(t.lw)
            deps.update(t.rd)
        if dma is not None:
            dd = dma.d
            if dd.lastdma is not None:
                deps.add(dd.lastdma)
            if fn is not None:
                dd.lastdma = o.id
            if dd.sem is None:
                dd.sem = True
                self.dma_deps.append(dd)
        deps.discard(o.id)
        best = {}
        for d in deps:
            od = self.ops[d]
            if od.dma is None and od.fn is not None:
                if d > best.get(od.eng, -1):
                    best[od.eng] = d
        for d in list(deps):
            od = self.ops[d]
            if od.dma is None and od.fn is not None and best[od.eng] != d:
                deps.discard(d)
        if eng == "pe":
            for d in list(deps):
                od = self.ops[d]
                if od.eng == "pe" and od.dma is None:
                    deps.discard(d)
        for d in deps:
            self.ops[d].signal = True
        if fn is not None:
            for b in R:
                b.d.rd.append(o.id)
            for b in W:
                b.d.lw = o.id
                b.d.rd = []
        return o

    def pe(self, fn, R=(), W=()):
        return self.op("pe", fn, R, W)

    def act(self, fn, R=(), W=()):
        return self.op("act", fn, R, W)

    def dve(self, fn, R=(), W=()):
        return self.op("dve", fn, R, W)

    def pool(self, fn, R=(), W=()):
        return self.op("pool", fn, R, W)

    def dma(self, q, out_ap, in_ap, R, W, semb):
        return self.op(q, lambda e: e.dma_start(out=out_ap, in_=in_ap), R, W, dma=semb)

    def mm(self, out_ap, lhsT, rhs, start, stop, R, W):
        return self.op("pe", lambda e: e.matmul(out_ap, lhsT=lhsT, rhs=rhs, start=start, stop=stop), R, W)

    def tr(self, out_ap, in_ap, ident_ap, R, W):
        return self.op("pe", lambda e: e.transpose(out=out_ap, in_=in_ap, identity=ident_ap), R, W)

    def barrier(self):
        allb = [Buf(None, d) for d in self.all_deps]
        for e in ENGS:
            self.op(e, None, R=(), W=allb)

    def finish(self):
        nc = self.nc
        st = self.st
        allb = [Buf(None, d) for d in self.all_deps]
        for e in ENGS:
            self.op(e, None, R=(), W=allb)
        pool = SEMPOOL[id(nc)]
        engsem = {}
        cnt = {e: 0 for e in ENGS}
        for e in ["pe", "act", "dve", "pool"]:
            engsem[e] = pool["eng"][e][0]
            cnt[e] = pool["eng"][e][1]
        swq = set()
        for o in self.ops:
            if o.dma is not None and o.eng == "pool":
                swq.add(id(o.dma.d))
        slots = []
        ih = isw = 0
        for d in self.dma_deps:
            if id(d) in swq:
                slots.append(pool["sw"][isw]); isw += 1
            else:
                slots.append(pool["dma"][ih]); ih += 1
        assert ih <= NDMASEM and isw <= NSWSEM, (ih, isw)
        for d, sl in zip(self.dma_deps, slots):
            d.sem = sl[0]
            d.dcount = sl[1]
        for o in self.ops:
            if o.fn is None:
                continue
            if o.dma is not None:
                o.dma.d.dcount += o.inc
                o.sigval = o.dma.d.dcount
                o.sem = o.dma.d.sem
            elif o.signal:
                cnt[o.eng] += 1
                o.sigval = cnt[o.eng]
                o.sem = engsem[o.eng]
        ops = self.ops

        def run(eng_name):
            def body(e):
                seen = {}
                for o in self.by_eng[eng_name]:
                    need = {}
                    for d in o.deps:
                        od = ops[d]
                        if od.fn is None:
                            continue
                        key = id(od.sem)
                        if od.sigval > need.get(key, (0, None))[0]:
                            need[key] = (od.sigval, od.sem)
                    for key, (val, sem) in need.items():
                        if seen.get(key, 0) >= val:
                            continue
                        e.wait_ge(sem, val)
                        seen[key] = val
                    if o.fn is None:
                        continue
                    ins = o.fn(e)
                    if o.dma is not None:
                        ins.then_inc(o.sem, o.inc)
                    elif o.signal:
                        ins.then_inc(o.sem, 1)
            return body

        for e in ["pe", "act", "dve", "pool"]:
            pool["eng"][e][1] = cnt[e]
        for d, sl in zip(self.dma_deps, slots):
            sl[1] = d.dcount
        block = st.enter_context(nc.Block())
        block.tensor(run("pe"))
        block.scalar(run("act"))
        block.vector(run("dve"))
        block.gpsimd(run("pool"))
        block.sync(run("sp"))
        st.close()


class Ctx:
    pass


def load_rows(ph, q, buf, dram_row_ap, n):
    ph.dma(q, buf[:, 0:n], dram_row_ap.partition_broadcast(128), [], [buf], buf)


def ln_rows(ph, s, mv, stats, rstd, nb, width, eps=EPS):
    nchunk = (width + 511) // 512
    for h in range(nchunk):
        lo, hi = h * 512, min(width, (h + 1) * 512)
        ph.dve(lambda e, h=h, lo=lo, hi=hi: e.bn_stats(out=stats[:, h, :], in_=s[:, lo:hi]), [s], [stats])
    ph.dve(lambda e: e.bn_aggr(out=mv[:, :], in_=stats[:, 0:nchunk, :].rearrange("p a b -> p (a b)")), [stats], [mv])
    ph.dve(lambda e: e.tensor_scalar(out=rstd[:, :], in0=mv[:, 1:2], scalar1=eps, scalar2=None, op0=ALU.add), [mv], [rstd])
    ph.act(lambda e: e.activation(out=rstd[:, :], in_=rstd[:, :], func=AF.Sqrt), [rstd], [rstd])
    ph.dve(lambda e: e.reciprocal(out=rstd[:, :], in_=rstd[:, :]), [rstd], [rstd])
    ph.dve(lambda e: e.scalar_tensor_tensor(out=nb[:, :], in0=mv[:, 0:1], scalar=-1.0, in1=rstd[:, :], op0=ALU.mult, op1=ALU.mult),
           [mv, rstd], [nb])


class Epi:
    def __init__(self, ph, cx, g_row, b_row, last, router=None, nbuf=2):
        self.ph = ph
        self.cx = cx
        self.last = last
        self.router = router
        self.grow = ph.sb("e_g", [128, D]); load_rows(ph, "sp", self.grow, g_row, D)
        self.brow = ph.sb("e_b", [128, D]); load_rows(ph, "sp", self.brow, b_row, D)
        self.nbuf = nbuf
        self.s = [ph.sb("e_s%d" % i, [128, D]) for i in range(nbuf)]
        self.y = [ph.sb("e_y%d" % i, [128, D]) for i in range(nbuf)]
        self.yb = [ph.sb("e_yb%d" % i, [128, D], BF16) for i in range(nbuf)]
        self.ya = self.s
        self.stats = ph.sb("e_st", [128, 2, 6]); self.mv = ph.sb("e_mv", [128, 2])
        self.rstd = ph.sb("e_rstd", [128, 1]); self.nb = ph.sb("e_nb", [128, 1])
        self.xTs = [ph.sb("e_xT%d" % i, [128, 8, 512], BF16) for i in range(nbuf)]
        self.k = 0
        if router is not None:
            self.rw = ph.sb("r_w", [128, 8, NE]); ph.dma("sp", self.rw[:, :, :], router["w"].rearrange("(c p) e -> p c e", p=128), [], [self.rw], self.rw)
            self.rb = ph.sb("r_b", [128, NE]); load_rows(ph, "sp", self.rb, router["b"], NE)
            self.yT = ph.sb("r_yT", [128, 8, 128])
            self.lg = ph.sb("r_lg", [128, NE]); self.top = ph.sb("r_top", [128, 8])
            self.g1 = ph.sb("r_g1", [128, 1]); self.g2 = ph.sb("r_g2", [128, 1]); self.dd = ph.sb("r_dd", [128, 1])
            self.c1 = ph.sb("r_c1", [128, NE]); self.c2 = ph.sb("r_c2", [128, NE])

    def run(self, n, res_ap_fn, res_R, idb, idf, trp, trf, rlp):
        ph, cx = self.ph, self.cx
        k = self.k; self.k += 1
        nbuf = self.nbuf
        s = self.s[k % nbuf]; y = self.y[k % nbuf]; yb = self.yb[k % nbuf]; ya = self.ya[k % nbuf]
        xTs = self.xTs[(n // 4) % nbuf]
        accr = cx.acc_dep[n]
        ph.dma("sp", s[:, :], cx.acc[n * 128:(n + 1) * 128, :], [accr], [s], s)
        if res_ap_fn is not None:
            for h in range(2):
                ph.dve(lambda e, h=h: e.tensor_tensor(out=s[:, h * 512:(h + 1) * 512], in0=res_ap_fn(h * 512, (h + 1) * 512),
                                                      in1=s[:, h * 512:(h + 1) * 512], op=ALU.add), [s] + list(res_R), [s])
        ln_rows(ph, s, self.mv, self.stats, self.rstd, self.nb, D)
        ph.act(lambda e: e.activation(out=y[:, :], in_=s[:, :], func=AF.Identity, bias=self.nb[:, :], scale=self.rstd[:, :]),
               [s, self.nb, self.rstd], [y])
        ph.pool(lambda e: e.tensor_tensor(out=y[:, :], in0=y[:, :], in1=self.grow[:, :], op=ALU.mult), [y, self.grow], [y])
        ph.pool(lambda e: e.tensor_tensor(out=y[:, :], in0=y[:, :], in1=self.brow[:, :], op=ALU.add), [y, self.brow], [y])
        if self.last:
            ph.dma("sp", cx.out[n * 128:(n + 1) * 128, :], y[:, :], [y], [cx.out_dep[n]], y)
            return
        ph.act(lambda e: e.activation(out=ya[:, :], in_=y[:, :], func=AF.Copy, scale=ALPHA), [y], [ya])
        ph.dma("sp", cx.acc[n * 128:(n + 1) * 128, :], ya[:, :], [ya], [accr], ya)
        ph.act(lambda e: e.activation(out=yb[:, :], in_=y[:, :], func=AF.Copy), [y], [yb])
        for c in range(8):
            ph.tr(trp[:, c * 128:(c + 1) * 128], yb[:, c * 128:(c + 1) * 128], idb[:, :], [yb, idb], [trp])
        sub = n % 4
        ph.dve(lambda e: e.tensor_copy(out=xTs[:, :, sub * 128:(sub + 1) * 128], in_=trp[:, :].rearrange("p (c t) -> p c t", c=8)),
               [trp], [xTs])
        if sub == 3:
            t0 = (n // 4) * 512
            ph.dma("sp", cx.xT.rearrange("c p s -> p c s")[:, :, t0:t0 + 512], xTs[:, :, :], [xTs], [cx.xT_dep[n // 4]], xTs)
        if self.router is not None:
            for half in range(2):
                tf = trf[half]
                for c in range(4):
                    cc = half * 4 + c
                    ph.tr(tf[:, c * 128:(c + 1) * 128], y[:, cc * 128:(cc + 1) * 128], idf[:, :], [y, idf], [tf])
                ph.act(lambda e, half=half, tf=tf: e.activation(out=self.yT[:, half * 4:(half + 1) * 4, :],
                                                                in_=tf[:, :].rearrange("p (c t) -> p c t", c=4), func=AF.Copy),
                       [tf], [self.yT])
            for c in range(8):
                ph.mm(rlp[:, 0:NE], self.yT[:, c, :], self.rw[:, c, :], c == 0, c == 7, [self.yT, self.rw], [rlp])
            ph.dve(lambda e: e.tensor_tensor(out=self.lg[:, :], in0=rlp[:, 0:NE], in1=self.rb[:, :], op=ALU.add), [rlp, self.rb], [self.lg])
            ph.dve(lambda e: e.max(out=self.top[:, :], in_=self.lg[:, :]), [self.lg], [self.top])
            ph.dve(lambda e: e.tensor_tensor(out=self.dd[:, :], in0=self.top[:, 0:1], in1=self.top[:, 1:2], op=ALU.subtract), [self.top], [self.dd])
            ph.act(lambda e: e.activation(out=self.g1[:, :], in_=self.dd[:, :], func=AF.Sigmoid), [self.dd], [self.g1])
            ph.dve(lambda e: e.tensor_scalar(out=self.g2[:, :], in0=self.g1[:, :], scalar1=-1.0, scalar2=1.0, op0=ALU.mult, op1=ALU.add),
                   [self.g1], [self.g2])
            ph.dve(lambda e: e.tensor_scalar(out=self.c1[:, :], in0=self.lg[:, :], scalar1=self.top[:, 0:1], scalar2=self.g1[:, :],
                                             op0=ALU.is_equal, op1=ALU.mult), [self.lg, self.top, self.g1], [self.c1])
            ph.dve(lambda e: e.tensor_scalar(out=self.c2[:, :], in0=self.lg[:, :], scalar1=self.top[:, 1:2], scalar2=self.g2[:, :],
                                             op0=ALU.is_equal, op1=ALU.mult), [self.lg, self.top, self.g2], [self.c2])
            ph.dve(lambda e: e.tensor_tensor(out=self.c1[:, :], in0=self.c1[:, :], in1=self.c2[:, :], op=ALU.add), [self.c1, self.c2], [self.c1])
            ph.dma("sp", cx.comb[n * 128:(n + 1) * 128, :], self.c1[:, :], [self.c1], [cx.comb_dep], self.c1)


def load_consts(ph, cx, ncols=None):
    ncols = ncols or cx.NCST
    c = ph.sb("cst", [128, ncols])
    ph.dma("sp", c[:, :], cx.cst[:, 0:ncols], [], [c], c)
    idb = ph.sb("idb", [128, 128], BF16)
    ph.dve(lambda e: e.tensor_copy(out=idb[:, :], in_=c[:, 0:128]), [c], [idb])
    return c, idb


def phase_prologue(nc, cx):
    ph = Phase(nc, "p0")
    mk_deps(ph, cx)
    c, idb = load_consts(ph, cx, 128)
    trp = ph.ps("trp", [128, 1024], BF16)
    xs = [ph.sb("x%d" % i, [128, D]) for i in range(2)]
    xa = [ph.sb("xa%d" % i, [128, D]) for i in range(2)]
    xb = [ph.sb("xb%d" % i, [128, D], BF16) for i in range(2)]
    xTs = [ph.sb("xT%d" % i, [128, 8, 512], BF16) for i in range(2)]
    for n in range(cx.NT):
        x = xs[n % 2]; a = xa[n % 2]; b = xb[n % 2]; xt = xTs[(n // 4) % 2]
        ph.dma("sp", x[:, :], cx.x[n * 128:(n + 1) * 128, :], [], [x], x)
        ph.act(lambda e, x=x, a=a: e.activation(out=a[:, :], in_=x[:, :], func=AF.Copy, scale=ALPHA), [x], [a])
        ph.dma("sp", cx.acc[n * 128:(n + 1) * 128, :], a[:, :], [a], [cx.acc_dep[n]], a)
        ph.dve(lambda e, x=x, b=b: e.tensor_copy(out=b[:, :], in_=x[:, :]), [x], [b])
        for cc in range(8):
            ph.tr(trp[:, cc * 128:(cc + 1) * 128], b[:, cc * 128:(cc + 1) * 128], idb[:, :], [b, idb], [trp])
        sub = n % 4
        ph.dve(lambda e, xt=xt, sub=sub: e.tensor_copy(out=xt[:, :, sub * 128:(sub + 1) * 128],
                                                        in_=trp[:, :].rearrange("p (c t) -> p c t", c=8)), [trp], [xt])
        if sub == 3:
            t0 = (n // 4) * 512
            ph.dma("sp", cx.xT.rearrange("c p s -> p c s")[:, :, t0:t0 + 512], xt[:, :, :], [xt], [cx.xT_dep[n // 4]], xt)
    ph.finish()


def phase_ffn(nc, cx, name, w1s, w3s, w2s, ln_g, ln_b, last, moe):
    ph = Phase(nc, name)
    mk_deps(ph, cx)
    c, idb = load_consts(ph, cx, 128)
    idf = Buf(c.t[:, 0:128], c.d)
    NEXP = len(w1s)
    w1g = [ph.sb("w1g%d" % g, [128, 8, n * 128], BF16) for g, (s0, n) in enumerate(FGROUPS)]
    w3g = [ph.sb("w3g%d" % g, [128, 8, n * 128], BF16) for g, (s0, n) in enumerate(FGROUPS)]
    w2g = [ph.sb("w2g%d" % g, [128, n, D], BF16) for g, (s0, n) in enumerate(FGROUPS)]

    import os
    noroll = bool(os.environ.get("NOROLL"))

    def load_w(e):
        xr = [acct[0]] if (noroll and e > 0) else []
        for g, (s0, n) in enumerate(FGROUPS):
            ph.dma("pool", w1g[g][:, :, :], w1s[e][:, s0 * 128:(s0 + n) * 128].rearrange("(c p) n -> p c n", p=128), xr, [w1g[g]], w1g[g])
            ph.dma("pool", w3g[g][:, :, :], w3s[e][:, s0 * 128:(s0 + n) * 128].rearrange("(c p) n -> p c n", p=128), xr, [w3g[g]], w3g[g])
        for g, (s0, n) in enumerate(FGROUPS):
            ph.dma("pool", w2g[g][:, :, :], w2s[e][s0 * 128:(s0 + n) * 128, :].rearrange("(c p) n -> p c n", p=128), xr, [w2g[g]], w2g[g])

    epi = Epi(ph, cx, ln_g, ln_b, last, nbuf=1)
    xTt = [ph.sb("xTt%d" % i, [128, 8, 512], BF16) for i in range(2)]
    gT = ph.sb("gT", [128, NFF, 512], BF16)
    gdeps = [Buf(gT.t, ph.dep("gT%d" % f)) for f in range(NFF)]
    sg = [ph.sb("sg%d" % i, [128, 512]) for i in range(2)]
    acct = [ph.sb("acct%d" % i, [128, D]) for i in range(1)]
    comb = None
    if moe:
        comb = ph.sb("comb", [128, cx.NT, NE])
        ph.dma("sp", comb[:, :, :], cx.comb.rearrange("(n p) e -> p n e", p=128), [cx.comb_dep], [comb], comb)
    hp = [ph.ps("hp%d" % i, [128, 512]) for i in range(4)]
    op = [ph.ps("op%d" % i, [128, 512]) for i in range(2)]
    trp = ph.ps("trp", [128, 1024], BF16)
    k = 0
    for e in range(NEXP):
        load_w(e)
        for tt in range(cx.NTT):
            xt = xTt[k % 2]
            ph.dma("sp", xt[:, :, :], cx.xT.rearrange("c p s -> p c s")[:, :, tt * 512:(tt + 1) * 512], [cx.xT_dep[tt]], [xt], xt)
            for f in range(NFF):
                g = min(f // 4, 5); fo = (f - FGROUPS[g][0]) * 128
                h1 = hp[(2 * f) % 4]; h3 = hp[(2 * f + 1) % 4]
                for kc in range(8):
                    ph.mm(h1[:, :], w1g[g][:, kc, fo:fo + 128], xt[:, kc, :], kc == 0, kc == 7, [w1g[g], xt], [h1])
                for kc in range(8):
                    ph.mm(h3[:, :], w3g[g][:, kc, fo:fo + 128], xt[:, kc, :], kc == 0, kc == 7, [w3g[g], xt], [h3])
                s = sg[f % 2]
                ph.act(lambda e_, h1=h1, s=s: e_.activation(out=s[:, :], in_=h1[:, :], func=AF.Silu), [h1], [s])
                ph.dve(lambda e_, h3=h3, s=s, f=f: e_.tensor_tensor(out=gT[:, f, :], in0=h3[:, :], in1=s[:, :], op=ALU.mult), [h3, s], [gdeps[f]])
            for sub in range(4):
                n = tt * 4 + sub
                for half in range(2):
                    o = op[half]
                    for f in range(NFF):
                        g = min(f // 4, 5); fi = f - FGROUPS[g][0]
                        ph.mm(o[:, :], gT[:, f, sub * 128:(sub + 1) * 128], w2g[g][:, fi, half * 512:(half + 1) * 512], f == 0, f == NFF - 1,
                              [gdeps[f], w2g[g]], [o])
                if e == NEXP - 1 and not moe:
                    epi.run(n, lambda lo, hi: op[lo // 512][:, :], [op[0], op[1]], idb, idf, trp, None, None)
                else:
                    a = acct[0]
                    ph.dma("sp", a[:, :], cx.acc[n * 128:(n + 1) * 128, :], [cx.acc_dep[n]], [a], a)
                    for half in range(2):
                        if moe:
                            ph.dve(lambda e_, a=a, half=half, n=n, e=e: e_.scalar_tensor_tensor(
                                out=a[:, half * 512:(half + 1) * 512], in0=op[half][:, :], scalar=comb[:, n, e:e + 1],
                                in1=a[:, half * 512:(half + 1) * 512], op0=ALU.mult, op1=ALU.add), [op[half], a, comb], [a])
                        else:
                            ph.dve(lambda e_, a=a, half=half: e_.tensor_tensor(out=a[:, half * 512:(half + 1) * 512], in0=op[half][:, :],
                                                                            in1=a[:, half * 512:(half + 1) * 512], op=ALU.add), [op[half], a], [a])
                    if e == NEXP - 1:
                        ph.dma("sp", cx.acc[n * 128:(n + 1) * 128, :], a[:, :], [a], [cx.acc_dep[n]], a)
                        epi.run(n, None, [], idb, idf, trp, None, None)
                    else:
                        ph.dma("sp", cx.acc[n * 128:(n + 1) * 128, :], a[:, :], [a], [cx.acc_dep[n]], a)
            k += 1
    ph.finish()


CST_ID, CST_ONES, CST_MTNEG, CST_M01S, CST_MT01, CST_SEL, CST_SC01, CST_SCNEG, NCST = 0, 128, 256, 384, 512, 640, 1152, 1664, 2176
PE_CW, PE_CB, PE_AG, PE_AB, PE_IB, PE_FB, NPE = 0, 124, 128, 132, 136, 137, 138


def phase_even_mixer(nc, cx, name, j):
    ph = Phase(nc, name)
    mk_deps(ph, cx)
    c, idb = load_consts(ph, cx)
    cd = [c]
    idf = Buf(c.t[:, 0:128], c.d)
    ones = c.t[:, CST_ONES:CST_ONES + 128]
    win = ph.sb("win", [128, 8, 3080], BF16)
    ph.dma("pool", win[:, :, :], cx.ab_w_in[j].rearrange("(c p) n -> p c n", p=128), [], [win], win)
    wout = ph.sb("wout", [128, 8, D], BF16)
    ph.dma("pool", wout[:, :, :], cx.ab_w_out[j].rearrange("(c p) n -> p c n", p=128), [], [wout], wout)
    pp = ph.sb("pp", [128, NPE])
    ph.dma("sp", pp[:, :], cx.pe[j], [], [pp], pp)
    nfb = ph.sb("nfb", [128, 1])
    ph.dve(lambda e: e.tensor_scalar(out=nfb[:, :], in0=pp[:, PE_FB:PE_FB + 1], scalar1=-1.0, scalar2=None, op0=ALU.mult), [pp], [nfb])
    hg = ph.sb("hg", [128, 512]); load_rows(ph, "sp", hg, cx.rows[cx.row_idx[("hg", j)]:cx.row_idx[("hg", j)] + 1, 0:512], 512)
    epi = Epi(ph, cx, cx.rows[cx.row_idx[("eln1g", j)]:cx.row_idx[("eln1g", j)] + 1, :], cx.rows[cx.row_idx[("eln1b", j)]:cx.row_idx[("eln1b", j)] + 1, :], False, nbuf=1)
    pj = [ph.ps("pj%d" % i, [128, 512]) for i in range(2)]
    pm = ph.ps("pm", [128, 512]); pm_st = pm; pm_ub = pm; pm_cols = pm; pm_sc = pm
    pn = ph.ps("pn", [128, 512]); pn_num = pn; pn_int = pn; pn_kv = pn
    pgi = ph.ps("pgi", [128, 512]); pgf = ph.ps("pgf", [128, 512])
    trp = ph.ps("trp", [128, 1024], BF16)
    pn2 = ph.ps("pn2", [128, 512])
    pnb = [pn, pn2, pgi, pgf]
    xTt = [ph.sb("xTt%d" % i, [128, 8, 512], BF16) for i in range(1)]
    glu = ph.sb("glu", [128, 4, 542])
    ph.dve(lambda e: e.memset(glu[:, :, 0:30], 0.0), [], [glu])
    sig = ph.sb("sig", [128, 512])
    cy = ph.sb("cy", [128, 4, 512]); sq = sig
    cyd = [Buf(cy.t, ph.dep("cy%d" % ch)) for ch in range(4)]
    mean = ph.sb("mean", [128, 512]); var = ph.sb("var", [128, 512]); tmpa = sig
    mixT = ph.sb("mixT", [128, 8, 512], BF16)
    qT = ph.sb("qT", [128, 4, 512], BF16); kT = ph.sb("kT", [128, 4, 512], BF16)
    ktok = ph.sb("ktok", [128, 4, 512], BF16)
    vext = ph.sb("vext", [128, 4, 4, 132], BF16)
    ph.dve(lambda e: e.memset(vext[:, :, :, 128:129], 1.0), [], [vext])
    osig = ph.sb("osig", [128, 4, 512])
    ig = ph.sb("ig", [128, 512]); sp_ = ph.sb("sp", [128, 512]); Bc = ph.sb("Bc", [128, 512]); lw = sp_
    Mx = ph.sb("Mx", [128, 512]); U = ph.sb("U", [128, 512]); rowsT = ph.sb("rowsT", [128, 512]); tmpr = ig
    ph.dve(lambda e: e.memset(rowsT[:, :], 0.0), [], [rowsT])
    ph.dve(lambda e: e.memset(U[:, :], 0.0), [], [U])
    am = ph.sb("am", [128, 4]); nam = ph.sb("nam", [128, 4]); mp = ph.sb("mp", [128, 5]); sv = ph.sb("sv", [128, 4, 2]); t1 = ph.sb("t1", [128, 1]); t2 = ph.sb("t2", [128, 1])
    ph.dve(lambda e: e.memset(sv[:, :, :], 0.0), [], [sv])
    mcar = ph.sb("mcar", [128, 1])
    ph.dve(lambda e: e.memset(mcar[:, :], 0.0), [], [mcar])
    cols = ph.sb("cols", [128, 4, 128]); scol = ph.sb("scol", [128, 4, 128])
    vt = ph.sb("vt", [128, 512]); rows2 = ph.sb("rows2", [128, 512]); ax = ph.sb("ax", [128, 4]); nax = ph.sb("nax", [128, 4])
    ph.dve(lambda e: e.memset(rows2[:, :], 0.0), [], [rows2])
    wgi = ph.sb("wgi", [128, 8, 128], BF16); wgf = ph.sb("wgf", [128, 8, 128], BF16)
    ph.dve(lambda e: e.memset(wgi[:, :, :], 0.0), [], [wgi])
    ph.dve(lambda e: e.memset(wgf[:, :, :], 0.0), [], [wgf])
    for g in range(4):
        ph.dve(lambda e, g=g: e.tensor_copy(out=wgi[:, :, 32 * g:32 * g + 4], in_=win[:, :, 3072:3076]), [win], [wgi])
        ph.dve(lambda e, g=g: e.tensor_copy(out=wgf[:, :, 32 * g:32 * g + 4], in_=win[:, :, 3076:3080]), [win], [wgf])
    Cf = ph.sb("Cf", [128, 4, 132]); Cb = ph.sb("Cb", [128, 4, 132], BF16)
    ph.dve(lambda e: e.memset(Cf[:, :, :], 0.0), [], [Cf])
    ph.dve(lambda e: e.memset(Cb[:, :, :], 0.0), [], [Cb])
    PTs = [ph.sb("PT%d" % h, [128, 128], BF16) for h in range(4)]
    tis = [ph.sb("ti%d" % h, [128, 129]) for h in range(4)]; tots = [ph.sb("tot%d" % h, [128, 129]) for h in range(4)]
    dd4 = ph.sb("dd4", [128, 4]); rec4 = ph.sb("rec4", [128, 4])
    kws = [ph.sb("kw%d" % h, [128, 128], BF16) for h in range(4)]
    vscs = [ph.sb("vsc%d" % h, [128, 132], BF16) for h in range(4)]
    Cfd = [Buf(None, ph.dep("Cf%d" % h)) for h in range(4)]
    ph.dve(lambda e: e.memset(Cf[:, :, :], 0.0), [Cf], Cfd)
    hraw = ph.sb("hraw", [128, 512]); hst = ph.sb("hst", [128, 4, 6]); hmv = ph.sb("hmv", [128, 4, 2]); hrs = ph.sb("hrs", [128, 4]); hb = ph.sb("hb", [128, 512], BF16)
    sc01 = c.t[:, CST_SC01:CST_SC01 + 512]; scneg = c.t[:, CST_SCNEG:CST_SCNEG + 512]
    k128 = 128 ** -0.5

    def proj_fm(col0, dst_fn, pjk):
        p = pj[pjk % 2]
        for kc in range(8):
            ph.mm(p[:, :], win[:, kc, col0:col0 + 128], xt[:, kc, :], kc == 0, kc == 7, [win, xt], [p])
        dst_fn(p)

    kpj = [0]
    import os
    emstop = int(os.environ.get("EMSTOP", "99"))
    for tt in range(cx.NTT if emstop > 0 else 0):
        xt = xTt[0]
        ph.dma("sp", xt[:, :, :], cx.xT.rearrange("c p s -> p c s")[:, :, tt * 512:(tt + 1) * 512], [cx.xT_dep[tt]], [xt], xt)
        for ch in range(4):
            p = pj[kpj[0] % 2]; kpj[0] += 1
            for kc in range(8):
                ph.mm(p[:, :], win[:, kc, 512 + ch * 128:512 + (ch + 1) * 128], xt[:, kc, :], kc == 0, kc == 7, [win, xt], [p])
            ph.act(lambda e, p=p: e.activation(out=sig[:, :], in_=p[:, :], func=AF.Sigmoid), [p], [sig])
            p2 = pj[kpj[0] % 2]; kpj[0] += 1
            for kc in range(8):
                ph.mm(p2[:, :], win[:, kc, ch * 128:(ch + 1) * 128], xt[:, kc, :], kc == 0, kc == 7, [win, xt], [p2])
            ph.dve(lambda e, p2=p2, ch=ch: e.tensor_tensor(out=glu[:, ch, 30:542], in0=p2[:, :], in1=sig[:, :], op=ALU.mult), [p2, sig], [glu])
        for ch in range(4):
            ph.dve(lambda e, ch=ch: e.tensor_scalar(out=cy[:, ch, :], in0=glu[:, ch, 0:512], scalar1=pp[:, PE_CW + ch * 31:PE_CW + ch * 31 + 1],
                                                    scalar2=pp[:, PE_CB + ch:PE_CB + ch + 1], op0=ALU.mult, op1=ALU.add), [glu, pp], [cyd[ch]])
        for jj in range(1, 31):
            for ch in range(4):
                ph.dve(lambda e, ch=ch, jj=jj: e.scalar_tensor_tensor(out=cy[:, ch, :], in0=glu[:, ch, jj:jj + 512],
                                                                     scalar=pp[:, PE_CW + ch * 31 + jj:PE_CW + ch * 31 + jj + 1],
                                                                     in1=cy[:, ch, :], op0=ALU.mult, op1=ALU.add), [glu, pp, cyd[ch]], [cyd[ch]])
        for ch in range(4):
            ph.pool(lambda e, ch=ch: e.tensor_copy(out=glu[:, ch, 0:30], in_=glu[:, ch, 512:542]), [glu] + cyd, [glu])
        if emstop <= 1:
            continue
        for ch in range(4):
            ph.mm(pgi[:, :], ones, cy[:, ch, :], ch == 0, ch == 3, [cyd[ch]] + cd, [pgi])
        for ch in range(4):
            ph.act(lambda e, ch=ch: e.activation(out=sq[:, :], in_=cy[:, ch, :], func=AF.Square), [cyd[ch]], [sq])
            ph.mm(pgf[:, :], ones, sq[:, :], ch == 0, ch == 3, [sq] + cd, [pgf])
        ph.act(lambda e: e.activation(out=mean[:, :], in_=pgi[:, :], func=AF.Copy, scale=1.0 / 512), [pgi], [mean])
        ph.dve(lambda e: e.tensor_tensor(out=tmpa[:, :], in0=mean[:, :], in1=mean[:, :], op=ALU.mult), [mean], [tmpa])
        ph.dve(lambda e: e.scalar_tensor_tensor(out=var[:, :], in0=pgf[:, :], scalar=1.0 / 512, in1=tmpa[:, :], op0=ALU.mult, op1=ALU.subtract),
               [pgf, tmpa], [var])
        ph.dve(lambda e: e.tensor_scalar(out=var[:, :], in0=var[:, :], scalar1=EPS, scalar2=None, op0=ALU.add), [var], [var])
        ph.act(lambda e: e.activation(out=var[:, :], in_=var[:, :], func=AF.Sqrt), [var], [var])
        ph.dve(lambda e: e.reciprocal(out=var[:, :], in_=var[:, :]), [var], [var])
        for ch in range(4):
            ph.dve(lambda e, ch=ch: e.tensor_tensor(out=cy[:, ch, :], in0=cy[:, ch, :], in1=mean[:, :], op=ALU.subtract), [cyd[ch], mean], [cyd[ch]])
            ph.dve(lambda e, ch=ch: e.tensor_tensor(out=cy[:, ch, :], in0=cy[:, ch, :], in1=var[:, :], op=ALU.mult), [cyd[ch], var], [cyd[ch]])
            ph.act(lambda e, ch=ch: e.activation(out=mixT[:, ch, :], in_=cy[:, ch, :], func=AF.Silu, bias=pp[:, PE_AB + ch:PE_AB + ch + 1],
                                                 scale=pp[:, PE_AG + ch:PE_AG + ch + 1]), [cyd[ch], pp], [mixT])
        if emstop <= 2:
            continue
        for h in range(4):
            p = pj[kpj[0] % 2]; kpj[0] += 1
            for kc in range(8):
                ph.mm(p[:, :], win[:, kc, 1024 + h * 128:1024 + (h + 1) * 128], xt[:, kc, :], kc == 0, kc == 7, [win, xt], [p])
            ph.act(lambda e, p=p, h=h: e.activation(out=qT[:, h, :], in_=p[:, :], func=AF.Copy), [p], [qT])
            p = pj[kpj[0] % 2]; kpj[0] += 1
            for kc in range(8):
                ph.mm(p[:, :], win[:, kc, 1536 + h * 128:1536 + (h + 1) * 128], xt[:, kc, :], kc == 0, kc == 7, [win, xt], [p])
            ph.act(lambda e, p=p, h=h: e.activation(out=kT[:, h, :], in_=p[:, :], func=AF.Copy, scale=k128), [p], [kT])
        for sub in range(4):
            p = pj[kpj[0] % 2]; kpj[0] += 1
            for kc in range(8):
                ph.mm(p[:, :], xt[:, kc, sub * 128:(sub + 1) * 128], win[:, kc, 1536:2048], kc == 0, kc == 7, [win, xt], [p])
            ph.act(lambda e, p=p, sub=sub: e.activation(out=ktok[:, sub, :], in_=p[:, :], func=AF.Copy, scale=k128), [p], [ktok])
            p = pj[kpj[0] % 2]; kpj[0] += 1
            for kc in range(8):
                ph.mm(p[:, :], xt[:, kc, sub * 128:(sub + 1) * 128], win[:, kc, 2048:2560], kc == 0, kc == 7, [win, xt], [p])
            ph.dve(lambda e, p=p, sub=sub: e.tensor_copy(out=vext[:, sub, :, 0:128], in_=p[:, :].rearrange("p (h d) -> p h d", h=4)), [p], [vext])
            p = pj[kpj[0] % 2]; kpj[0] += 1
            for kc in range(8):
                ph.mm(p[:, :], xt[:, kc, sub * 128:(sub + 1) * 128], win[:, kc, 2560:3072], kc == 0, kc == 7, [win, xt], [p])
            ph.act(lambda e, p=p, sub=sub: e.activation(out=osig[:, sub, :], in_=p[:, :], func=AF.Sigmoid), [p], [osig])
        for kc in range(8):
            ph.mm(pgi[:, :], wgi[:, kc, :], xt[:, kc, :], kc == 0, kc == 7, [wgi, xt], [pgi])
        for kc in range(8):
            ph.mm(pgf[:, :], wgf[:, kc, :], xt[:, kc, :], kc == 0, kc == 7, [wgf, xt], [pgf])
        if emstop <= 3:
            continue
        G = [slice(32 * g, 32 * g + 4) for g in range(4)]
        ph.act(lambda e: e.activation(out=ig[:, :], in_=pgi[:, :], func=AF.Identity, bias=pp[:, PE_IB:PE_IB + 1]), [pgi, pp], [ig])
        ph.act(lambda e: e.activation(out=sp_[:, :], in_=pgf[:, :], func=AF.Exp, bias=nfb[:, :], scale=-1.0), [pgf, nfb], [sp_])
        ph.act(lambda e: e.activation(out=sp_[:, :], in_=sp_[:, :], func=AF.Ln, bias=1.0), [sp_], [sp_])
        ph.dve(lambda e: e.tensor_tensor_scan(out=Bc[:, :], data0=sc01, data1=sp_[:, :], initial=0.0, op0=ALU.mult, op1=ALU.add), [sp_] + cd, [Bc])
        ph.dve(lambda e: e.tensor_tensor(out=vt[:, :], in0=ig[:, :], in1=Bc[:, :], op=ALU.add), [ig, Bc], [vt])
        ph.dve(lambda e: e.tensor_tensor_scan(out=Mx[:, :], data0=scneg, data1=vt[:, :], initial=NEG, op0=ALU.add, op1=ALU.max), [vt] + cd, [Mx])
        ph.dve(lambda e: e.tensor_copy(out=ax[:, :], in_=Mx[:, :].rearrange("p (c t) -> p c t", c=4)[:, :, 127]), [Mx], [ax])
        ph.dve(lambda e: e.tensor_scalar(out=nax[:, :], in0=ax[:, :], scalar1=-1.0, scalar2=None, op0=ALU.mult), [ax], [nax])
        for cch in range(4):
            cs = slice(cch * 128, (cch + 1) * 128)
            ph.act(lambda e, cs=cs, cch=cch: e.activation(out=rowsT[G[0], cs], in_=vt[G[0], cs], func=AF.Exp, bias=nax[G[0], cch:cch + 1]), [vt, nax], [rowsT])
        for cch in range(4):
            cs = slice(cch * 128, (cch + 1) * 128)
            ph.dve(lambda e, cs=cs, cch=cch: e.tensor_scalar(out=lw[:, cs], in0=vt[:, cs], scalar1=Bc[:, cch * 128 + 127:cch * 128 + 128], scalar2=None,
                                                            op0=ALU.subtract), [vt, Bc], [lw])
        ph.dve(lambda e: e.tensor_reduce(out=am[:, :], in_=lw[:, :].rearrange("p (c t) -> p c t", c=4), axis=AX.X, op=ALU.max), [lw], [am])
        ph.dve(lambda e: e.tensor_scalar(out=nam[:, :], in0=am[:, :], scalar1=-1.0, scalar2=None, op0=ALU.mult), [am], [nam])
        for cch in range(4):
            cs = slice(cch * 128, (cch + 1) * 128)
            ph.act(lambda e, cs=cs, cch=cch: e.activation(out=rowsT[G[1], cs], in_=lw[G[1], cs], func=AF.Exp, bias=nam[G[1], cch:cch + 1]), [lw, nam], [rowsT])
        ph.dve(lambda e: e.tensor_copy(out=mp[:, 0:1], in_=mcar[:, :]), [mcar], [mp])
        for cch in range(4):
            be = Bc[:, cch * 128 + 127:cch * 128 + 128]
            ph.dve(lambda e, cch=cch, be=be: e.tensor_tensor(out=t1[:, :], in0=mp[:, cch:cch + 1], in1=be, op=ALU.subtract), [mp, Bc], [t1])
            ph.dve(lambda e, cch=cch: e.tensor_tensor(out=mp[:, cch + 1:cch + 2], in0=t1[:, :], in1=am[:, cch:cch + 1], op=ALU.max), [t1, am], [mp])
            ph.dve(lambda e, cch=cch: e.tensor_tensor(out=t1[:, :], in0=t1[:, :], in1=mp[:, cch + 1:cch + 2], op=ALU.subtract), [t1, mp], [t1])
            ph.dve(lambda e, cch=cch: e.tensor_tensor(out=t2[:, :], in0=am[:, cch:cch + 1], in1=mp[:, cch + 1:cch + 2], op=ALU.subtract), [am, mp], [t2])
            ph.act(lambda e, cch=cch: e.activation(out=sv[:, cch, 0:1], in_=t1[:, :], func=AF.Exp), [t1], [sv])
            ph.act(lambda e, cch=cch: e.activation(out=sv[:, cch, 1:2], in_=t2[:, :], func=AF.Exp), [t2], [sv])
        ph.dve(lambda e: e.tensor_copy(out=mcar[:, :], in_=mp[:, 4:5]), [mp], [mcar])
        for cch in range(4):
            cs = slice(cch * 128, (cch + 1) * 128)
            ph.dve(lambda e, cs=cs, cch=cch: e.tensor_scalar(out=U[:, cs], in0=Mx[:, cs], scalar1=mp[:, cch:cch + 1], scalar2=-1.0, op0=ALU.max, op1=ALU.mult),
                   [Mx, mp], [U])
            ph.act(lambda e, cs=cs, cch=cch: e.activation(out=rowsT[G[2], cs], in_=U[G[2], cs], func=AF.Exp, bias=mp[G[2], cch:cch + 1]), [U, mp], [rowsT])
            ph.act(lambda e, cs=cs, cch=cch: e.activation(out=rows2[G[2], cs], in_=U[G[2], cs], func=AF.Exp, bias=ax[G[2], cch:cch + 1]), [U, ax], [rows2])
            ph.dve(lambda e, cs=cs, cch=cch: e.tensor_scalar(out=rows2[G[0], cs], in0=c.t[G[0], CST_ONES:CST_ONES + 128], scalar1=sv[G[0], cch, 0:1], scalar2=None,
                                                            op0=ALU.mult), [sv] + cd, [rows2])
            ph.dve(lambda e, cs=cs, cch=cch: e.tensor_scalar(out=rows2[G[1], cs], in0=c.t[G[1], CST_ONES:CST_ONES + 128], scalar1=sv[G[1], cch, 1:2], scalar2=None,
                                                            op0=ALU.mult), [sv] + cd, [rows2])
        ph.dve(lambda e: e.tensor_tensor(out=tmpr[:, :], in0=Bc[:, :], in1=U[:, :], op=ALU.add), [Bc, U], [tmpr])
        ph.act(lambda e: e.activation(out=rowsT[G[3], :], in_=tmpr[G[3], :], func=AF.Exp), [tmpr], [rowsT])
        if emstop <= 4:
            continue
        for sub in range(4):
            ph.tr(pm[:, 256:384], rowsT[:, sub * 128:(sub + 1) * 128], idf[:, :], [rowsT, idf], [pm_cols])
            ph.act(lambda e, sub=sub: e.activation(out=cols[:, sub, :], in_=pm[:, 256:384], func=AF.Copy), [pm_cols], [cols])
            ph.tr(pm[:, 384:512], rows2[:, sub * 128:(sub + 1) * 128], idf[:, :], [rows2, idf], [pm_sc])
            ph.act(lambda e, sub=sub: e.activation(out=scol[:, sub, :], in_=pm[:, 384:512], func=AF.Copy), [pm_sc], [scol])
        if emstop <= 5:
            continue
        hstop = int(os.environ.get("HSTOP", "99"))
        for sub in range(4):
            cs = slice(sub * 128, (sub + 1) * 128)
            H4 = range(4)
            for h in H4:
                ph.mm(pm[:, h * 128:(h + 1) * 128], kT[:, h, cs], qT[:, h, cs], True, True, [kT, qT], [pm])
            for h in H4:
                ph.dve(lambda e, h=h: e.tensor_tensor(out=PTs[h][:, :], in0=pm[:, h * 128:(h + 1) * 128], in1=c.t[:, CST_MT01:CST_MT01 + 128], op=ALU.mult), [pm] + cd, [PTs[h]])
                ph.pool(lambda e, sub=sub, h=h: e.tensor_scalar(out=vscs[h][:, 0:130], in0=vext[:, sub, h, 0:130], scalar1=cols[:, sub, h:h + 1], scalar2=None, op0=ALU.mult),
                        [vext, cols], [vscs[h]])
            for h in H4:
                ph.mm(pnb[h][:, 0:129], PTs[h][:, :], vscs[h][:, 0:129], True, True, [PTs[h], vscs[h]], [pnb[h]])
                ph.mm(pnb[h][:, 129:258], qT[:, h, cs], Cb[:, h, 0:129], True, True, [qT, Cb], [pnb[h]])
            for h in H4:
                ph.act(lambda e, sub=sub, h=h: e.activation(out=tis[h][:, :], in_=pnb[h][:, 129:258], func=AF.Copy, scale=cols[:, sub, 64 + h:64 + h + 1]), [pnb[h], cols], [tis[h]])
            for h in H4:
                ph.dve(lambda e, sub=sub, h=h: e.scalar_tensor_tensor(out=tots[h][:, :], in0=pnb[h][:, 0:129], scalar=scol[:, sub, 64 + h:64 + h + 1], in1=tis[h][:, :],
                                                                      op0=ALU.mult, op1=ALU.add), [pnb[h], tis[h], scol], [tots[h]])
            for h in H4:
                ph.act(lambda e, h=h: e.activation(out=dd4[:, h:h + 1], in_=tots[h][:, 128:129], func=AF.Abs), [tots[h]], [dd4])
            ph.dve(lambda e, sub=sub: e.tensor_tensor(out=dd4[:, :], in0=dd4[:, :], in1=cols[:, sub, 96:100], op=ALU.max), [dd4, cols], [dd4])
            ph.dve(lambda e: e.reciprocal(out=rec4[:, :], in_=dd4[:, :]), [dd4], [rec4])
            for h in H4:
                ph.dve(lambda e, h=h: e.tensor_scalar(out=hraw[:, h * 128:(h + 1) * 128], in0=tots[h][:, 0:128], scalar1=rec4[:, h:h + 1], scalar2=None, op0=ALU.mult),
                       [tots[h], rec4], [hraw])
            for h in H4:
                ph.dve(lambda e, sub=sub, h=h: e.tensor_scalar(out=kws[h][:, :], in0=ktok[:, sub, h * 128:(h + 1) * 128], scalar1=cols[:, sub, 32 + h:32 + h + 1],
                                                               scalar2=None, op0=ALU.mult), [ktok, cols], [kws[h]])
            for h in H4:
                ph.mm(pnb[h][:, 258:387], kws[h][:, :], vext[:, sub, h, 0:129], True, True, [kws[h], vext], [pnb[h]])
            for h in H4:
                ph.dve(lambda e, sub=sub, h=h: e.tensor_scalar(out=Cf[:, h, 0:129], in0=Cf[:, h, 0:129], scalar1=scol[:, sub, h:h + 1], scalar2=None, op0=ALU.mult),
                       [Cfd[h], scol], [Cfd[h]])
                ph.dve(lambda e, sub=sub, h=h: e.scalar_tensor_tensor(out=Cf[:, h, 0:129], in0=pnb[h][:, 258:387], scalar=scol[:, sub, 32 + h:32 + h + 1], in1=Cf[:, h, 0:129],
                                                                      op0=ALU.mult, op1=ALU.add), [pnb[h], scol, Cfd[h]], [Cfd[h]])
            for h in H4:
                ph.act(lambda e, h=h: e.activation(out=Cb[:, h, 0:129], in_=Cf[:, h, 0:129], func=AF.Copy), [Cfd[h]], [Cb])
            if hstop <= 4:
                continue
            for h in range(4):
                ph.dve(lambda e, h=h: e.bn_stats(out=hst[:, h, :], in_=hraw[:, h * 128:(h + 1) * 128]), [hraw], [hst])
                ph.dve(lambda e, h=h: e.bn_aggr(out=hmv[:, h, :], in_=hst[:, h, :]), [hst], [hmv])
            ph.dve(lambda e: e.tensor_scalar(out=hrs[:, :], in0=hmv[:, :, 1], scalar1=EPS, scalar2=None, op0=ALU.add), [hmv], [hrs])
            ph.act(lambda e: e.activation(out=hrs[:, :], in_=hrs[:, :], func=AF.Sqrt), [hrs], [hrs])
            ph.dve(lambda e: e.reciprocal(out=hrs[:, :], in_=hrs[:, :]), [hrs], [hrs])
            for h in range(4):
                ph.dve(lambda e, h=h: e.tensor_scalar(out=hraw[:, h * 128:(h + 1) * 128], in0=hraw[:, h * 128:(h + 1) * 128], scalar1=hmv[:, h, 0:1],
                                                      scalar2=hrs[:, h:h + 1], op0=ALU.subtract, op1=ALU.mult), [hraw, hmv, hrs], [hraw])
            ph.pool(lambda e: e.tensor_tensor(out=hraw[:, :], in0=hraw[:, :], in1=hg[:, :], op=ALU.mult), [hraw, hg], [hraw])
            ph.dve(lambda e, sub=sub: e.tensor_tensor(out=hb[:, :], in0=hraw[:, :], in1=osig[:, sub, :], op=ALU.mult), [hraw, osig], [hb])
            for h in range(4):
                ph.tr(trp[:, h * 128:(h + 1) * 128], hb[:, h * 128:(h + 1) * 128], idb[:, :], [hb, idb], [trp])
            ph.act(lambda e, cs=cs: e.activation(out=mixT[:, 4:8, cs], in_=trp[:, 0:512].rearrange("p (c t) -> p c t", c=4), func=AF.Copy), [trp], [mixT])
        if emstop <= 6:
            continue
        for sub in range(4):
            n = tt * 4 + sub
            for half in range(2):
                for kc in range(8):
                    ph.mm(pj[half][:, :], mixT[:, kc, sub * 128:(sub + 1) * 128], wout[:, kc, half * 512:(half + 1) * 512], kc == 0, kc == 7, [mixT, wout], [pj[half]])
            epi.run(n, lambda lo, hi: pj[lo // 512][:, :], [pj[0], pj[1]], idb, idf, trp, None, None)
    ph.finish()


def phase_odd_mixer(nc, cx, name, j):
    ph = Phase(nc, name)
    mk_deps(ph, cx)
    c, idb = load_consts(ph, cx, 640)
    cd = [c]
    idf = Buf(c.t[:, 0:128], c.d)
    S = cx.S
    win = ph.sb("win", [128, 8, 2560], BF16)
    ph.dma("pool", win[:, :, :], cx.cd_w_in[j].rearrange("(c p) n -> p c n", p=128), [], [win], win)
    wout = ph.sb("wout", [128, 8, D], BF16)
    ph.dma("pool", wout[:, :, :], cx.cd_w_out[j].rearrange("(c p) n -> p c n", p=128), [], [wout], wout)
    ri = cx.row_idx
    cng = ph.sb("cng", [128, 512]); load_rows(ph, "sp", cng, cx.rows[ri[("cng", j)]:ri[("cng", j)] + 1, 0:512], 512)
    cnb = ph.sb("cnb", [128, 512]); load_rows(ph, "sp", cnb, cx.rows[ri[("cnb", j)]:ri[("cnb", j)] + 1, 0:512], 512)
    bsb = ph.sb("bsb", [128, 512]); load_rows(ph, "sp", bsb, cx.rows[ri[("bs", j)]:ri[("bs", j)] + 1, 0:512], 512)
    wsf = ph.sb("wsf", [128, 4, 128])
    ph.dma("sp", wsf[:, :, :], cx.wst[j].rearrange("g s t -> s g t"), [], [wsf], wsf)
    WcT = ph.sb("WcT", [128, 4, 128], BF16)
    for g in range(4):
        ph.dve(lambda e, g=g: e.tensor_tensor(out=WcT[:, g, :], in0=wsf[:, g, :], in1=c.t[:, CST_MT01:CST_MT01 + 128], op=ALU.mult), [wsf] + cd, [WcT])
    epi = Epi(ph, cx, cx.rows[ri[("oln1g", j)]:ri[("oln1g", j)] + 1, :], cx.rows[ri[("oln1b", j)]:ri[("oln1b", j)] + 1, :], False,
              router={"w": cx.router_w[j], "b": cx.rows[ri[("rb", j)]:ri[("rb", j)] + 1, 0:NE]}, nbuf=1)
    kTres = ph.sb("kTres", [128, 4, S], BF16)
    Vres = ph.sb("Vres", [128, cx.NT, 512], BF16)
    pj = [ph.ps("pj%d" % i, [128, 512]) for i in range(2)]
    pz = [ph.ps("pz%d" % i, [128, 512]) for i in range(2)]
    pg = ph.ps("pg", [128, 512]); pos = [ph.ps("po%d" % i, [128, 512]) for i in range(2)]
    trp = ph.ps("trp", [128, 1024], BF16)
    xt = ph.sb("xTt", [128, 8, 512], BF16)
    uT = ph.sb("uT", [128, 4, 512], BF16)
    zf = ph.sb("zf", [128, 512]); zb = ph.sb("zb", [128, 512], BF16)
    zst = ph.sb("zst", [128, 1, 6]); zmv = ph.sb("zmv", [128, 2]); zrs = ph.sb("zrs", [128, 1]); znb = ph.sb("znb", [128, 1])
    gtmp = zf
    mixT = ph.sb("mixT", [128, 8, 512], BF16)
    qT = ph.sb("qT", [128, 4, 512], BF16)
    NB = 2
    ebuf = [ph.sb("ebuf%d" % i, [128, 512]) for i in range(NB)]
    spb = [ph.sb("spb%d" % i, [128, 513]) for i in range(NB)]
    csb = [ph.sb("csb%d" % i, [128, 513]) for i in range(NB)]
    attb = [ph.sb("attb%d" % i, [128, 512], BF16) for i in range(NB)]
    attT = ph.sb("attT", [128, 2, 4, 128], BF16)
    for i in range(NB):
        ph.dve(lambda e, i=i: e.memset(spb[i][:, 0:1], 0.0), [], [spb[i]])
    cars = [ph.sb("car%d" % i, [128, 1]) for i in range(NB)]
    nbb = [ph.sb("nbb%d" % i, [128, 1]) for i in range(NB)]
    dout = ph.sb("dout", [128, 512], BF16)
    onesr = c.t[:, CST_ONES:CST_ONES + 128]
    ones513 = ph.sb("ones513", [128, 513], BF16)
    ph.dve(lambda e: e.memset(ones513[:, :], 1.0), [], [ones513])
    m01s = c.t[:, CST_M01S:CST_M01S + 128]
    kpj = [0]
    kz = [0]
    import os
    for tt in range(int(os.environ.get("OMT", cx.NTT))):
        t0 = tt * 512
        ph.dma("sp", xt[:, :, :], cx.xT.rearrange("c p s -> p c s")[:, :, t0:t0 + 512], [cx.xT_dep[tt]], [xt], xt)
        for ch in range(4):
            p = pj[kpj[0] % 2]; kpj[0] += 1
            for kc in range(8):
                ph.mm(p[:, :], win[:, kc, ch * 128:(ch + 1) * 128], xt[:, kc, :], kc == 0, kc == 7, [win, xt], [p])
            ph.act(lambda e, p=p, ch=ch: e.activation(out=uT[:, ch, :], in_=p[:, :], func=AF.Gelu), [p], [uT])
        for pr in range(4):
            p = pj[kpj[0] % 2]; kpj[0] += 1
            for kc in range(8):
                ph.mm(p[:, :], win[:, kc, 1024 + pr * 128:1024 + (pr + 1) * 128], xt[:, kc, :], kc == 0, kc == 7, [win, xt], [p])
            ph.act(lambda e, p=p, pr=pr: e.activation(out=qT[:, pr, :], in_=p[:, :], func=AF.Copy, scale=0.125), [p], [qT])
            p = pj[kpj[0] % 2]; kpj[0] += 1
            for kc in range(8):
                ph.mm(p[:, :], win[:, kc, 1536 + pr * 128:1536 + (pr + 1) * 128], xt[:, kc, :], kc == 0, kc == 7, [win, xt], [p])
            ph.dve(lambda e, p=p, pr=pr, t0=t0: e.tensor_copy(out=kTres[:, pr, t0:t0 + 512], in_=p[:, :]), [p], [kTres])
        for sub in range(4):
            n = tt * 4 + sub
            cs = slice(sub * 128, (sub + 1) * 128)
            p = pj[kpj[0] % 2]; kpj[0] += 1
            for kc in range(8):
                ph.mm(p[:, :], xt[:, kc, cs], win[:, kc, 2048:2560], kc == 0, kc == 7, [win, xt], [p])
            ph.act(lambda e, p=p, n=n: e.activation(out=Vres[:, n, :], in_=p[:, :], func=AF.Copy), [p], [Vres])
            p = pj[kpj[0] % 2]; kpj[0] += 1
            for kc in range(8):
                ph.mm(p[:, :], xt[:, kc, cs], win[:, kc, 512:1024], kc == 0, kc == 7, [win, xt], [p])
            ph.act(lambda e, p=p: e.activation(out=zf[:, :], in_=p[:, :], func=AF.Gelu), [p], [zf])
            ln_rows(ph, zf, zmv, zst, zrs, znb, 512)
            ph.act(lambda e: e.activation(out=zf[:, :], in_=zf[:, :], func=AF.Identity, bias=znb[:, :], scale=zrs[:, :]), [zf, znb, zrs], [zf])
            ph.pool(lambda e: e.tensor_tensor(out=zf[:, :], in0=zf[:, :], in1=cng[:, :], op=ALU.mult), [zf, cng], [zf])
            ph.pool(lambda e: e.tensor_tensor(out=zb[:, :], in0=zf[:, :], in1=cnb[:, :], op=ALU.add), [zf, cnb], [zb])
            for g in range(4):
                ph.mm(pg[:, g * 128:(g + 1) * 128], zb[:, g * 128:(g + 1) * 128], WcT[:, g, :], True, True, [zb, WcT], [pg])
            ph.dve(lambda e: e.tensor_tensor(out=gtmp[:, :], in0=pg[:, :], in1=bsb[:, :], op=ALU.add), [pg, bsb], [gtmp])
            ph.dve(lambda e, cs=cs: e.tensor_tensor(out=mixT[:, 0:4, cs], in0=gtmp[:, :].rearrange("p (g t) -> p g t", g=4), in1=uT[:, :, cs], op=ALU.mult),
                   [gtmp, uT], [mixT])
            kend = (n + 1) * 128
            KT = (kend + 511) // 512
            for pr in range(4):
                J = (0, 1)
                for j in J:
                    ph.dve(lambda e, j=j: e.memset(cars[j][:, :], 0.0), [], [cars[j]])
                first = [True, True]
                steps = []
                for kt in reversed(range(KT)):
                    k0 = kt * 512
                    w = min(kend, k0 + 512) - k0
                    steps.append((kt, k0, w, w // 128))

                def emit_z(st):
                    kt_, k0_, w_, _ = st
                    for j in J:
                        b0 = j * 64
                        ph.mm(pz[j][:, 0:w_], qT[b0:b0 + 64, pr, cs], kTres[b0:b0 + 64, pr, k0_:k0_ + w_], True, True, [qT, kTres], [pz[j]])

                emit_z(steps[0])
                for si, (kt, k0, w, nblk) in enumerate(steps):
                    for j in J:
                        ph.act(lambda e, j=j, w=w: e.activation(out=ebuf[j][:, 0:w], in_=pz[j][:, 0:w], func=AF.Exp), [pz[j]], [ebuf[j]])
                    if si + 1 < len(steps):
                        emit_z(steps[si + 1])
                    for j in J:
                        ph.act(lambda e, j=j, w=w: e.activation(out=spb[j][:, 1:w + 1], in_=ebuf[j][:, 0:w], func=AF.Ln, bias=1.0), [ebuf[j]], [spb[j]])
                    if kt == KT - 1:
                        for j in J:
                            ph.pool(lambda e, j=j, w=w: e.tensor_tensor(out=spb[j][:, 1 + w - 128:1 + w], in0=spb[j][:, 1 + w - 128:1 + w], in1=m01s, op=ALU.mult),
                                    [spb[j]] + cd, [spb[j]])
                            ph.pool(lambda e, j=j, w=w: e.tensor_tensor(out=ebuf[j][:, w - 128:w], in0=ebuf[j][:, w - 128:w], in1=m01s, op=ALU.mult),
                                    [ebuf[j]] + cd, [ebuf[j]])
                    for j in J:
                        ph.dve(lambda e, j=j, w=w: e.tensor_tensor_scan(out=csb[j][:, 0:w + 1], data0=ones513[:, 0:w + 1], data1=spb[j][:, 0:w + 1], initial=0.0,
                                                                       op0=ALU.mult, op1=ALU.add), [spb[j], ones513], [csb[j]])
                    for j in J:
                        ph.dve(lambda e, j=j, w=w: e.tensor_tensor(out=cars[j][:, :], in0=csb[j][:, w:w + 1], in1=cars[j][:, :], op=ALU.add), [csb[j], cars[j]], [cars[j]])
                        ph.dve(lambda e, j=j: e.tensor_scalar(out=nbb[j][:, :], in0=cars[j][:, :], scalar1=-1.0, scalar2=None, op0=ALU.mult), [cars[j]], [nbb[j]])
                    for j in J:
                        ph.act(lambda e, j=j, w=w: e.activation(out=csb[j][:, 0:w], in_=csb[j][:, 0:w], func=AF.Exp, bias=nbb[j][:, :]), [csb[j], nbb[j]], [csb[j]])
                    for j in J:
                        ph.pool(lambda e, j=j, w=w: e.tensor_tensor(out=attb[j][:, 0:w], in0=ebuf[j][:, 0:w], in1=csb[j][:, 0:w], op=ALU.mult), [ebuf[j], csb[j]], [attb[j]])
                    for j in J:
                        for jb in range(nblk):
                            ph.tr(trp[:, j * 512 + jb * 128:j * 512 + (jb + 1) * 128], attb[j][:, jb * 128:(jb + 1) * 128], idb[:, :], [attb[j], idb], [trp])
                    ph.dve(lambda e, nblk=nblk: e.tensor_copy(out=attT[:, :, 0:nblk, :], in_=trp[:, :].rearrange("p (j b t) -> p j b t", j=2, b=4)[:, :, 0:nblk, :]),
                           [trp], [attT])
                    for j in J:
                        h = 2 * pr + j
                        for jb in range(nblk):
                            kb = kt * 4 + jb
                            last = (kt == 0 and jb == nblk - 1)
                            ph.mm(pos[j][:, h * 64:(h + 1) * 64], attT[:, j, jb, :], Vres[:, kb, h * 64:(h + 1) * 64], first[j], last, [attT, Vres], [pos[j]])
                            first[j] = False
            for j in (0, 1):
                ph.act(lambda e, j=j: e.activation(out=dout[:, :].rearrange("p (r j d) -> p r j d", r=4, j=2)[:, :, j, :],
                                                   in_=pos[j][:, :].rearrange("p (r j d) -> p r j d", r=4, j=2)[:, :, j, :], func=AF.Copy), [pos[j]], [dout])
            for cc in range(4):
                ph.tr(trp[:, cc * 128:(cc + 1) * 128], dout[:, cc * 128:(cc + 1) * 128], idb[:, :], [dout, idb], [trp])
            ph.dve(lambda e, cs=cs: e.tensor_copy(out=mixT[:, 4:8, cs], in_=trp[:, 0:512].rearrange("p (c t) -> p c t", c=4)), [trp], [mixT])
        for sub in range(4):
            n = tt * 4 + sub
            for half in range(2):
                for kc in range(8):
                    ph.mm(pj[half][:, :], mixT[:, kc, sub * 128:(sub + 1) * 128], wout[:, kc, half * 512:(half + 1) * 512], kc == 0, kc == 7, [mixT, wout], [pj[half]])
            epi.run(n, lambda lo, hi: pj[lo // 512][:, :], [pj[0], pj[1]], idb, idf, trp, pz, pg)
    ph.finish()


def mk_deps(ph, cx):
    cx.acc_dep = [ph.region("acc%d" % n) for n in range(cx.NT)]
    cx.out_dep = [ph.region("out%d" % n) for n in range(cx.NT)]
    cx.xT_dep = [ph.region("xT%d" % t) for t in range(cx.NTT)]
    cx.comb_dep = ph.region("comb")


ROW_KEYS_EVEN = ["eln1g", "eln1b", "eln2g", "eln2b", "hg"]
ROW_KEYS_ODD = ["oln1g", "oln1b", "oln2g", "oln2b", "cng", "cnb", "bs", "rb"]


def row_index():
    idx = {}
    k = 0
    for j in range(2):
        for key in ROW_KEYS_EVEN + ROW_KEYS_ODD:
            idx[(key, j)] = k
            k += 1
    return idx, k


def build(S, layers):
    nc = bass.Bass("TRN2", target_bir_lowering=False)
    gst = ExitStack()
    make_sempool(nc, gst)
    nc._gst = gst
    cx = Ctx()
    cx.S = S; cx.NT = S // 128; cx.NTT = S // 512; cx.NCST = NCST
    cx.row_idx, nrows = row_index()

    def inp(name, shape):
        return nc.dram_tensor(name, list(shape), F32, kind="ExternalInput").ap()

    cx.x = inp("x", [S, D])
    cx.out = nc.dram_tensor("out", [S, D], F32, kind="ExternalOutput").ap()
    cx.cst = inp("cst", [128, NCST])
    cx.rows = inp("rows", [nrows, D])
    cx.pe = [inp("pe0", [128, NPE]), inp("pe1", [128, NPE])]
    cx.wst = inp("wst", [2, 4, 128, 128])
    cx.wkeys = []
    if any(l % 2 == 0 for l in layers):
        cx.ab_w_in = inp("ab_w_in", [2, D, 3080]); cx.ab_w_out = inp("ab_w_out", [2, D, D])
        cx.ffn_w1 = inp("ffn_w1", [2, D, DFF]); cx.ffn_w3 = inp("ffn_w3", [2, D, DFF]); cx.ffn_w2 = inp("ffn_w2", [2, DFF, D])
        cx.wkeys += ["ab_w_in", "ab_w_out", "ffn_w1", "ffn_w3", "ffn_w2"]
    if any(l % 2 == 1 for l in layers):
        cx.cd_w_in = inp("cd_w_in", [2, D, 2560]); cx.cd_w_out = inp("cd_w_out", [2, D, D])
        cx.router_w = inp("router_w", [2, D, NE])
        cx.moe_w1 = inp("moe_w1", [2, NE, D, DFF]); cx.moe_w3 = inp("moe_w3", [2, NE, D, DFF]); cx.moe_w2 = inp("moe_w2", [2, NE, DFF, D])
        cx.wkeys += ["cd_w_in", "cd_w_out", "router_w", "moe_w1", "moe_w3", "moe_w2"]
    nc._wkeys = cx.wkeys
    cx.acc = nc.dram_tensor("acc", [S, D], F32).ap()
    cx.xT = nc.dram_tensor("xT", [8, 128, S], BF16).ap()
    cx.comb = nc.dram_tensor("comb", [S, NE], F32).ap()
    ri = cx.row_idx
    import os
    kstop = int(os.environ.get("KSTOP", "99"))
    phase_prologue(nc, cx)
    if kstop <= 0:
        return nc
    for li, l in enumerate(layers):
        j = l // 2
        last = (li == len(layers) - 1)
        if l % 2 == 0:
            phase_even_mixer(nc, cx, "em%d" % l, j)
            if kstop <= 1:
                return nc
            phase_ffn(nc, cx, "ef%d" % l, [cx.ffn_w1[j]], [cx.ffn_w3[j]], [cx.ffn_w2[j]],
                      cx.rows[ri[("eln2g", j)]:ri[("eln2g", j)] + 1, :], cx.rows[ri[("eln2b", j)]:ri[("eln2b", j)] + 1, :], last, False)
        else:
            phase_odd_mixer(nc, cx, "om%d" % l, j)
            if kstop <= 1:
                phase_dump(nc, cx)
                return nc
            phase_ffn(nc, cx, "of%d" % l, [cx.moe_w1[j][e] for e in range(NE)], [cx.moe_w3[j][e] for e in range(NE)],
                      [cx.moe_w2[j][e] for e in range(NE)],
                      cx.rows[ri[("oln2g", j)]:ri[("oln2g", j)] + 1, :], cx.rows[ri[("oln2b", j)]:ri[("oln2b", j)] + 1, :], last, True)
    return nc


def host_consts():
    c = np.zeros((128, NCST), np.float32)
    i = np.arange(128)
    c[:, CST_ID:CST_ID + 128] = np.eye(128, dtype=np.float32)
    c[:, CST_ONES:CST_ONES + 128] = 1.0
    c[:, CST_MTNEG:CST_MTNEG + 128] = np.where(i[:, None] <= i[None, :], 0.0, NEG)
    c[:, CST_M01S:CST_M01S + 128] = (i[None, :] < i[:, None]).astype(np.float32)
    c[:, CST_MT01:CST_MT01 + 128] = (i[:, None] <= i[None, :]).astype(np.float32)
    for h in range(4):
        c[h, CST_SEL + h * 128:CST_SEL + (h + 1) * 128] = 1.0
    t = np.arange(512)
    c[:, CST_SC01:CST_SC01 + 512] = (t % 128 != 0).astype(np.float32)[None, :]
    c[:, CST_SCNEG:CST_SCNEG + 512] = np.where(t % 128 == 0, NEG, 0.0)[None, :]
    return c


def host_layout(inp):
    f = lambda a: np.asarray(a, dtype=np.float32)
    idx, nrows = row_index()
    rows = np.zeros((nrows, D), np.float32)

    def put(key, j, v):
        v = f(v).reshape(-1)
        rows[idx[(key, j)], :v.size] = v

    pes = []
    for j in range(2):
        put("eln1g", j, inp["ab_ln1_g"][j]); put("eln1b", j, inp["ab_ln1_b"][j])
        put("eln2g", j, inp["ab_ln2_g"][j]); put("eln2b", j, inp["ab_ln2_b"][j])
        put("hg", j, inp["b_norm_g"][j])
        put("oln1g", j, inp["cd_ln1_g"][j]); put("oln1b", j, inp["cd_ln1_b"][j])
        put("oln2g", j, inp["cd_ln2_g"][j]); put("oln2b", j, inp["cd_ln2_b"][j])
        put("cng", j, inp["c_norm_g"][j]); put("cnb", j, inp["c_norm_b"][j])
        put("bs", j, inp["c_b_s"][j]); put("rb", j, inp["router_b"][j])
        pe = np.zeros((128, NPE), np.float32)
        cw = f(inp["a_conv_w"][j])
        pe[:, PE_CW:PE_CW + 124] = cw.reshape(31, 4, 128).transpose(2, 1, 0).reshape(128, 124)
        pe[:, PE_CB:PE_CB + 4] = f(inp["a_conv_b"][j]).reshape(4, 128).T
        pe[:, PE_AG:PE_AG + 4] = f(inp["a_norm_g"][j]).reshape(4, 128).T
        pe[:, PE_AB:PE_AB + 4] = f(inp["a_norm_b"][j]).reshape(4, 128).T
        gb = f(inp["ab_gate_bias"][j])
        for g in range(4):
            pe[32 * g:32 * g + 4, PE_IB] = gb[0:4]
            pe[32 * g:32 * g + 4, PE_FB] = gb[4:8]
        pes.append(pe)
    wst = np.ascontiguousarray(f(inp["c_w_s"]).transpose(0, 1, 3, 2))
    return rows, pes, wst


WKEYS = ["ab_w_in", "ab_w_out", "ffn_w1", "ffn_w3", "ffn_w2", "cd_w_in", "cd_w_out", "router_w", "moe_w1", "moe_w3", "moe_w2"]
_CACHE = {}


def run(inputs, S, layers, ncores):
    key = (S, tuple(layers))
    if key not in _CACHE:
        _CACHE[key] = build(S, layers)
    nc = _CACHE[key]
    rows, pes, wst = host_layout(inputs)
    cst = host_consts()
    shared = {"cst": cst, "rows": rows, "pe0": pes[0], "pe1": pes[1], "wst": wst}
    for k in nc._wkeys:
        shared[k] = np.ascontiguousarray(np.asarray(inputs[k], dtype=np.float32))
    x = np.asarray(inputs["x"], dtype=np.float32)
    in_maps = []
    for c in range(ncores):
        m = dict(shared)
        m["x"] = np.ascontiguousarray(x[c, :S, :])
        in_maps.append(m)
    import os
    if os.environ.get("KTRACE"):
        res = run_bass_kernel_spmd(nc, in_maps, core_ids=list(range(ncores)), trace=True)
        print("EXEC_NS", res.exec_time_ns)
    else:
        res = run_bass_kernel_spmd(nc, in_maps, core_ids=list(range(ncores)))
    return np.stack([res.results[c]["out"] for c in range(ncores)], axis=0)


def kernel(**inputs):
    return run(inputs, 4096, [0, 1, 2, 3], 8)


def phase_dump(nc, cx):
    ph = Phase(nc, "dump")
    mk_deps(ph, cx)
    t = ph.sb("t", [128, D])
    for n in range(cx.NT):
        ph.dma("sp", t[:, :], cx.acc[n * 128:(n + 1) * 128, :], [], [t], t)
        ph.dma("sp", cx.out[n * 128:(n + 1) * 128, :], t[:, :], [t], [cx.out_dep[n]], t)
    ph.finish()
```
